# Optimizing a Trainium2 kernel written in Bass

```python
import jax, jax.numpy as jnp
from jax import lax
import numpy as np

D_MODEL = 1024
BATCH = 8
SEQ = 2048
DEPTH = 4

GLA_HEADS = 4
GLA_DK = D_MODEL // 2
GLA_DV = D_MODEL
GLA_HEAD_DK = GLA_DK // GLA_HEADS
GLA_HEAD_DV = GLA_DV // GLA_HEADS
GLA_RANK = 16
GLA_GATE_TAU = 16.0
GLA_CHUNK = 64
LRU_WIDTH = D_MODEL
LRU_BLOCKS = 4
LRU_BLOCK = LRU_WIDTH // LRU_BLOCKS
CONV_WIDTH = 4
LRU_C = 8.0
SB_HEAD_DIM = 128
SB_HEADS = D_MODEL // SB_HEAD_DIM
SB_DIM = SB_HEADS * SB_HEAD_DIM
SB_QBLOCK = 128
N_BRANCHES = 3
D_FF = 4 * D_MODEL
EPS = 1e-6
IN_SIZES = (GLA_DK, GLA_DK, GLA_DV, GLA_DV, GLA_RANK, LRU_WIDTH, LRU_WIDTH, SB_DIM, SB_DIM, SB_DIM, N_BRANCHES * D_MODEL)
IN_COLS = 2 * GLA_DK + 2 * GLA_DV + GLA_RANK + 2 * LRU_WIDTH + 3 * SB_DIM + N_BRANCHES * D_MODEL

kernel_name = "hybrid_gla_rglru_stickbreaking_block"


def rms_norm(x, g):
    x32 = x.astype(jnp.float32)
    y = x32 * lax.rsqrt(jnp.mean(x32 * x32, axis=-1, keepdims=True) + EPS)
    return (y * g.astype(jnp.float32)).astype(x.dtype)


def split_heads(t, n_heads):
    b, s, _ = t.shape
    return t.reshape(b, s, n_heads, -1).transpose(0, 2, 1, 3)


def merge_heads(t):
    b, h, s, d = t.shape
    return t.transpose(0, 2, 1, 3).reshape(b, s, h * d)


def gla_mixer(q, k, v, g_out, a_down, w_up, b_alpha, norm_g):
    f32 = jnp.float32
    b, s, _ = q.shape
    n = s // GLA_CHUNK
    log_alpha = jax.nn.log_sigmoid((a_down @ w_up + b_alpha).astype(f32)) / GLA_GATE_TAU

    def chunks(t):
        return split_heads(t, GLA_HEADS).reshape(b, GLA_HEADS, n, GLA_CHUNK, -1)

    qc = chunks(q.astype(f32)) * (GLA_HEAD_DK ** -0.5)
    kc = chunks(k.astype(f32))
    vc = chunks(v.astype(f32))
    cum = jnp.cumsum(chunks(log_alpha), axis=3)
    last = cum[:, :, :, -1:, :]
    q_dec = qc * jnp.exp(cum)
    k_inv = kc * jnp.exp(-cum)
    k_end = kc * jnp.exp(last - cum)
    pos = jnp.arange(GLA_CHUNK)
    causal = pos[:, None] >= pos[None, :]
    scores = jnp.where(causal, jnp.einsum('bhnti,bhnsi->bhnts', q_dec, k_inv), 0.0)
    o_intra = jnp.einsum('bhnts,bhnsj->bhntj', scores, vc)
    kv_chunk = jnp.einsum('bhnsi,bhnsj->nbhij', k_end, vc)
    decay = jnp.exp(last[:, :, :, 0, :]).transpose(2, 0, 1, 3)

    def step(state, inp):
        d, kv = inp
        return d[..., None] * state + kv, state

    init = jnp.zeros((b, GLA_HEADS, GLA_HEAD_DK, GLA_HEAD_DV), f32)
    _, s_prev = lax.scan(step, init, (decay, kv_chunk))
    o_inter = jnp.einsum('bhnti,nbhij->bhntj', q_dec, s_prev)
    o = (o_intra + o_inter).reshape(b, GLA_HEADS, s, GLA_HEAD_DV)
    o = o * lax.rsqrt(jnp.mean(o * o, axis=-1, keepdims=True) + EPS) * norm_g.astype(f32)
    o = merge_heads(o) * jax.nn.silu(g_out.astype(f32))
    return o.astype(q.dtype)


def rglru_mixer(x_in, gate_in, conv_w, conv_b, w_a, b_a, w_x, b_x, lam):
    f32 = jnp.float32
    xc = lax.conv_general_dilated(
        x_in, conv_w[:, None, :], window_strides=(1,), padding=[(CONV_WIDTH - 1, 0)],
        dimension_numbers=('NWC', 'WIO', 'NWC'), feature_group_count=LRU_WIDTH) + conv_b
    b, s, _ = xc.shape
    xb = xc.reshape(b, s, LRU_BLOCKS, LRU_BLOCK)
    r = jax.nn.sigmoid((jnp.einsum('bthi,hij->bthj', xb, w_a).reshape(b, s, LRU_WIDTH) + b_a).astype(f32))
    i = jax.nn.sigmoid((jnp.einsum('bthi,hij->bthj', xb, w_x).reshape(b, s, LRU_WIDTH) + b_x).astype(f32))
    log_a = -LRU_C * r * jax.nn.softplus(-lam.astype(f32))
    a = jnp.exp(log_a)
    u = jnp.sqrt(-jnp.expm1(2.0 * log_a)) * i * xc.astype(f32)

    def combine(left, right):
        a1, b1 = left
        a2, b2 = right
        return a1 * a2, a2 * b1 + b2

    _, h = lax.associative_scan(combine, (a, u), axis=1)
    y = h * jax.nn.gelu(gate_in.astype(f32))
    return y.astype(x_in.dtype)


def stick_breaking_mixer(q, k, v, q_g, k_g):
    f32 = jnp.float32
    b, s, _ = q.shape

    def qk_norm(t, g):
        t = split_heads(t.astype(f32), SB_HEADS)
        return t * lax.rsqrt(jnp.mean(t * t, axis=-1, keepdims=True) + EPS) * g.astype(f32)

    qh = qk_norm(q, q_g) * (SB_HEAD_DIM ** -0.5)
    kh = qk_norm(k, k_g)
    vh = split_heads(v.astype(f32), SB_HEADS)
    outs = []
    for blk in range(s // SB_QBLOCK):
        q0 = blk * SB_QBLOCK
        q1 = q0 + SB_QBLOCK
        z = jnp.einsum('bhqd,bhkd->bhqk', qh[:, :, q0:q1], kh[:, :, :q1])
        valid = jnp.arange(q1)[None, :] < jnp.arange(q0, q1)[:, None]
        log_keep = jnp.where(valid, jax.nn.log_sigmoid(-z), 0.0)
        after = lax.cumsum(log_keep, axis=3, reverse=True) - log_keep
        w = jnp.where(valid, jnp.exp(jax.nn.log_sigmoid(z) + after), 0.0)
        outs.append(jnp.einsum('bhqk,bhkd->bhqd', w, vh[:, :, :q1]))
    o = jnp.concatenate(outs, axis=2)
    return merge_heads(o).astype(v.dtype)


def hybrid_layer(x, norm_mix_g, w_in, gla_w_up, gla_b_alpha, gla_norm_g,
                 lru_conv_w, lru_conv_b, lru_w_a, lru_b_a, lru_w_x, lru_b_x, lru_lambda,
                 sb_q_norm_g, sb_k_norm_g, w_branch_a, w_branch_b, w_branch_c, b_gate,
                 w_out, norm_mlp_g, w_mlp_up, w_mlp_down):
    b, s, _ = x.shape
    xn = rms_norm(x, norm_mix_g)
    proj = xn @ w_in
    offsets = [int(o) for o in np.cumsum(IN_SIZES)[:-1]]
    gq, gk, gv, gg, gdown, lx, lgate, sq, sk, sv, gate_logits = jnp.split(proj, offsets, axis=-1)
    o_a = gla_mixer(gq, gk, gv, gg, gdown, gla_w_up, gla_b_alpha, gla_norm_g)
    o_b = rglru_mixer(lx, lgate, lru_conv_w, lru_conv_b, lru_w_a, lru_b_a, lru_w_x, lru_b_x, lru_lambda)
    o_c = stick_breaking_mixer(sq, sk, sv, sb_q_norm_g, sb_k_norm_g)
    gates = jax.nn.sigmoid((gate_logits + b_gate).astype(jnp.float32)).astype(x.dtype)
    gates = gates.reshape(b, s, N_BRANCHES, D_MODEL)
    merged = (gates[:, :, 0] * (o_a @ w_branch_a)
              + gates[:, :, 1] * (o_b @ w_branch_b)
              + gates[:, :, 2] * (o_c @ w_branch_c))
    x = x + merged @ w_out
    h = rms_norm(x, norm_mlp_g)
    x = x + jnp.square(jax.nn.relu(h @ w_mlp_up)) @ w_mlp_down
    return x


def setup_inputs(seed: int = 0) -> dict:
    key = jax.random.key(seed)
    ks = jax.random.split(key, 23)
    f32 = jnp.float32

    def dense(k, shape, fan_in, scale=1.0):
        return jax.random.normal(k, shape, f32) * (scale * fan_in ** -0.5)

    def gain(k, shape):
        return 1.0 + 0.02 * jax.random.normal(k, shape, f32)

    def bias(k, shape, scale=0.02):
        return scale * jax.random.normal(k, shape, f32)

    a0 = jax.random.uniform(ks[12], (DEPTH, LRU_WIDTH), f32, minval=0.9, maxval=0.999)
    return {
        'x': jax.random.normal(ks[0], (BATCH, SEQ, D_MODEL), f32),
        'norm_mix_g': gain(ks[1], (DEPTH, D_MODEL)),
        'w_in': dense(ks[2], (DEPTH, D_MODEL, IN_COLS), D_MODEL),
        'gla_w_up': dense(ks[3], (DEPTH, GLA_RANK, GLA_DK), GLA_RANK),
        'gla_b_alpha': bias(ks[4], (DEPTH, GLA_DK), 0.1),
        'gla_norm_g': gain(ks[5], (DEPTH, GLA_HEAD_DV)),
        'lru_conv_w': dense(ks[6], (DEPTH, CONV_WIDTH, LRU_WIDTH), CONV_WIDTH),
        'lru_conv_b': bias(ks[7], (DEPTH, LRU_WIDTH)),
        'lru_w_a': dense(ks[8], (DEPTH, LRU_BLOCKS, LRU_BLOCK, LRU_BLOCK), LRU_BLOCK),
        'lru_b_a': bias(ks[9], (DEPTH, LRU_WIDTH)),
        'lru_w_x': dense(ks[10], (DEPTH, LRU_BLOCKS, LRU_BLOCK, LRU_BLOCK), LRU_BLOCK),
        'lru_b_x': bias(ks[11], (DEPTH, LRU_WIDTH)),
        'lru_lambda': jnp.log(a0) - jnp.log1p(-a0),
        'sb_q_norm_g': gain(ks[13], (DEPTH, SB_HEAD_DIM)),
        'sb_k_norm_g': gain(ks[14], (DEPTH, SB_HEAD_DIM)),
        'w_branch_a': dense(ks[15], (DEPTH, GLA_DV, D_MODEL), GLA_DV),
        'w_branch_b': dense(ks[16], (DEPTH, LRU_WIDTH, D_MODEL), LRU_WIDTH),
        'w_branch_c': dense(ks[17], (DEPTH, SB_DIM, D_MODEL), SB_DIM),
        'b_gate': bias(ks[18], (DEPTH, N_BRANCHES * D_MODEL)),
        'w_out': dense(ks[19], (DEPTH, D_MODEL, D_MODEL), D_MODEL, 0.5),
        'norm_mlp_g': gain(ks[20], (DEPTH, D_MODEL)),
        'w_mlp_up': dense(ks[21], (DEPTH, D_MODEL, D_FF), D_MODEL),
        'w_mlp_down': dense(ks[22], (DEPTH, D_FF, D_MODEL), D_FF, 0.5),
    }


def reference(x, norm_mix_g, w_in, gla_w_up, gla_b_alpha, gla_norm_g,
              lru_conv_w, lru_conv_b, lru_w_a, lru_b_a, lru_w_x, lru_b_x, lru_lambda,
              sb_q_norm_g, sb_k_norm_g, w_branch_a, w_branch_b, w_branch_c, b_gate,
              w_out, norm_mlp_g, w_mlp_up, w_mlp_down):
    for l in range(DEPTH):
        x = hybrid_layer(x, norm_mix_g[l], w_in[l], gla_w_up[l], gla_b_alpha[l], gla_norm_g[l],
                         lru_conv_w[l], lru_conv_b[l], lru_w_a[l], lru_b_a[l], lru_w_x[l], lru_b_x[l],
                         lru_lambda[l], sb_q_norm_g[l], sb_k_norm_g[l], w_branch_a[l], w_branch_b[l],
                         w_branch_c[l], b_gate[l], w_out[l], norm_mlp_g[l], w_mlp_up[l], w_mlp_down[l])
    return x
```

```python
from contextlib import ExitStack
import numpy as np
import concourse.bass as bass
import concourse.mybir as mybir
from concourse.bass_utils import run_bass_kernel_spmd

F32 = mybir.dt.float32
BF16 = mybir.dt.bfloat16
AF = mybir.ActivationFunctionType
ALU = mybir.AluOpType

FUSED = False
DEPTH = 4
T = 2048
TT = 4
TW = 512
KC = 8
NW = 4
EPS = 1e-6
C_GQ, C_GK, C_GV, C_GG, C_GD, C_LX, C_LG, C_SQ, C_SK, C_SV, C_GATE = 0, 512, 1024, 2048, 3072, 3088, 4112, 5136, 6160, 7184, 8208
IN_COLS = 11280
P_GMIX, P_GMLP, P_GLAN, P_CW, P_CB, P_BA, P_BX, P_LAM, P_SQG, P_SKG, P_BG, NPP = 0, 8, 16, 18, 50, 58, 66, 74, 82, 83, 84, 108


class Buf:
    __slots__ = ("w", "r")

    def __init__(self):
        self.w = None
        self.r = {}


ENGS = ("pe", "act", "dve", "pool", "sp")
EPOCH = 20000
DMA_EPOCH = 1000


class Sched:
    def __init__(self, nc, n_dma_ch=8):
        self.nc = nc
        self.streams = {e: [] for e in ENGS}
        self.cnt = {e: 0 for e in ENGS}
        self.epoch = {e: 0 for e in ENGS}
        self.waited = {e: {} for e in ENGS}
        self.n_dma_ch = n_dma_ch
        self.dcnt = [0] * n_dma_ch
        self.depoch = [0] * n_dma_ch
        self.dnext = 0
        self.semkeys = []
        self._seen = set()
        self.latest = {}
        self.ninst = 0

    def _key(self, k):
        if k not in self._seen:
            self._seen.add(k)
            self.semkeys.append(k)
        return k

    def _filter(self, eng, need):
        out = []
        wd = self.waited[eng]
        for s, v in need.items():
            if eng == "pe" and s[0] == "pe":
                continue
            if wd.get(s, 0) < v:
                wd[s] = v
                out.append((s, v))
        return out

    def _deps(self, eng, reads, writes):
        need = {}
        for b in reads:
            if b.w is not None:
                s, v = b.w
                if need.get(s, 0) < v:
                    need[s] = v
        for b in writes:
            if b.w is not None:
                s, v = b.w
                if need.get(s, 0) < v:
                    need[s] = v
            for s, v in b.r.items():
                if need.get(s, 0) < v:
                    need[s] = v
        return self._filter(eng, need)

    def _mark(self, ev, reads, writes):
        s, v = ev
        self.latest[s] = v
        for b in reads:
            if b.r.get(s, 0) < v:
                b.r[s] = v
        for b in writes:
            b.w = ev
            b.r = {}

    def op(self, eng, fn, reads=(), writes=()):
        deps = self._deps(eng, reads, writes)
        if self.cnt[eng] >= EPOCH:
            self.epoch[eng] += 1
            self.cnt[eng] = 0
        self.cnt[eng] += 1
        key = self._key((eng, self.epoch[eng]))
        self.streams[eng].append((deps, fn, key, 1))
        self._mark((key, self.cnt[eng]), reads, writes)
        self.ninst += 1

    def dma(self, queue, fn, reads=(), writes=()):
        deps = self._deps(queue, reads, writes)
        ch = self.dnext
        self.dnext = (self.dnext + 1) % self.n_dma_ch
        if self.dcnt[ch] >= DMA_EPOCH:
            self.depoch[ch] += 1
            self.dcnt[ch] = 0
        self.dcnt[ch] += 1
        key = self._key(("dma%d" % ch, self.depoch[ch]))
        self.streams[queue].append((deps, fn, key, 16))
        self._mark((key, 16 * self.dcnt[ch]), reads, writes)
        self.ninst += 1

    def barrier(self):
        for eng in ENGS:
            deps = self._filter(eng, dict(self.latest))
            if deps:
                self.streams[eng].append((deps, None, None, 0))

    def emit(self, stack):
        nc = self.nc
        sems = {}
        for k in self.semkeys:
            sems[k] = stack.enter_context(nc.semaphore("s_%s_%d" % k))
        block = stack.enter_context(nc.Block())
        streams = self.streams

        def run(engobj, lst):
            for deps, fn, key, inc in lst:
                for s, v in deps:
                    engobj.wait_ge(sems[s], v)
                if fn is not None:
                    fn(engobj).then_inc(sems[key], inc)

        @block.tensor
        def _(e):
            run(e, streams["pe"])

        @block.scalar
        def _(e):
            run(e, streams["act"])

        @block.vector
        def _(e):
            run(e, streams["dve"])

        @block.gpsimd
        def _(e):
            run(e, streams["pool"])

        @block.sync
        def _(e):
            run(e, streams["sp"])


class Arena:
    def __init__(self, ap):
        self.ap = ap
        self.n = ap.shape[1]
        self.off = 0

    def f32(self, n):
        a = self.ap[:, self.off:self.off + n]
        self.off += n
        assert self.off <= self.n, ("arena overflow", self.off, self.n)
        return a

    def bf16(self, n):
        nn = (n + 1) // 2
        a = self.ap[:, self.off:self.off + nn].bitcast(BF16)
        self.off += nn
        assert self.off <= self.n, ("arena overflow", self.off, self.n)
        return a

    def reset(self):
        self.off = 0


class Ring:
    def __init__(self, aps):
        self.items = [(a, Buf()) for a in aps]
        self.i = 0

    def get(self):
        it = self.items[self.i]
        self.i = (self.i + 1) % len(self.items)
        return it


def tsl(tt):
    return slice(tt * TW, (tt + 1) * TW)


def build(NL, dbg=False):
    nc = bass.Bass("TRN2", target_bir_lowering=False)

    def din(name, shape):
        return nc.dram_tensor(name, shape, F32, kind="ExternalInput").ap()

    xT = din("xT", [1024, T])
    pp = din("pp", [NL, 128, NPP])
    w_in = din("w_in", [NL, 1024, IN_COLS])
    gla_w_up = din("gla_w_up", [NL, 16, 512])
    gla_b_alpha = din("gla_b_alpha", [NL, 512])
    lru_w_a = din("lru_w_a", [NL, 4, 256, 256])
    lru_w_x = din("lru_w_x", [NL, 4, 256, 256])
    w_br = [din("w_branch_a", [NL, 1024, 1024]), din("w_branch_b", [NL, 1024, 1024]), din("w_branch_c", [NL, 1024, 1024])]
    w_out = din("w_out", [NL, 1024, 1024])
    w_up = din("w_mlp_up", [NL, 1024, 4096])
    w_dn = din("w_mlp_down", [NL, 4096, 1024])
    yT = nc.dram_tensor("yT", [1024, T], F32, kind="ExternalOutput").ap()
    dbg_t = None
    if dbg:
        dbg_t = nc.dram_tensor("dbg", [6, 1024, T], F32, kind="ExternalOutput").ap()

    with ExitStack() as st:
        def sb(name, shape, dt):
            return st.enter_context(nc.sbuf_tensor(name, shape, dt))

        def psum(name, shape, dt):
            return st.enter_context(nc.psum_tensor(name, shape, dt))

        S = Sched(nc)
        xbuf_t = sb("xbuf", [128, KC * T], F32)
        xb = xbuf_t[:].rearrange("p (k t) -> p k t", k=KC)
        XB = [[Buf() for _ in range(TT)] for _ in range(KC)]
        xn_t = sb("xn", [128, KC * T], BF16)
        xn = xn_t[:].rearrange("p (k t) -> p k t", k=KC)
        XN = [[Buf() for _ in range(TT)] for _ in range(KC)]
        ob_t = sb("ob", [128, KC * T], BF16)
        ob = ob_t[:].rearrange("p (k t) -> p k t", k=KC)
        OB = [[Buf() for _ in range(TT)] for _ in range(KC)]
        wsl_t = sb("wsl", [128, NW * KC * 256], BF16)
        wsl = [wsl_t[:, i * KC * 256:(i + 1) * KC * 256].rearrange("p (k n) -> p k n", k=KC) for i in range(NW)]
        WB = [Buf() for _ in range(NW)]
        ppt = sb("ppt", [128, NL * NPP], F32)
        PPB = Buf()
        cst_bf = sb("cst_bf", [128, 128 * 5 + 256 + 2048 + 4 * 512 * 2], BF16)
        CB = Buf()
        cst_f = sb("cst_f", [128, 4], F32)
        ar_t = sb("arena", [128, 12200], F32)
        AR = Arena(ar_t[:])
        AR2 = Arena(xbuf_t[:])
        YT = [Buf() for _ in range(KC)]
        DBG = Buf()

        pbanks = [psum("pb%d" % i, [128, 512], F32) for i in range(7)]
        prot = Ring([pbanks[i][:] for i in range(4)])
        P4, P5, P6 = pbanks[4][:], pbanks[5][:], pbanks[6][:]
        PB4, PB5, PB6 = Buf(), Buf(), Buf()
        ptr_t = psum("ptr", [128, 1024], BF16)
        ptr = ptr_t[:]
        PTR = Buf()

        def getps():
            return prot.get()

        def mm(out, lhsT, rhs, start, stop, r, w):
            S.op("pe", lambda e: e.matmul(out, lhsT, rhs, start=start, stop=stop), r, w)

        def act(out, in_, func, r, w, bias=None, scale=None):
            kw = {}
            if bias is not None:
                kw["bias"] = bias
            if scale is not None:
                kw["scale"] = scale
            S.op("act", lambda e: e.activation(out=out, in_=in_, func=func, **kw), r, w)

        def tto(eng, out, in0, in1, op, r, w):
            S.op(eng, lambda e: e.tensor_tensor(out=out, in0=in0, in1=in1, op=op), r, w)

        def stt(eng, out, in0, scalar, in1, op0, op1, r, w):
            S.op(eng, lambda e: e.scalar_tensor_tensor(out=out, in0=in0, scalar=scalar, in1=in1, op0=op0, op1=op1), r, w)

        def tsc(eng, out, in0, s1, s2, op0, op1, r, w):
            S.op(eng, lambda e: e.tensor_scalar(out=out, in0=in0, scalar1=s1, scalar2=s2, op0=op0, op1=op1), r, w)

        def tsm(eng, out, in0, s1, r, w):
            S.op(eng, lambda e: e.tensor_scalar_mul(out=out, in0=in0, scalar1=s1), r, w)

        def cp(eng, out, in_, r, w):
            if eng == "act":
                S.op("act", lambda e: e.activation(out=out, in_=in_, func=AF.Copy), r, w)
            else:
                S.op(eng, lambda e: e.tensor_copy(out=out, in_=in_), r, w)

        def memset(eng, ap, val, w):
            S.op(eng, lambda e: e.memset(ap, val), (), w)

        def recip(out, in_, r, w):
            S.op("dve", lambda e: e.reciprocal(out=out, in_=in_), r, w)

        def dma(q, out, in_, r, w):
            S.dma(q, lambda e: e.dma_start(out=out, in_=in_), r, w)

        def asel(ap, pattern, cmp, base, cm, w):
            S.op("pool", lambda e: e.affine_select(out=ap, in_=ap, pattern=pattern, compare_op=cmp, fill=0.0, base=base, channel_multiplier=cm), w, w)

        o = 0
        ident_bf = cst_bf[:, o:o + 128]; o += 128
        ones_bf = cst_bf[:, o:o + 128]; o += 128
        maskG = cst_bf[:, o:o + 128]; o += 128
        TriS = cst_bf[:, o:o + 128]; o += 128
        NU = cst_bf[:, o:o + 128]; o += 128
        Eb = cst_bf[:, o:o + 256].rearrange("p (b m) -> p b m", b=16); o += 256
        Xb = cst_bf[:, o:o + 2048].rearrange("p (a s) -> p a s", a=16); o += 2048
        DM = []
        NM = []
        for r_ in range(4):
            DM.append(cst_bf[:, o:o + 512]); o += 512
        for r_ in range(4):
            NM.append(cst_bf[:, o:o + 512]); o += 512
        eps_t = cst_f[:, 0:1]
        one_t = cst_f[:, 1:2]
        memset("dve", cst_f[:, 0:1], EPS, [CB])
        memset("dve", cst_f[:, 1:2], 1.0, [CB])
        tmpc = AR.f32(2048)
        TB = Buf()

        def mk(dst, np_, nf, val, pattern, cmp, base, cm, post=None):
            t = tmpc[0:np_, 0:nf]
            memset("pool", t, val, [TB])
            if pattern is not None:
                tv = t if len(pattern) == 1 else t.rearrange("p (a b) -> p a b", a=pattern[0][1])
                asel(tv, pattern, cmp, base, cm, [TB])
            if post is not None:
                post(t)
            cp("dve", dst, t, [TB], [CB])

        mk(ident_bf, 128, 128, 1.0, [[-1, 128]], ALU.is_equal, 0, 1)
        mk(ones_bf, 128, 128, 1.0, None, None, 0, 0)
        mk(maskG, 128, 128, 1.0, [[1, 128]], ALU.is_ge, 0, -1, post=lambda t: memset("pool", t[0:64, 64:128], 0.0, [TB]))
        tsm("dve", TriS, maskG, -1.0 / 16.0, [CB], [CB])
        mk(NU, 128, 128, -1.0, [[-1, 128]], ALU.is_ge, 0, 1)
        mk(cst_bf[:, 640:896], 128, 256, 1.0, [[1, 16], [-1, 16]], ALU.is_equal, 0, 0)
        mk(cst_bf[0:16, 896:896 + 2048], 16, 2048, -1.0, [[-1, 16], [0, 128]], ALU.is_gt, 0, 1)
        for r_ in range(4):
            mk(DM[r_], 128, 512, 1.0, [[1, 512]], ALU.is_gt, -128 * r_, -1)
            tsc("dve", NM[r_], DM[r_], 30000.0, -30000.0, ALU.mult, ALU.add, [CB], [CB])
        for l in range(NL):
            dma("sp", ppt[:, l * NPP:(l + 1) * NPP], pp[l], [], [PPB])

        def par(l, col):
            return ppt[:, l * NPP + col:l * NPP + col + 1]

        class WQ:
            def __init__(self):
                self.plan = []
                self.issued = 0
                self.consumed = 0

            def extend(self, items):
                self.plan.extend(items)

            def get(self, D=2):
                while self.issued < min(len(self.plan), self.consumed + 1 + D):
                    src, n = self.plan[self.issued]
                    slot = self.issued % NW
                    dma("pool", wsl[slot][:, :, 0:n], src.rearrange("(k p) n -> p k n", p=128), [], [WB[slot]])
                    self.issued += 1
                slot = self.consumed % NW
                self.consumed += 1
                return slot

        wq = WQ()

        def proj(slot, c0, n, tt, src=None, RB=None):
            if src is None:
                src, RB = xn, XN
            ps, pb = getps()
            for k in range(KC):
                mm(ps[0:n, :], wsl[slot][:, k, c0:c0 + n], src[:, k, tsl(tt)], k == 0, k == KC - 1, [WB[slot], RB[k][tt]], [pb])
            return ps, pb

        def dump(idx, src, RBs, bf):
            if not dbg:
                return
            for k in range(KC):
                dma("pool" if bf else "sp", dbg_t[idx, k * 128:(k + 1) * 128, :], src[:, k, :], RBs[k], [DBG])

        def rmsnorm(l, gcol):
            AR.reset()
            sqr = Ring([AR.bf16(TW) for _ in range(3)])
            srr = Ring([AR.f32(TW) for _ in range(2)])
            for tt in range(TT):
                ps, pb = getps()
                for k in range(KC):
                    sq, sqb = sqr.get()
                    act(sq, xb[:, k, tsl(tt)], AF.Square, [XB[k][tt]], [sqb])
                    mm(ps, ones_bf, sq, k == 0, k == KC - 1, [sqb, CB], [pb])
                sr, srb = srr.get()
                act(sr, ps, AF.Sqrt, [pb, CB], [srb], bias=eps_t, scale=1.0 / 1024.0)
                recip(sr, sr, [srb], [srb])
                for k in range(KC):
                    stt("dve", xn[:, k, tsl(tt)], xb[:, k, tsl(tt)], par(l, gcol + k), sr, ALU.mult, ALU.mult,
                        [XB[k][tt], srb, PPB], [XN[k][tt]])

        def spill_x():
            for k in range(KC):
                dma("sp", yT[k * 128:(k + 1) * 128, :], xb[:, k, :], XB[k], [YT[k]])

        def reload_x():
            for k in range(KC):
                dma("sp", xb[:, k, :], yT[k * 128:(k + 1) * 128, :], [YT[k]], XB[k])

        def gla(l):
            AR.reset()
            AR2.reset()
            A = AR2
            adT = A.bf16(T); ADT = [Buf() for _ in range(TT)]
            wupa = A.bf16(512); WUPA = Buf()
            e1r = Ring([A.f32(TW) for _ in range(2)])
            Pr = Ring([A.bf16(TW) for _ in range(2)])
            ecr = Ring([A.f32(TW) for _ in range(2)])
            eir = Ring([A.f32(TW) for _ in range(2)])
            qd = A.bf16(T); QD = [Buf() for _ in range(TT)]
            ki = A.bf16(T); KI = [Buf() for _ in range(TT)]
            ki_tm = A.bf16(T); KITM = [Buf() for _ in range(TT)]
            v_tm = A.bf16(16 * 256); VTM = [Buf() for _ in range(8)]
            sg = [A.bf16(T), A.bf16(T)]; SG = [[Buf() for _ in range(TT)] for _ in range(2)]
            Sst = A.f32(256); SST = Buf()
            tmpS = Ring([A.f32(256) for _ in range(2)])
            S_bf = [A.bf16(256), A.bf16(256)]; SBF = [Buf(), Buf()]
            scr = Ring([A.bf16(128) for _ in range(3)])
            dec = A.f32(32); DEC = Buf()
            sqr = Ring([A.bf16(TW) for _ in range(2)])
            srr = Ring([A.f32(TW) for _ in range(2)])
            t1r = Ring([A.f32(TW) for _ in range(2)])
            for tt in range(TT):
                memset("dve", adT[0:33, tsl(tt)], 1.0, [ADT[tt]])
            memset("dve", wupa[0:33, :], 0.0, [WUPA])
            dma("pool", wupa[0:16, :], gla_w_up[l], [], [WUPA])
            dma("pool", wupa[32:33, :], gla_b_alpha[l:l + 1, :], [], [WUPA])
            plan = [(w_in[l][:, C_GD:C_GD + 16], 16)]
            for h in range(4):
                plan += [(w_in[l][:, C_GQ + h * 128:C_GQ + (h + 1) * 128], 128),
                         (w_in[l][:, C_GK + h * 128:C_GK + (h + 1) * 128], 128),
                         (w_in[l][:, C_GV + h * 256:C_GV + (h + 1) * 256], 256),
                         (w_in[l][:, C_GG + h * 256:C_GG + (h + 1) * 256], 256)]
            wq.extend(plan)
            s_ = wq.get(D=1)
            for tt in range(TT):
                ps, pb = proj(s_, 0, 16, tt)
                cp("act", adT[0:16, tsl(tt)], ps[0:16, :], [pb], [ADT[tt]])
            for h in range(4):
                s_q = wq.get(D=2)
                s_k = wq.get(D=2)
                for tt in range(TT):
                    psy, pyb = getps()
                    for mi in range(4):
                        m = tt * 4 + mi
                        mm(psy[:, mi * 128:(mi + 1) * 128], adT[0:33, m * 128:(m + 1) * 128], wupa[0:33, h * 128:(h + 1) * 128],
                           True, True, [ADT[tt], WUPA], [pyb])
                    e1, e1b = e1r.get()
                    act(e1, psy, AF.Exp, [pyb], [e1b], scale=-1.0)
                    Pt, Ptb = Pr.get()
                    act(Pt, e1, AF.Ln, [e1b, CB], [Ptb], bias=one_t)
                    psc, pcb = getps()
                    for mi in range(4):
                        mm(psc[:, mi * 128:(mi + 1) * 128], Pt[:, mi * 128:(mi + 1) * 128], TriS, True, True, [Ptb, CB], [pcb])
                    ec, ecb = ecr.get()
                    ei, eib = eir.get()
                    act(ec, psc, AF.Exp, [pcb], [ecb])
                    act(ei, psc, AF.Exp, [pcb], [eib], scale=-1.0)
                    cp("dve", dec[:, tt * 8:(tt + 1) * 8], ec[:, 63::64], [ecb], [DEC])
                    psq, pqb = proj(s_q, 0, 128, tt)
                    stt("dve", qd[:, tsl(tt)], psq, 128.0 ** -0.5, ec, ALU.mult, ALU.mult, [pqb, ecb], [QD[tt]])
                    psk, pkb = proj(s_k, 0, 128, tt)
                    tto("dve", ki[:, tsl(tt)], psk, ei, ALU.mult, [pkb, eib], [KI[tt]])
                    for mi in range(4):
                        m = tt * 4 + mi
                        S.op("pe", (lambda o_, i_: (lambda e: e.transpose(o_, i_, ident_bf)))(ptr[:, mi * 128:(mi + 1) * 128], ki[:, m * 128:(m + 1) * 128]),
                             [KI[tt], CB], [PTR])
                    cp("act", ki_tm[:, tsl(tt)], ptr[:, 0:512], [PTR], [KITM[tt]])
                s_v = wq.get(D=2)
                for m2 in range(8):
                    ps, pb = getps()
                    for mj in range(2):
                        m = m2 * 2 + mj
                        for k in range(KC):
                            mm(ps[:, mj * 256:(mj + 1) * 256], xn[:, k, m * 128:(m + 1) * 128], wsl[s_v][:, k, 0:256],
                               k == 0, k == KC - 1, [XN[k][m // 4], WB[s_v]], [pb])
                    cp("act", v_tm[:, m2 * 512:(m2 + 1) * 512], ps, [pb], [VTM[m2]])
                s_g = wq.get(D=2)
                for jb in range(2):
                    for tt in range(TT):
                        ps, pb = proj(s_g, jb * 128, 128, tt)
                        act(sg[jb][:, tsl(tt)], ps, AF.Silu, [pb], [SG[jb][tt]])
                memset("dve", Sst, 0.0, [SST])
                memset("dve", S_bf[0], 0.0, [SBF[0]])
                for tt in range(TT):
                    po = [P4, P5]
                    POB = [PB4, PB5]
                    for mi in range(4):
                        m = tt * 4 + mi
                        pss, psb = getps()
                        mm(pss[:, 0:128], ki[:, m * 128:(m + 1) * 128], qd[:, m * 128:(m + 1) * 128], True, True, [KI[tt], QD[tt]], [psb])
                        sc, scb = scr.get()
                        tto("dve", sc, pss[:, 0:128], maskG, ALU.mult, [psb, CB], [scb])
                        for half in range(2):
                            c = 2 * m + half
                            cur = c % 2
                            for jb in range(2):
                                col = mi * 128 + half * 64
                                mm(po[jb][:, col:col + 64], S_bf[cur][:, jb * 128:(jb + 1) * 128], qd[:, c * 64:(c + 1) * 64],
                                   True, False, [SBF[cur], QD[tt]], [POB[jb]])
                                mm(po[jb][:, col:col + 64], v_tm[:, m * 256 + jb * 128:m * 256 + (jb + 1) * 128], sc[:, half * 64:(half + 1) * 64],
                                   False, True, [VTM[m // 2], scb], [POB[jb]])
                            pkv, pkb = getps()
                            mm(pkv[:, 0:256], ki_tm[half * 64:(half + 1) * 64, m * 128:(m + 1) * 128],
                               v_tm[half * 64:(half + 1) * 64, m * 256:(m + 1) * 256], True, True, [KITM[tt], VTM[m // 2]], [pkb])
                            ts_, tsb = tmpS.get()
                            tto("dve", ts_, pkv[:, 0:256], Sst, ALU.add, [pkb, SST], [tsb])
                            tsm("dve", Sst, ts_, dec[:, c:c + 1], [tsb, DEC], [SST])
                            act(S_bf[1 - cur], ts_, AF.Identity, [tsb, DEC], [SBF[1 - cur]], scale=dec[:, c:c + 1])
                    pn, pnb = getps()
                    for jb in range(2):
                        sq, sqb = sqr.get()
                        act(sq, po[jb], AF.Square, [POB[jb]], [sqb])
                        mm(pn, ones_bf, sq, jb == 0, jb == 1, [sqb, CB], [pnb])
                    sr, srb = srr.get()
                    act(sr, pn, AF.Sqrt, [pnb, CB], [srb], bias=eps_t, scale=1.0 / 256.0)
                    recip(sr, sr, [srb], [srb])
                    for jb in range(2):
                        t1, t1b = t1r.get()
                        tto("dve", t1, po[jb], sr, ALU.mult, [POB[jb], srb], [t1b])
                        stt("dve", ob[:, h * 2 + jb, tsl(tt)], t1, par(l, P_GLAN + jb), sg[jb][:, tsl(tt)], ALU.mult, ALU.mult,
                            [t1b, PPB, SG[jb][tt]], [OB[h * 2 + jb][tt]])

        def branch(l, b):
            AR.reset()
            gr = Ring([AR.f32(TW) for _ in range(2)])
            tr = Ring([AR.f32(TW) for _ in range(2)])
            plan = []
            for nbp in range(4):
                plan += [(w_br[b][l][:, nbp * 256:(nbp + 1) * 256], 256),
                         (w_in[l][:, C_GATE + b * 1024 + nbp * 256:C_GATE + b * 1024 + (nbp + 1) * 256], 256)]
            wq.extend(plan)
            for nbp in range(4):
                s1 = wq.get(D=2)
                s2 = wq.get(D=2)
                for nbi in range(2):
                    nb = nbp * 2 + nbi
                    for tt in range(TT):
                        ps1, p1b = proj(s1, nbi * 128, 128, tt, ob, OB)
                        ps2, p2b = proj(s2, nbi * 128, 128, tt)
                        g, gb = gr.get()
                        act(g, ps2, AF.Sigmoid, [p2b, PPB], [gb], bias=par(l, P_BG + b * 8 + nb))
                        if b == 0:
                            tto("dve", xb[:, nb, tsl(tt)], ps1, g, ALU.mult, [p1b, gb], [XB[nb][tt]])
                        else:
                            t_, tb = tr.get()
                            tto("dve", t_, ps1, g, ALU.mult, [p1b, gb], [tb])
                            tto("dve", xb[:, nb, tsl(tt)], xb[:, nb, tsl(tt)], t_, ALU.add, [tb, XB[nb][tt]], [XB[nb][tt]])

        def lru(l):
            AR.reset()
            A = AR
            cl = A.f32(8); CL = Buf()
            tmp8 = A.f32(8)
            lxbf = [A.bf16(T + 4), A.bf16(T + 4)]; LXB = [[Buf() for _ in range(TT)] for _ in range(2)]
            dg = [[A.bf16(128) for _ in range(4)] for _ in range(2)]; DG = Buf()
            wab = A.bf16(512).rearrange("p (i j) -> p i j", i=2); wxb = A.bf16(512).rearrange("p (i j) -> p i j", i=2); WAB = Buf(); WXB = Buf()
            xcf = [Ring([A.f32(TW) for _ in range(2)]) for _ in range(2)]
            xcb = [Ring([A.bf16(TW) for _ in range(1)]) for _ in range(2)]
            rr = Ring([A.f32(TW) for _ in range(2)])
            igr = Ring([A.f32(TW) for _ in range(2)])
            n2r = Ring([A.f32(TW) for _ in range(1)])
            hr = [Ring([A.f32(TW) for _ in range(2)]) for _ in range(2)]
            glr = Ring([A.f32(TW) for _ in range(2)])
            g2r = Ring([A.f32(TW) for _ in range(1)])
            act(tmp8, ppt[:, l * NPP + P_LAM:l * NPP + P_LAM + 8], AF.Exp, [PPB], [CL], scale=-1.0)
            act(tmp8, tmp8, AF.Ln, [CL, CB], [CL], bias=one_t)
            tsm("dve", cl, tmp8, -8.0, [CL], [CL])
            plan = []
            for hb in range(4):
                plan += [(w_in[l][:, C_LX + hb * 256:C_LX + (hb + 1) * 256], 256), (w_in[l][:, C_LG + hb * 256:C_LG + (hb + 1) * 256], 256)]
            wq.extend(plan)
            for hb in range(4):
                slx = wq.get(D=2)
                slg = wq.get(D=2)
                dma("pool", wab, lru_w_a[l, hb].rearrange("(i p) j -> p i j", p=128), [], [WAB])
                dma("pool", wxb, lru_w_x[l, hb].rearrange("(i p) j -> p i j", p=128), [], [WXB])
                for cbl in range(2):
                    memset("dve", lxbf[cbl][:, 0:3], 0.0, [LXB[cbl][0]])
                    for w_ in range(4):
                        tsm("dve", dg[cbl][w_], ident_bf, par(l, P_CW + (hb * 2 + cbl) * 4 + w_), [CB, PPB], [DG])
                hprev = [None, None]
                for tt in range(TT):
                    for cbl in range(2):
                        ps, pb = proj(slx, cbl * 128, 128, tt)
                        cp("act", lxbf[cbl][:, 3 + tt * TW:3 + (tt + 1) * TW], ps, [pb], [LXB[cbl][tt]])
                    xf = []
                    xh = []
                    for cbl in range(2):
                        ps, pb = getps()
                        rd = [LXB[cbl][tt], DG] + ([LXB[cbl][tt - 1]] if tt > 0 else [])
                        for w_ in range(4):
                            mm(ps, dg[cbl][w_], lxbf[cbl][:, tt * TW + w_:tt * TW + w_ + TW], w_ == 0, w_ == 3, rd, [pb])
                        f_, fb = xcf[cbl].get()
                        act(f_, ps, AF.Identity, [pb, PPB], [fb], bias=par(l, P_CB + hb * 2 + cbl))
                        h_, hb_ = xcb[cbl].get()
                        cp("dve", h_, f_, [fb], [hb_])
                        xf.append((f_, fb))
                        xh.append((h_, hb_))
                    for jb in range(2):
                        c = hb * 2 + jb
                        psr, prb = getps()
                        for ib in range(2):
                            mm(psr, wab[:, ib, jb * 128:(jb + 1) * 128], xh[ib][0], ib == 0, ib == 1, [WAB, xh[ib][1]], [prb])
                        psi, pib = getps()
                        for ib in range(2):
                            mm(psi, wxb[:, ib, jb * 128:(jb + 1) * 128], xh[ib][0], ib == 0, ib == 1, [WXB, xh[ib][1]], [pib])
                        r_, rb = rr.get()
                        act(r_, psr, AF.Sigmoid, [prb, PPB], [rb], bias=par(l, P_BA + c))
                        a_, ab = r_, rb
                        act(a_, r_, AF.Exp, [rb, CL], [ab], scale=cl[:, c:c + 1])
                        ig, igb = igr.get()
                        act(ig, psi, AF.Sigmoid, [pib, PPB], [igb], bias=par(l, P_BX + c))
                        n2, n2b = n2r.get()
                        stt("dve", n2, a_, -1.0, a_, ALU.mult, ALU.mult, [ab], [n2b])
                        act(n2, n2, AF.Sqrt, [n2b, CB], [n2b], bias=one_t)
                        u_, ub = ig, igb
                        tto("dve", u_, n2, ig, ALU.mult, [n2b, igb], [ub])
                        tto("dve", u_, u_, xf[jb][0], ALU.mult, [ub, xf[jb][1]], [ub])
                        hh, hhb = hr[jb].get()
                        if hprev[jb] is None:
                            S.op("dve", (lambda o_, a0, u0: (lambda e: e.tensor_tensor_scan(out=o_, data0=a0, data1=u0, initial=0.0, op0=ALU.mult, op1=ALU.add)))(hh, a_, u_),
                                 [ab, ub], [hhb])
                        else:
                            hp, hpb = hprev[jb]
                            S.op("dve", (lambda o_, a0, u0, i0: (lambda e: e.tensor_tensor_scan(out=o_, data0=a0, data1=u0, initial=i0, op0=ALU.mult, op1=ALU.add)))(hh, a_, u_, hp[:, TW - 1:TW]),
                                 [ab, ub, hpb], [hhb])
                        hprev[jb] = (hh, hhb)
                        psg, pgb = proj(slg, jb * 128, 128, tt)
                        gl, glb = glr.get()
                        g2, g2b = g2r.get()
                        act(gl, psg, AF.Identity, [pgb], [glb])
                        tto("dve", g2, gl, gl, ALU.mult, [glb], [g2b])
                        tsc("dve", g2, g2, 0.044715, 1.0, ALU.mult, ALU.add, [g2b], [g2b])
                        tto("dve", g2, g2, gl, ALU.mult, [g2b, glb], [g2b])
                        act(g2, g2, AF.Sigmoid, [g2b], [g2b], scale=1.5957691216057308)
                        tto("dve", gl, gl, hh, ALU.mult, [glb, hhb], [glb])
                        tto("dve", ob[:, c, tsl(tt)], gl, g2, ALU.mult, [glb, g2b], [OB[c][tt]])

        def sbattn(l):
            AR.reset()
            A = AR
            Lk = A.bf16(16 * TW); LK = [Buf() for _ in range(16)]
            qn = A.bf16(T); QN = [Buf() for _ in range(TT)]
            kn = A.bf16(T); KN = [Buf() for _ in range(TT)]
            v_tm = A.bf16(T); VT = [Buf() for _ in range(4)]
            Er = Ring([A.f32(TW) for _ in range(2)])
            wr = Ring([A.bf16(TW) for _ in range(3)])
            sqr = Ring([A.bf16(TW) for _ in range(2)])
            srr = Ring([A.f32(TW) for _ in range(2)])
            csr = Ring([A.bf16(TW) for _ in range(2)])
            gq2 = A.f32(2); GQ = Buf()
            tsm("dve", gq2[:, 0:1], par(l, P_SQG), 128.0 ** -0.5, [PPB], [GQ])
            cp("dve", gq2[:, 1:2], par(l, P_SKG), [PPB], [GQ])
            plan = []
            for h in range(8):
                plan += [(w_in[l][:, C_SQ + h * 128:C_SQ + (h + 1) * 128], 128), (w_in[l][:, C_SK + h * 128:C_SK + (h + 1) * 128], 128),
                         (w_in[l][:, C_SV + h * 128:C_SV + (h + 1) * 128], 128)]
            wq.extend(plan)
            for h in range(8):
                for which, (dst, DB) in enumerate(((qn, QN), (kn, KN))):
                    s_ = wq.get(D=2)
                    for tt in range(TT):
                        ps, pb = proj(s_, 0, 128, tt)
                        sq, sqb = sqr.get()
                        act(sq, ps, AF.Square, [pb], [sqb])
                        pn, pnb = getps()
                        mm(pn, ones_bf, sq, True, True, [sqb, CB], [pnb])
                        sr, srb = srr.get()
                        act(sr, pn, AF.Sqrt, [pnb, CB], [srb], bias=eps_t, scale=1.0 / 128.0)
                        recip(sr, sr, [srb], [srb])
                        stt("dve", dst[:, tsl(tt)], ps, gq2[:, which:which + 1], sr, ALU.mult, ALU.mult, [pb, srb, GQ], [DB[tt]])
                s_v = wq.get(D=2)
                for m4 in range(4):
                    ps, pb = getps()
                    for mi in range(4):
                        m = m4 * 4 + mi
                        for k in range(KC):
                            mm(ps[:, mi * 128:(mi + 1) * 128], xn[:, k, m * 128:(m + 1) * 128], wsl[s_v][:, k, 0:128],
                               k == 0, k == KC - 1, [XN[k][m4], WB[s_v]], [pb])
                    cp("act", v_tm[:, m4 * 512:(m4 + 1) * 512], ps, [pb], [VT[m4]])
                for qt in range(TT):
                    nk = 4 * (qt + 1)
                    for a in range(nk):
                        psz, pzb = getps()
                        mm(psz, kn[:, a * 128:(a + 1) * 128], qn[:, tsl(qt)], True, True, [KN[a // 4], QN[qt]], [pzb])
                        E, Eb_ = Er.get()
                        act(E, psz, AF.Exp, [pzb], [Eb_])
                        La = Lk[:, a * TW:(a + 1) * TW]
                        act(La, E, AF.Ln, [Eb_, CB], [LK[a]], bias=one_t)
                        if a >= 4 * qt:
                            tto("dve", La, La, DM[a - 4 * qt], ALU.mult, [LK[a], CB], [LK[a]])
                        mm(P4[0:16, :], Eb[:, a, :], La, a == 0, a == nk - 1, [LK[a], CB], [PB4])
                    cs, csb = csr.get()
                    cp("dve", cs[0:16, :], P4[0:16, :], [PB4], [csb])
                    po, pob = (P5, PB5) if qt % 2 == 0 else (P6, PB6)
                    for a in range(nk):
                        diag = a >= 4 * qt
                        psw, pwb = getps()
                        La = Lk[:, a * TW:(a + 1) * TW]
                        mm(psw, kn[:, a * 128:(a + 1) * 128], qn[:, tsl(qt)], True, False, [KN[a // 4], QN[qt]], [pwb])
                        mm(psw, NU, La, False, False, [CB, LK[a]], [pwb])
                        mm(psw, Xb[0:16, a, :], cs[0:16, :], False, not diag, [CB, csb], [pwb])
                        if diag:
                            mm(psw, ident_bf, NM[a - 4 * qt], False, True, [CB], [pwb])
                        w_, wb_ = wr.get()
                        act(w_, psw, AF.Exp, [pwb], [wb_])
                        mm(po, v_tm[:, a * 128:(a + 1) * 128], w_, a == 0, a == nk - 1, [VT[a // 4], wb_], [pob])
                    cp("dve", ob[:, h, tsl(qt)], po, [pob], [OB[h][qt]])

        def wout(l):
            AR.reset()
            for nb in range(KC):
                for tt in range(TT):
                    cp("act" if (nb + tt) % 2 == 0 else "dve", ob[:, nb, tsl(tt)], xb[:, nb, tsl(tt)], [XB[nb][tt]], [OB[nb][tt]])
            dump(4, ob, OB, True)
            reload_x()
            wq.extend([(w_out[l][:, i * 256:(i + 1) * 256], 256) for i in range(4)])
            for i in range(4):
                s_ = wq.get(D=2)
                for nbi in range(2):
                    nb = i * 2 + nbi
                    for tt in range(TT):
                        ps, pb = proj(s_, nbi * 128, 128, tt, ob, OB)
                        tto("dve", xb[:, nb, tsl(tt)], ps, xb[:, nb, tsl(tt)], ALU.add, [pb, XB[nb][tt]], [XB[nb][tt]])

        def mlp(l):
            rmsnorm(l, P_GMLP)
            rr = Ring([AR.f32(TW) for _ in range(3)])
            plan = []
            for fg in range(4):
                plan += [(w_up[l][:, fg * 1024 + i * 256:fg * 1024 + (i + 1) * 256], 256) for i in range(4)]
                plan += [(w_dn[l][fg * 1024:(fg + 1) * 1024, i * 256:(i + 1) * 256], 256) for i in range(4)]
            wq.extend(plan)
            for fg in range(4):
                for i in range(4):
                    s_ = wq.get(D=3)
                    for fbi in range(2):
                        fb = i * 2 + fbi
                        for tt in range(TT):
                            ps, pb = proj(s_, fbi * 128, 128, tt)
                            r_, rb = rr.get()
                            act(r_, ps, AF.Relu, [pb], [rb])
                            tto("dve", ob[:, fb, tsl(tt)], r_, r_, ALU.mult, [rb], [OB[fb][tt]])
                for i in range(4):
                    s_ = wq.get(D=3)
                    for nbi in range(2):
                        nb = i * 2 + nbi
                        for tt in range(TT):
                            ps, pb = proj(s_, nbi * 128, 128, tt, ob, OB)
                            tto("dve", xb[:, nb, tsl(tt)], ps, xb[:, nb, tsl(tt)], ALU.add, [pb, XB[nb][tt]], [XB[nb][tt]])

        S.barrier()
        for k in range(KC):
            dma("sp", xb[:, k, :], xT[k * 128:(k + 1) * 128, :], [], XB[k])
        for l in range(NL):
            rmsnorm(l, P_GMIX)
            if l == 0:
                dump(0, xn, XN, True)
            spill_x()
            S.barrier()
            gla(l)
            if l == 0:
                dump(1, ob, OB, True)
            S.barrier()
            branch(l, 0)
            S.barrier()
            lru(l)
            if l == 0:
                dump(2, ob, OB, True)
            S.barrier()
            branch(l, 1)
            S.barrier()
            sbattn(l)
            if l == 0:
                dump(3, ob, OB, True)
            S.barrier()
            branch(l, 2)
            S.barrier()
            wout(l)
            if l == 0:
                dump(5, xb, XB, False)
            S.barrier()
            mlp(l)
            S.barrier()
        for k in range(KC):
            dma("sp", yT[k * 128:(k + 1) * 128, :], xb[:, k, :], XB[k], [YT[k]])
        S.barrier()
        S.emit(st)
        print("instructions:", S.ninst, {e: len(S.streams[e]) for e in ENGS})
    return nc


def pack_params(inp, layers):
    def fm(v, nb):
        return np.ascontiguousarray(np.asarray(v, np.float32).reshape(nb, 128).T)

    out = np.zeros((len(layers), 128, NPP), np.float32)
    for i, l in enumerate(layers):
        out[i, :, P_GMIX:P_GMIX + 8] = fm(inp["norm_mix_g"][l], 8)
        out[i, :, P_GMLP:P_GMLP + 8] = fm(inp["norm_mlp_g"][l], 8)
        out[i, :, P_GLAN:P_GLAN + 2] = fm(inp["gla_norm_g"][l], 2)
        cw = np.asarray(inp["lru_conv_w"][l], np.float32)
        for cb in range(8):
            for w_ in range(4):
                out[i, :, P_CW + cb * 4 + w_] = cw[w_, cb * 128:(cb + 1) * 128]
        out[i, :, P_CB:P_CB + 8] = fm(inp["lru_conv_b"][l], 8)
        out[i, :, P_BA:P_BA + 8] = fm(inp["lru_b_a"][l], 8)
        out[i, :, P_BX:P_BX + 8] = fm(inp["lru_b_x"][l], 8)
        out[i, :, P_LAM:P_LAM + 8] = fm(inp["lru_lambda"][l], 8)
        out[i, :, P_SQG] = np.asarray(inp["sb_q_norm_g"][l], np.float32)
        out[i, :, P_SKG] = np.asarray(inp["sb_k_norm_g"][l], np.float32)
        out[i, :, P_BG:P_BG + 24] = fm(inp["b_gate"][l], 24)
    return out


_NC_CACHE = {}
WNAMES = ["w_in", "gla_w_up", "gla_b_alpha", "lru_w_a", "lru_w_x", "w_branch_a", "w_branch_b", "w_branch_c", "w_out", "w_mlp_up", "w_mlp_down"]


def _get_nc(NL, dbg=False):
    key = (NL, dbg)
    if key not in _NC_CACHE:
        _NC_CACHE[key] = build(NL, dbg)
    return _NC_CACHE[key]


def run_layers(inp, xT_list, layers, dbg=False, cores=8):
    nc = _get_nc(len(layers), dbg)
    ppk = pack_params(inp, layers)
    shared = {"pp": ppk}
    for n in WNAMES:
        shared[n] = np.ascontiguousarray(np.asarray(inp[n], np.float32)[layers[0]:layers[-1] + 1])
    in_maps = []
    for c in range(cores):
        d = dict(shared)
        d["xT"] = xT_list[c]
        in_maps.append(d)
    res = run_bass_kernel_spmd(nc, in_maps, core_ids=list(range(cores)))
    return res.results


def kernel(**inputs):
    x = np.asarray(inputs["x"], np.float32)
    B = x.shape[0]
    xT = [np.ascontiguousarray(x[b].T) for b in range(B)]
    if FUSED:
        res = run_layers(inputs, xT, list(range(DEPTH)))
        xT = [r["yT"] for r in res]
    else:
        for l in range(DEPTH):
            res = run_layers(inputs, xT, [l])
            xT = [np.ascontiguousarray(r["yT"]) for r in res]
    return np.stack([np.asarray(t).T for t in xT], axis=0).astype(np.float32)
```

```python
from contextlib import ExitStack
import numpy as np
import concourse.bass as bass
import concourse.mybir as mybir
from concourse.bass_utils import run_bass_kernel_spmd

F32 = mybir.dt.float32
BF16 = mybir.dt.bfloat16
AF = mybir.ActivationFunctionType
ALU = mybir.AluOpType

FUSED = True
DEPTH = 4
T = 2048
TT = 4
TW = 512
KC = 8
NW = 4
EPS = 1e-6
C_GQ, C_GK, C_GV, C_GG, C_GD, C_LX, C_LG, C_SQ, C_SK, C_SV, C_GATE = 0, 512, 1024, 2048, 3072, 3088, 4112, 5136, 6160, 7184, 8208
IN_COLS = 11280
P_GMIX, P_GMLP, P_GLAN, P_CW, P_CB, P_BA, P_BX, P_LAM, P_SQG, P_SKG, P_BG, NPP = 0, 8, 16, 18, 50, 58, 66, 74, 82, 83, 84, 108


class Buf:
    __slots__ = ("w", "r")

    def __init__(self):
        self.w = None
        self.r = {}


ENGS = ("pe", "act", "dve", "pool", "sp")
EPOCH = 20000
DMA_EPOCH = 1000


class Sched:
    def __init__(self, nc, n_dma_ch=8):
        self.nc = nc
        self.streams = {e: [] for e in ENGS}
        self.cnt = {e: 0 for e in ENGS}
        self.epoch = {e: 0 for e in ENGS}
        self.waited = {e: {} for e in ENGS}
        self.n_dma_ch = n_dma_ch
        self.dcnt = [0] * n_dma_ch
        self.depoch = [0] * n_dma_ch
        self.dnext = 0
        self.semkeys = []
        self._seen = set()
        self.latest = {}
        self.ninst = 0

    def _key(self, k):
        if k not in self._seen:
            self._seen.add(k)
            self.semkeys.append(k)
        return k

    def _filter(self, eng, need):
        out = []
        wd = self.waited[eng]
        for s, v in need.items():
            if eng == "pe" and s[0] == "pe":
                continue
            if wd.get(s, 0) < v:
                wd[s] = v
                out.append((s, v))
        return out

    def _deps(self, eng, reads, writes):
        need = {}
        for b in reads:
            if b.w is not None:
                s, v = b.w
                if need.get(s, 0) < v:
                    need[s] = v
        for b in writes:
            if b.w is not None:
                s, v = b.w
                if need.get(s, 0) < v:
                    need[s] = v
            for s, v in b.r.items():
                if need.get(s, 0) < v:
                    need[s] = v
        return self._filter(eng, need)

    def _mark(self, ev, reads, writes):
        s, v = ev
        self.latest[s] = v
        for b in reads:
            if b.r.get(s, 0) < v:
                b.r[s] = v
        for b in writes:
            b.w = ev
            b.r = {}

    def op(self, eng, fn, reads=(), writes=()):
        deps = self._deps(eng, reads, writes)
        if self.cnt[eng] >= EPOCH:
            self.epoch[eng] += 1
            self.cnt[eng] = 0
        self.cnt[eng] += 1
        key = self._key((eng, self.epoch[eng]))
        self.streams[eng].append((deps, fn, key, 1))
        self._mark((key, self.cnt[eng]), reads, writes)
        self.ninst += 1

    def dma(self, queue, fn, reads=(), writes=()):
        deps = self._deps(queue, reads, writes)
        ch = self.dnext
        self.dnext = (self.dnext + 1) % self.n_dma_ch
        if self.dcnt[ch] >= DMA_EPOCH:
            self.depoch[ch] += 1
            self.dcnt[ch] = 0
        self.dcnt[ch] += 1
        key = self._key(("dma%d" % ch, self.depoch[ch]))
        self.streams[queue].append((deps, fn, key, 16))
        self._mark((key, 16 * self.dcnt[ch]), reads, writes)
        self.ninst += 1

    def barrier(self):
        for eng in ENGS:
            deps = self._filter(eng, dict(self.latest))
            if deps:
                self.streams[eng].append((deps, None, None, 0))

    def emit(self, stack):
        nc = self.nc
        sems = {}
        for k in self.semkeys:
            sems[k] = stack.enter_context(nc.semaphore("s_%s_%d" % k))
        block = stack.enter_context(nc.Block())
        streams = self.streams

        def run(engobj, lst):
            for deps, fn, key, inc in lst:
                for s, v in deps:
                    engobj.wait_ge(sems[s], v)
                if fn is not None:
                    fn(engobj).then_inc(sems[key], inc)

        @block.tensor
        def _(e):
            run(e, streams["pe"])

        @block.scalar
        def _(e):
            run(e, streams["act"])

        @block.vector
        def _(e):
            run(e, streams["dve"])

        @block.gpsimd
        def _(e):
            run(e, streams["pool"])

        @block.sync
        def _(e):
            run(e, streams["sp"])


class Arena:
    def __init__(self, ap):
        self.ap = ap
        self.n = ap.shape[1]
        self.off = 0

    def f32(self, n):
        a = self.ap[:, self.off:self.off + n]
        self.off += n
        assert self.off <= self.n, ("arena overflow", self.off, self.n)
        return a

    def bf16(self, n):
        nn = (n + 1) // 2
        a = self.ap[:, self.off:self.off + nn].bitcast(BF16)
        self.off += nn
        assert self.off <= self.n, ("arena overflow", self.off, self.n)
        return a

    def reset(self):
        self.off = 0


class Ring:
    def __init__(self, aps):
        self.items = [(a, Buf()) for a in aps]
        self.i = 0

    def get(self):
        it = self.items[self.i]
        self.i = (self.i + 1) % len(self.items)
        return it


def tsl(tt):
    return slice(tt * TW, (tt + 1) * TW)


def build(NL, dbg=False):
    nc = bass.Bass("TRN2", target_bir_lowering=False)

    def din(name, shape):
        return nc.dram_tensor(name, shape, F32, kind="ExternalInput").ap()

    xT = din("xT", [1024, T])
    pp = din("pp", [NL, 128, NPP])
    w_in = din("w_in", [NL, 1024, IN_COLS])
    gla_w_up = din("gla_w_up", [NL, 16, 512])
    gla_b_alpha = din("gla_b_alpha", [NL, 512])
    lru_w_a = din("lru_w_a", [NL, 4, 256, 256])
    lru_w_x = din("lru_w_x", [NL, 4, 256, 256])
    w_br = [din("w_branch_a", [NL, 1024, 1024]), din("w_branch_b", [NL, 1024, 1024]), din("w_branch_c", [NL, 1024, 1024])]
    w_out = din("w_out", [NL, 1024, 1024])
    w_up = din("w_mlp_up", [NL, 1024, 4096])
    w_dn = din("w_mlp_down", [NL, 4096, 1024])
    yT = nc.dram_tensor("yT", [1024, T], F32, kind="ExternalOutput").ap()
    dbg_t = None
    if dbg:
        dbg_t = nc.dram_tensor("dbg", [6, 1024, T], F32, kind="ExternalOutput").ap()

    with ExitStack() as st:
        def sb(name, shape, dt):
            return st.enter_context(nc.sbuf_tensor(name, shape, dt))

        def psum(name, shape, dt):
            return st.enter_context(nc.psum_tensor(name, shape, dt))

        S = Sched(nc)
        xbuf_t = sb("xbuf", [128, KC * T], F32)
        xb = xbuf_t[:].rearrange("p (k t) -> p k t", k=KC)
        XB = [[Buf() for _ in range(TT)] for _ in range(KC)]
        xn_t = sb("xn", [128, KC * T], BF16)
        xn = xn_t[:].rearrange("p (k t) -> p k t", k=KC)
        XN = [[Buf() for _ in range(TT)] for _ in range(KC)]
        ob_t = sb("ob", [128, KC * T], BF16)
        ob = ob_t[:].rearrange("p (k t) -> p k t", k=KC)
        OB = [[Buf() for _ in range(TT)] for _ in range(KC)]
        wsl_t = sb("wsl", [128, NW * KC * 256], BF16)
        wsl = [wsl_t[:, i * KC * 256:(i + 1) * KC * 256].rearrange("p (k n) -> p k n", k=KC) for i in range(NW)]
        WB = [Buf() for _ in range(NW)]
        ppt = sb("ppt", [128, NL * NPP], F32)
        PPB = Buf()
        cst_bf = sb("cst_bf", [128, 128 * 5 + 256 + 2048 + 4 * 512 * 2], BF16)
        CB = Buf()
        cst_f = sb("cst_f", [128, 4], F32)
        ar_t = sb("arena", [128, 12200], F32)
        AR = Arena(ar_t[:])
        AR2 = Arena(xbuf_t[:])
        YT = [Buf() for _ in range(KC)]
        DBG = Buf()

        pbanks = [psum("pb%d" % i, [128, 512], F32) for i in range(7)]
        prot = Ring([pbanks[i][:] for i in range(4)])
        P4, P5, P6 = pbanks[4][:], pbanks[5][:], pbanks[6][:]
        PB4, PB5, PB6 = Buf(), Buf(), Buf()
        ptr_t = psum("ptr", [128, 1024], BF16)
        ptr = ptr_t[:]
        PTR = Buf()

        def getps():
            return prot.get()

        def mm(out, lhsT, rhs, start, stop, r, w):
            S.op("pe", lambda e: e.matmul(out, lhsT, rhs, start=start, stop=stop), r, w)

        def act(out, in_, func, r, w, bias=None, scale=None):
            kw = {}
            if bias is not None:
                kw["bias"] = bias
            if scale is not None:
                kw["scale"] = scale
            S.op("act", lambda e: e.activation(out=out, in_=in_, func=func, **kw), r, w)

        def tto(eng, out, in0, in1, op, r, w):
            S.op(eng, lambda e: e.tensor_tensor(out=out, in0=in0, in1=in1, op=op), r, w)

        def stt(eng, out, in0, scalar, in1, op0, op1, r, w):
            S.op(eng, lambda e: e.scalar_tensor_tensor(out=out, in0=in0, scalar=scalar, in1=in1, op0=op0, op1=op1), r, w)

        def tsc(eng, out, in0, s1, s2, op0, op1, r, w):
            S.op(eng, lambda e: e.tensor_scalar(out=out, in0=in0, scalar1=s1, scalar2=s2, op0=op0, op1=op1), r, w)

        def tsm(eng, out, in0, s1, r, w):
            S.op(eng, lambda e: e.tensor_scalar_mul(out=out, in0=in0, scalar1=s1), r, w)

        def cp(eng, out, in_, r, w):
            if eng == "act":
                S.op("act", lambda e: e.activation(out=out, in_=in_, func=AF.Copy), r, w)
            else:
                S.op(eng, lambda e: e.tensor_copy(out=out, in_=in_), r, w)

        def memset(eng, ap, val, w):
            S.op(eng, lambda e: e.memset(ap, val), (), w)

        def recip(out, in_, r, w):
            S.op("dve", lambda e: e.reciprocal(out=out, in_=in_), r, w)

        def dma(q, out, in_, r, w):
            S.dma(q, lambda e: e.dma_start(out=out, in_=in_), r, w)

        def asel(ap, pattern, cmp, base, cm, w):
            S.op("pool", lambda e: e.affine_select(out=ap, in_=ap, pattern=pattern, compare_op=cmp, fill=0.0, base=base, channel_multiplier=cm), w, w)

        o = 0
        ident_bf = cst_bf[:, o:o + 128]; o += 128
        ones_bf = cst_bf[:, o:o + 128]; o += 128
        maskG = cst_bf[:, o:o + 128]; o += 128
        TriS = cst_bf[:, o:o + 128]; o += 128
        NU = cst_bf[:, o:o + 128]; o += 128
        Eb = cst_bf[:, o:o + 256].rearrange("p (b m) -> p b m", b=16); o += 256
        Xb = cst_bf[:, o:o + 2048].rearrange("p (a s) -> p a s", a=16); o += 2048
        DM = []
        NM = []
        for r_ in range(4):
            DM.append(cst_bf[:, o:o + 512]); o += 512
        for r_ in range(4):
            NM.append(cst_bf[:, o:o + 512]); o += 512
        eps_t = cst_f[:, 0:1]
        one_t = cst_f[:, 1:2]
        memset("dve", cst_f[:, 0:1], EPS, [CB])
        memset("dve", cst_f[:, 1:2], 1.0, [CB])
        tmpc = AR.f32(2048)
        TB = Buf()

        def mk(dst, np_, nf, val, pattern, cmp, base, cm, post=None):
            t = tmpc[0:np_, 0:nf]
            memset("pool", t, val, [TB])
            if pattern is not None:
                tv = t if len(pattern) == 1 else t.rearrange("p (a b) -> p a b", a=pattern[0][1])
                asel(tv, pattern, cmp, base, cm, [TB])
            if post is not None:
                post(t)
            cp("dve", dst, t, [TB], [CB])

        mk(ident_bf, 128, 128, 1.0, [[-1, 128]], ALU.is_equal, 0, 1)
        mk(ones_bf, 128, 128, 1.0, None, None, 0, 0)
        mk(maskG, 128, 128, 1.0, [[1, 128]], ALU.is_ge, 0, -1, post=lambda t: memset("pool", t[0:64, 64:128], 0.0, [TB]))
        tsm("dve", TriS, maskG, -1.0 / 16.0, [CB], [CB])
        mk(NU, 128, 128, -1.0, [[-1, 128]], ALU.is_ge, 0, 1)
        mk(cst_bf[:, 640:896], 128, 256, 1.0, [[1, 16], [-1, 16]], ALU.is_equal, 0, 0)
        mk(cst_bf[0:16, 896:896 + 2048], 16, 2048, -1.0, [[-1, 16], [0, 128]], ALU.is_gt, 0, 1)
        for r_ in range(4):
            mk(DM[r_], 128, 512, 1.0, [[1, 512]], ALU.is_gt, -128 * r_, -1)
            tsc("dve", NM[r_], DM[r_], 30000.0, -30000.0, ALU.mult, ALU.add, [CB], [CB])
        for l in range(NL):
            dma("sp", ppt[:, l * NPP:(l + 1) * NPP], pp[l], [], [PPB])

        def par(l, col):
            return ppt[:, l * NPP + col:l * NPP + col + 1]

        class WQ:
            def __init__(self):
                self.plan = []
                self.issued = 0
                self.consumed = 0

            def extend(self, items):
                self.plan.extend(items)

            def get(self, D=2):
                while self.issued < min(len(self.plan), self.consumed + 1 + D):
                    src, n = self.plan[self.issued]
                    slot = self.issued % NW
                    dma("pool", wsl[slot][:, :, 0:n], src.rearrange("(k p) n -> p k n", p=128), [], [WB[slot]])
                    self.issued += 1
                slot = self.consumed % NW
                self.consumed += 1
                return slot

        wq = WQ()

        def proj(slot, c0, n, tt, src=None, RB=None):
            if src is None:
                src, RB = xn, XN
            ps, pb = getps()
            for k in range(KC):
                mm(ps[0:n, :], wsl[slot][:, k, c0:c0 + n], src[:, k, tsl(tt)], k == 0, k == KC - 1, [WB[slot], RB[k][tt]], [pb])
            return ps, pb

        def dump(idx, src, RBs, bf):
            if not dbg:
                return
            for k in range(KC):
                dma("pool" if bf else "sp", dbg_t[idx, k * 128:(k + 1) * 128, :], src[:, k, :], RBs[k], [DBG])

        def rmsnorm(l, gcol):
            AR.reset()
            sqr = Ring([AR.bf16(TW) for _ in range(3)])
            srr = Ring([AR.f32(TW) for _ in range(2)])
            for tt in range(TT):
                ps, pb = getps()
                for k in range(KC):
                    sq, sqb = sqr.get()
                    act(sq, xb[:, k, tsl(tt)], AF.Square, [XB[k][tt]], [sqb])
                    mm(ps, ones_bf, sq, k == 0, k == KC - 1, [sqb, CB], [pb])
                sr, srb = srr.get()
                act(sr, ps, AF.Sqrt, [pb, CB], [srb], bias=eps_t, scale=1.0 / 1024.0)
                recip(sr, sr, [srb], [srb])
                for k in range(KC):
                    stt("dve", xn[:, k, tsl(tt)], xb[:, k, tsl(tt)], par(l, gcol + k), sr, ALU.mult, ALU.mult,
                        [XB[k][tt], srb, PPB], [XN[k][tt]])

        def spill_x():
            for k in range(KC):
                dma("sp", yT[k * 128:(k + 1) * 128, :], xb[:, k, :], XB[k], [YT[k]])

        def reload_x():
            for k in range(KC):
                dma("sp", xb[:, k, :], yT[k * 128:(k + 1) * 128, :], [YT[k]], XB[k])

        def gla(l):
            AR.reset()
            AR2.reset()
            A = AR2
            adT = A.bf16(T); ADT = [Buf() for _ in range(TT)]
            wupa = A.bf16(512); WUPA = Buf()
            e1r = Ring([A.f32(TW) for _ in range(2)])
            Pr = Ring([A.bf16(TW) for _ in range(2)])
            ecr = Ring([A.f32(TW) for _ in range(2)])
            eir = Ring([A.f32(TW) for _ in range(2)])
            qd = A.bf16(T); QD = [Buf() for _ in range(TT)]
            ki = A.bf16(T); KI = [Buf() for _ in range(TT)]
            ki_tm = A.bf16(T); KITM = [Buf() for _ in range(TT)]
            v_tm = A.bf16(16 * 256); VTM = [Buf() for _ in range(8)]
            sg = [A.bf16(T), A.bf16(T)]; SG = [[Buf() for _ in range(TT)] for _ in range(2)]
            Sst = A.f32(256); SST = Buf()
            tmpS = Ring([A.f32(256) for _ in range(2)])
            S_bf = [A.bf16(256), A.bf16(256)]; SBF = [Buf(), Buf()]
            scr = Ring([A.bf16(128) for _ in range(3)])
            dec = A.f32(32); DEC = Buf()
            sqr = Ring([A.bf16(TW) for _ in range(2)])
            srr = Ring([A.f32(TW) for _ in range(2)])
            t1r = Ring([A.f32(TW) for _ in range(2)])
            for tt in range(TT):
                memset("dve", adT[0:33, tsl(tt)], 1.0, [ADT[tt]])
            memset("dve", wupa[0:33, :], 0.0, [WUPA])
            dma("pool", wupa[0:16, :], gla_w_up[l], [], [WUPA])
            dma("pool", wupa[32:33, :], gla_b_alpha[l:l + 1, :], [], [WUPA])
            plan = [(w_in[l][:, C_GD:C_GD + 16], 16)]
            for h in range(4):
                plan += [(w_in[l][:, C_GQ + h * 128:C_GQ + (h + 1) * 128], 128),
                         (w_in[l][:, C_GK + h * 128:C_GK + (h + 1) * 128], 128),
                         (w_in[l][:, C_GV + h * 256:C_GV + (h + 1) * 256], 256),
                         (w_in[l][:, C_GG + h * 256:C_GG + (h + 1) * 256], 256)]
            wq.extend(plan)
            s_ = wq.get(D=1)
            for tt in range(TT):
                ps, pb = proj(s_, 0, 16, tt)
                cp("act", adT[0:16, tsl(tt)], ps[0:16, :], [pb], [ADT[tt]])
            for h in range(4):
                s_q = wq.get(D=2)
                s_k = wq.get(D=2)
                for tt in range(TT):
                    psy, pyb = getps()
                    for mi in range(4):
                        m = tt * 4 + mi
                        mm(psy[:, mi * 128:(mi + 1) * 128], adT[0:33, m * 128:(m + 1) * 128], wupa[0:33, h * 128:(h + 1) * 128],
                           True, True, [ADT[tt], WUPA], [pyb])
                    e1, e1b = e1r.get()
                    act(e1, psy, AF.Exp, [pyb], [e1b], scale=-1.0)
                    Pt, Ptb = Pr.get()
                    act(Pt, e1, AF.Ln, [e1b, CB], [Ptb], bias=one_t)
                    psc, pcb = getps()
                    for mi in range(4):
                        mm(psc[:, mi * 128:(mi + 1) * 128], Pt[:, mi * 128:(mi + 1) * 128], TriS, True, True, [Ptb, CB], [pcb])
                    ec, ecb = ecr.get()
                    ei, eib = eir.get()
                    act(ec, psc, AF.Exp, [pcb], [ecb])
                    act(ei, psc, AF.Exp, [pcb], [eib], scale=-1.0)
                    cp("dve", dec[:, tt * 8:(tt + 1) * 8], ec[:, 63::64], [ecb], [DEC])
                    psq, pqb = proj(s_q, 0, 128, tt)
                    stt("dve", qd[:, tsl(tt)], psq, 128.0 ** -0.5, ec, ALU.mult, ALU.mult, [pqb, ecb], [QD[tt]])
                    psk, pkb = proj(s_k, 0, 128, tt)
                    tto("dve", ki[:, tsl(tt)], psk, ei, ALU.mult, [pkb, eib], [KI[tt]])
                    for mi in range(4):
                        m = tt * 4 + mi
                        S.op("pe", (lambda o_, i_: (lambda e: e.transpose(o_, i_, ident_bf)))(ptr[:, mi * 128:(mi + 1) * 128], ki[:, m * 128:(m + 1) * 128]),
                             [KI[tt], CB], [PTR])
                    cp("act", ki_tm[:, tsl(tt)], ptr[:, 0:512], [PTR], [KITM[tt]])
                s_v = wq.get(D=2)
                for m2 in range(8):
                    ps, pb = getps()
                    for mj in range(2):
                        m = m2 * 2 + mj
                        for k in range(KC):
                            mm(ps[:, mj * 256:(mj + 1) * 256], xn[:, k, m * 128:(m + 1) * 128], wsl[s_v][:, k, 0:256],
                               k == 0, k == KC - 1, [XN[k][m // 4], WB[s_v]], [pb])
                    cp("act", v_tm[:, m2 * 512:(m2 + 1) * 512], ps, [pb], [VTM[m2]])
                s_g = wq.get(D=2)
                for jb in range(2):
                    for tt in range(TT):
                        ps, pb = proj(s_g, jb * 128, 128, tt)
                        act(sg[jb][:, tsl(tt)], ps, AF.Silu, [pb], [SG[jb][tt]])
                memset("dve", Sst, 0.0, [SST])
                memset("dve", S_bf[0], 0.0, [SBF[0]])
                for tt in range(TT):
                    po = [P4, P5]
                    POB = [PB4, PB5]
                    for mi in range(4):
                        m = tt * 4 + mi
                        pss, psb = getps()
                        mm(pss[:, 0:128], ki[:, m * 128:(m + 1) * 128], qd[:, m * 128:(m + 1) * 128], True, True, [KI[tt], QD[tt]], [psb])
                        sc, scb = scr.get()
                        tto("dve", sc, pss[:, 0:128], maskG, ALU.mult, [psb, CB], [scb])
                        for half in range(2):
                            c = 2 * m + half
                            cur = c % 2
                            for jb in range(2):
                                col = mi * 128 + half * 64
                                mm(po[jb][:, col:col + 64], S_bf[cur][:, jb * 128:(jb + 1) * 128], qd[:, c * 64:(c + 1) * 64],
                                   True, False, [SBF[cur], QD[tt]], [POB[jb]])
                                mm(po[jb][:, col:col + 64], v_tm[:, m * 256 + jb * 128:m * 256 + (jb + 1) * 128], sc[:, half * 64:(half + 1) * 64],
                                   False, True, [VTM[m // 2], scb], [POB[jb]])
                            pkv, pkb = getps()
                            mm(pkv[:, 0:256], ki_tm[half * 64:(half + 1) * 64, m * 128:(m + 1) * 128],
                               v_tm[half * 64:(half + 1) * 64, m * 256:(m + 1) * 256], True, True, [KITM[tt], VTM[m // 2]], [pkb])
                            ts_, tsb = tmpS.get()
                            tto("dve", ts_, pkv[:, 0:256], Sst, ALU.add, [pkb, SST], [tsb])
                            tsm("dve", Sst, ts_, dec[:, c:c + 1], [tsb, DEC], [SST])
                            act(S_bf[1 - cur], ts_, AF.Identity, [tsb, DEC], [SBF[1 - cur]], scale=dec[:, c:c + 1])
                    pn, pnb = getps()
                    for jb in range(2):
                        sq, sqb = sqr.get()
                        act(sq, po[jb], AF.Square, [POB[jb]], [sqb])
                        mm(pn, ones_bf, sq, jb == 0, jb == 1, [sqb, CB], [pnb])
                    sr, srb = srr.get()
                    act(sr, pn, AF.Sqrt, [pnb, CB], [srb], bias=eps_t, scale=1.0 / 256.0)
                    recip(sr, sr, [srb], [srb])
                    for jb in range(2):
                        t1, t1b = t1r.get()
                        tto("dve", t1, po[jb], sr, ALU.mult, [POB[jb], srb], [t1b])
                        stt("dve", ob[:, h * 2 + jb, tsl(tt)], t1, par(l, P_GLAN + jb), sg[jb][:, tsl(tt)], ALU.mult, ALU.mult,
                            [t1b, PPB, SG[jb][tt]], [OB[h * 2 + jb][tt]])

        def branch(l, b):
            AR.reset()
            gr = Ring([AR.f32(TW) for _ in range(2)])
            tr = Ring([AR.f32(TW) for _ in range(2)])
            plan = []
            for nbp in range(4):
                plan += [(w_br[b][l][:, nbp * 256:(nbp + 1) * 256], 256),
                         (w_in[l][:, C_GATE + b * 1024 + nbp * 256:C_GATE + b * 1024 + (nbp + 1) * 256], 256)]
            wq.extend(plan)
            for nbp in range(4):
                s1 = wq.get(D=2)
                s2 = wq.get(D=2)
                for nbi in range(2):
                    nb = nbp * 2 + nbi
                    for tt in range(TT):
                        ps1, p1b = proj(s1, nbi * 128, 128, tt, ob, OB)
                        ps2, p2b = proj(s2, nbi * 128, 128, tt)
                        g, gb = gr.get()
                        act(g, ps2, AF.Sigmoid, [p2b, PPB], [gb], bias=par(l, P_BG + b * 8 + nb))
                        if b == 0:
                            tto("dve", xb[:, nb, tsl(tt)], ps1, g, ALU.mult, [p1b, gb], [XB[nb][tt]])
                        else:
                            t_, tb = tr.get()
                            tto("dve", t_, ps1, g, ALU.mult, [p1b, gb], [tb])
                            tto("dve", xb[:, nb, tsl(tt)], xb[:, nb, tsl(tt)], t_, ALU.add, [tb, XB[nb][tt]], [XB[nb][tt]])

        def lru(l):
            AR.reset()
            A = AR
            cl = A.f32(8); CL = Buf()
            tmp8 = A.f32(8)
            lxbf = [A.bf16(T + 4), A.bf16(T + 4)]; LXB = [[Buf() for _ in range(TT)] for _ in range(2)]
            dg = [[A.bf16(128) for _ in range(4)] for _ in range(2)]; DG = Buf()
            wab = A.bf16(512).rearrange("p (i j) -> p i j", i=2); wxb = A.bf16(512).rearrange("p (i j) -> p i j", i=2); WAB = Buf(); WXB = Buf()
            xcf = [Ring([A.f32(TW) for _ in range(2)]) for _ in range(2)]
            xcb = [Ring([A.bf16(TW) for _ in range(1)]) for _ in range(2)]
            rr = Ring([A.f32(TW) for _ in range(2)])
            igr = Ring([A.f32(TW) for _ in range(2)])
            n2r = Ring([A.f32(TW) for _ in range(1)])
            hr = [Ring([A.f32(TW) for _ in range(2)]) for _ in range(2)]
            glr = Ring([A.f32(TW) for _ in range(2)])
            g2r = Ring([A.f32(TW) for _ in range(1)])
            act(tmp8, ppt[:, l * NPP + P_LAM:l * NPP + P_LAM + 8], AF.Exp, [PPB], [CL], scale=-1.0)
            act(tmp8, tmp8, AF.Ln, [CL, CB], [CL], bias=one_t)
            tsm("dve", cl, tmp8, -8.0, [CL], [CL])
            plan = []
            for hb in range(4):
                plan += [(w_in[l][:, C_LX + hb * 256:C_LX + (hb + 1) * 256], 256), (w_in[l][:, C_LG + hb * 256:C_LG + (hb + 1) * 256], 256)]
            wq.extend(plan)
            for hb in range(4):
                slx = wq.get(D=2)
                slg = wq.get(D=2)
                dma("pool", wab, lru_w_a[l, hb].rearrange("(i p) j -> p i j", p=128), [], [WAB])
                dma("pool", wxb, lru_w_x[l, hb].rearrange("(i p) j -> p i j", p=128), [], [WXB])
                for cbl in range(2):
                    memset("dve", lxbf[cbl][:, 0:3], 0.0, [LXB[cbl][0]])
                    for w_ in range(4):
                        tsm("dve", dg[cbl][w_], ident_bf, par(l, P_CW + (hb * 2 + cbl) * 4 + w_), [CB, PPB], [DG])
                hprev = [None, None]
                for tt in range(TT):
                    for cbl in range(2):
                        ps, pb = proj(slx, cbl * 128, 128, tt)
                        cp("act", lxbf[cbl][:, 3 + tt * TW:3 + (tt + 1) * TW], ps, [pb], [LXB[cbl][tt]])
                    xf = []
                    xh = []
                    for cbl in range(2):
                        ps, pb = getps()
                        rd = [LXB[cbl][tt], DG] + ([LXB[cbl][tt - 1]] if tt > 0 else [])
                        for w_ in range(4):
                            mm(ps, dg[cbl][w_], lxbf[cbl][:, tt * TW + w_:tt * TW + w_ + TW], w_ == 0, w_ == 3, rd, [pb])
                        f_, fb = xcf[cbl].get()
                        act(f_, ps, AF.Identity, [pb, PPB], [fb], bias=par(l, P_CB + hb * 2 + cbl))
                        h_, hb_ = xcb[cbl].get()
                        cp("dve", h_, f_, [fb], [hb_])
                        xf.append((f_, fb))
                        xh.append((h_, hb_))
                    for jb in range(2):
                        c = hb * 2 + jb
                        psr, prb = getps()
                        for ib in range(2):
                            mm(psr, wab[:, ib, jb * 128:(jb + 1) * 128], xh[ib][0], ib == 0, ib == 1, [WAB, xh[ib][1]], [prb])
                        psi, pib = getps()
                        for ib in range(2):
                            mm(psi, wxb[:, ib, jb * 128:(jb + 1) * 128], xh[ib][0], ib == 0, ib == 1, [WXB, xh[ib][1]], [pib])
                        r_, rb = rr.get()
                        act(r_, psr, AF.Sigmoid, [prb, PPB], [rb], bias=par(l, P_BA + c))
                        a_, ab = r_, rb
                        act(a_, r_, AF.Exp, [rb, CL], [ab], scale=cl[:, c:c + 1])
                        ig, igb = igr.get()
                        act(ig, psi, AF.Sigmoid, [pib, PPB], [igb], bias=par(l, P_BX + c))
                        n2, n2b = n2r.get()
                        stt("dve", n2, a_, -1.0, a_, ALU.mult, ALU.mult, [ab], [n2b])
                        act(n2, n2, AF.Sqrt, [n2b, CB], [n2b], bias=one_t)
                        u_, ub = ig, igb
                        tto("dve", u_, n2, ig, ALU.mult, [n2b, igb], [ub])
                        tto("dve", u_, u_, xf[jb][0], ALU.mult, [ub, xf[jb][1]], [ub])
                        hh, hhb = hr[jb].get()
                        if hprev[jb] is None:
                            S.op("dve", (lambda o_, a0, u0: (lambda e: e.tensor_tensor_scan(out=o_, data0=a0, data1=u0, initial=0.0, op0=ALU.mult, op1=ALU.add)))(hh, a_, u_),
                                 [ab, ub], [hhb])
                        else:
                            hp, hpb = hprev[jb]
                            S.op("dve", (lambda o_, a0, u0, i0: (lambda e: e.tensor_tensor_scan(out=o_, data0=a0, data1=u0, initial=i0, op0=ALU.mult, op1=ALU.add)))(hh, a_, u_, hp[:, TW - 1:TW]),
                                 [ab, ub, hpb], [hhb])
                        hprev[jb] = (hh, hhb)
                        psg, pgb = proj(slg, jb * 128, 128, tt)
                        gl, glb = glr.get()
                        g2, g2b = g2r.get()
                        act(gl, psg, AF.Identity, [pgb], [glb])
                        tto("dve", g2, gl, gl, ALU.mult, [glb], [g2b])
                        tsc("dve", g2, g2, 0.044715, 1.0, ALU.mult, ALU.add, [g2b], [g2b])
                        tto("dve", g2, g2, gl, ALU.mult, [g2b, glb], [g2b])
                        act(g2, g2, AF.Sigmoid, [g2b], [g2b], scale=1.5957691216057308)
                        tto("dve", gl, gl, hh, ALU.mult, [glb, hhb], [glb])
                        tto("dve", ob[:, c, tsl(tt)], gl, g2, ALU.mult, [glb, g2b], [OB[c][tt]])

        def sbattn(l):
            AR.reset()
            A = AR
            Lk = A.bf16(16 * TW); LK = [Buf() for _ in range(16)]
            qn = A.bf16(T); QN = [Buf() for _ in range(TT)]
            kn = A.bf16(T); KN = [Buf() for _ in range(TT)]
            v_tm = A.bf16(T); VT = [Buf() for _ in range(4)]
            Er = Ring([A.f32(TW) for _ in range(2)])
            wr = Ring([A.bf16(TW) for _ in range(3)])
            sqr = Ring([A.bf16(TW) for _ in range(2)])
            srr = Ring([A.f32(TW) for _ in range(2)])
            csr = Ring([A.bf16(TW) for _ in range(2)])
            gq2 = A.f32(2); GQ = Buf()
            tsm("dve", gq2[:, 0:1], par(l, P_SQG), 128.0 ** -0.5, [PPB], [GQ])
            cp("dve", gq2[:, 1:2], par(l, P_SKG), [PPB], [GQ])
            plan = []
            for h in range(8):
                plan += [(w_in[l][:, C_SQ + h * 128:C_SQ + (h + 1) * 128], 128), (w_in[l][:, C_SK + h * 128:C_SK + (h + 1) * 128], 128),
                         (w_in[l][:, C_SV + h * 128:C_SV + (h + 1) * 128], 128)]
            wq.extend(plan)
            for h in range(8):
                for which, (dst, DB) in enumerate(((qn, QN), (kn, KN))):
                    s_ = wq.get(D=2)
                    for tt in range(TT):
                        ps, pb = proj(s_, 0, 128, tt)
                        sq, sqb = sqr.get()
                        act(sq, ps, AF.Square, [pb], [sqb])
                        pn, pnb = getps()
                        mm(pn, ones_bf, sq, True, True, [sqb, CB], [pnb])
                        sr, srb = srr.get()
                        act(sr, pn, AF.Sqrt, [pnb, CB], [srb], bias=eps_t, scale=1.0 / 128.0)
                        recip(sr, sr, [srb], [srb])
                        stt("dve", dst[:, tsl(tt)], ps, gq2[:, which:which + 1], sr, ALU.mult, ALU.mult, [pb, srb, GQ], [DB[tt]])
                s_v = wq.get(D=2)
                for m4 in range(4):
                    ps, pb = getps()
                    for mi in range(4):
                        m = m4 * 4 + mi
                        for k in range(KC):
                            mm(ps[:, mi * 128:(mi + 1) * 128], xn[:, k, m * 128:(m + 1) * 128], wsl[s_v][:, k, 0:128],
                               k == 0, k == KC - 1, [XN[k][m4], WB[s_v]], [pb])
                    cp("act", v_tm[:, m4 * 512:(m4 + 1) * 512], ps, [pb], [VT[m4]])
                for qt in range(TT):
                    nk = 4 * (qt + 1)
                    for a in range(nk):
                        psz, pzb = getps()
                        mm(psz, kn[:, a * 128:(a + 1) * 128], qn[:, tsl(qt)], True, True, [KN[a // 4], QN[qt]], [pzb])
                        E, Eb_ = Er.get()
                        act(E, psz, AF.Exp, [pzb], [Eb_])
                        La = Lk[:, a * TW:(a + 1) * TW]
                        act(La, E, AF.Ln, [Eb_, CB], [LK[a]], bias=one_t)
                        if a >= 4 * qt:
                            tto("dve", La, La, DM[a - 4 * qt], ALU.mult, [LK[a], CB], [LK[a]])
                        mm(P4[0:16, :], Eb[:, a, :], La, a == 0, a == nk - 1, [LK[a], CB], [PB4])
                    cs, csb = csr.get()
                    cp("dve", cs[0:16, :], P4[0:16, :], [PB4], [csb])
                    po, pob = (P5, PB5) if qt % 2 == 0 else (P6, PB6)
                    for a in range(nk):
                        diag = a >= 4 * qt
                        psw, pwb = getps()
                        La = Lk[:, a * TW:(a + 1) * TW]
                        mm(psw, kn[:, a * 128:(a + 1) * 128], qn[:, tsl(qt)], True, False, [KN[a // 4], QN[qt]], [pwb])
                        mm(psw, NU, La, False, False, [CB, LK[a]], [pwb])
                        mm(psw, Xb[0:16, a, :], cs[0:16, :], False, not diag, [CB, csb], [pwb])
                        if diag:
                            mm(psw, ident_bf, NM[a - 4 * qt], False, True, [CB], [pwb])
                        w_, wb_ = wr.get()
                        act(w_, psw, AF.Exp, [pwb], [wb_])
                        mm(po, v_tm[:, a * 128:(a + 1) * 128], w_, a == 0, a == nk - 1, [VT[a // 4], wb_], [pob])
                    cp("dve", ob[:, h, tsl(qt)], po, [pob], [OB[h][qt]])

        def wout(l):
            AR.reset()
            for nb in range(KC):
                for tt in range(TT):
                    cp("act" if (nb + tt) % 2 == 0 else "dve", ob[:, nb, tsl(tt)], xb[:, nb, tsl(tt)], [XB[nb][tt]], [OB[nb][tt]])
            dump(4, ob, OB, True)
            reload_x()
            wq.extend([(w_out[l][:, i * 256:(i + 1) * 256], 256) for i in range(4)])
            for i in range(4):
                s_ = wq.get(D=2)
                for nbi in range(2):
                    nb = i * 2 + nbi
                    for tt in range(TT):
                        ps, pb = proj(s_, nbi * 128, 128, tt, ob, OB)
                        tto("dve", xb[:, nb, tsl(tt)], ps, xb[:, nb, tsl(tt)], ALU.add, [pb, XB[nb][tt]], [XB[nb][tt]])

        def mlp(l):
            rmsnorm(l, P_GMLP)
            rr = Ring([AR.f32(TW) for _ in range(3)])
            plan = []
            for fg in range(4):
                plan += [(w_up[l][:, fg * 1024 + i * 256:fg * 1024 + (i + 1) * 256], 256) for i in range(4)]
                plan += [(w_dn[l][fg * 1024:(fg + 1) * 1024, i * 256:(i + 1) * 256], 256) for i in range(4)]
            wq.extend(plan)
            for fg in range(4):
                for i in range(4):
                    s_ = wq.get(D=3)
                    for fbi in range(2):
                        fb = i * 2 + fbi
                        for tt in range(TT):
                            ps, pb = proj(s_, fbi * 128, 128, tt)
                            r_, rb = rr.get()
                            act(r_, ps, AF.Relu, [pb], [rb])
                            tto("dve", ob[:, fb, tsl(tt)], r_, r_, ALU.mult, [rb], [OB[fb][tt]])
                for i in range(4):
                    s_ = wq.get(D=3)
                    for nbi in range(2):
                        nb = i * 2 + nbi
                        for tt in range(TT):
                            ps, pb = proj(s_, nbi * 128, 128, tt, ob, OB)
                            tto("dve", xb[:, nb, tsl(tt)], ps, xb[:, nb, tsl(tt)], ALU.add, [pb, XB[nb][tt]], [XB[nb][tt]])

        S.barrier()
        for k in range(KC):
            dma("sp", xb[:, k, :], xT[k * 128:(k + 1) * 128, :], [], XB[k])
        for l in range(NL):
            rmsnorm(l, P_GMIX)
            if l == 0:
                dump(0, xn, XN, True)
            spill_x()
            S.barrier()
            gla(l)
            if l == 0:
                dump(1, ob, OB, True)
            S.barrier()
            branch(l, 0)
            S.barrier()
            lru(l)
            if l == 0:
                dump(2, ob, OB, True)
            S.barrier()
            branch(l, 1)
            S.barrier()
            sbattn(l)
            if l == 0:
                dump(3, ob, OB, True)
            S.barrier()
            branch(l, 2)
            S.barrier()
            wout(l)
            if l == 0:
                dump(5, xb, XB, False)
            S.barrier()
            mlp(l)
            S.barrier()
        for k in range(KC):
            dma("sp", yT[k * 128:(k + 1) * 128, :], xb[:, k, :], XB[k], [YT[k]])
        S.barrier()
        S.emit(st)
        print("instructions:", S.ninst, {e: len(S.streams[e]) for e in ENGS})
    return nc


def pack_params(inp, layers):
    def fm(v, nb):
        return np.ascontiguousarray(np.asarray(v, np.float32).reshape(nb, 128).T)

    out = np.zeros((len(layers), 128, NPP), np.float32)
    for i, l in enumerate(layers):
        out[i, :, P_GMIX:P_GMIX + 8] = fm(inp["norm_mix_g"][l], 8)
        out[i, :, P_GMLP:P_GMLP + 8] = fm(inp["norm_mlp_g"][l], 8)
        out[i, :, P_GLAN:P_GLAN + 2] = fm(inp["gla_norm_g"][l], 2)
        cw = np.asarray(inp["lru_conv_w"][l], np.float32)
        for cb in range(8):
            for w_ in range(4):
                out[i, :, P_CW + cb * 4 + w_] = cw[w_, cb * 128:(cb + 1) * 128]
        out[i, :, P_CB:P_CB + 8] = fm(inp["lru_conv_b"][l], 8)
        out[i, :, P_BA:P_BA + 8] = fm(inp["lru_b_a"][l], 8)
        out[i, :, P_BX:P_BX + 8] = fm(inp["lru_b_x"][l], 8)
        out[i, :, P_LAM:P_LAM + 8] = fm(inp["lru_lambda"][l], 8)
        out[i, :, P_SQG] = np.asarray(inp["sb_q_norm_g"][l], np.float32)
        out[i, :, P_SKG] = np.asarray(inp["sb_k_norm_g"][l], np.float32)
        out[i, :, P_BG:P_BG + 24] = fm(inp["b_gate"][l], 24)
    return out


_NC_CACHE = {}
WNAMES = ["w_in", "gla_w_up", "gla_b_alpha", "lru_w_a", "lru_w_x", "w_branch_a", "w_branch_b", "w_branch_c", "w_out", "w_mlp_up", "w_mlp_down"]


def _get_nc(NL, dbg=False):
    key = (NL, dbg)
    if key not in _NC_CACHE:
        _NC_CACHE[key] = build(NL, dbg)
    return _NC_CACHE[key]


def run_layers(inp, xT_list, layers, dbg=False, cores=8):
    nc = _get_nc(len(layers), dbg)
    ppk = pack_params(inp, layers)
    shared = {"pp": ppk}
    for n in WNAMES:
        shared[n] = np.ascontiguousarray(np.asarray(inp[n], np.float32)[layers[0]:layers[-1] + 1])
    in_maps = []
    for c in range(cores):
        d = dict(shared)
        d["xT"] = xT_list[c]
        in_maps.append(d)
    res = run_bass_kernel_spmd(nc, in_maps, core_ids=list(range(cores)))
    return res.results


def kernel(**inputs):
    x = np.asarray(inputs["x"], np.float32)
    B = x.shape[0]
    xT = [np.ascontiguousarray(x[b].T) for b in range(B)]
    if FUSED:
        res = run_layers(inputs, xT, list(range(DEPTH)))
        xT = [r["yT"] for r in res]
    else:
        for l in range(DEPTH):
            res = run_layers(inputs, xT, [l])
            xT = [np.ascontiguousarray(r["yT"]) for r in res]
    return np.stack([np.asarray(t).T for t in xT], axis=0).astype(np.float32)
```

```python
from contextlib import ExitStack
import numpy as np
import concourse.bass as bass
import concourse.mybir as mybir
from concourse.bass_utils import run_bass_kernel_spmd

F32 = mybir.dt.float32
BF16 = mybir.dt.bfloat16
AF = mybir.ActivationFunctionType
ALU = mybir.AluOpType

FUSED = True
DEPTH = 4
T = 2048
TT = 4
TW = 512
KC = 8
NW = 4
EPS = 1e-6
C_GQ, C_GK, C_GV, C_GG, C_GD, C_LX, C_LG, C_SQ, C_SK, C_SV, C_GATE = 0, 512, 1024, 2048, 3072, 3088, 4112, 5136, 6160, 7184, 8208
IN_COLS = 11280
P_GMIX, P_GMLP, P_GLAN, P_CW, P_CB, P_BA, P_BX, P_LAM, P_SQG, P_SKG, P_BG, NPP = 0, 8, 16, 18, 50, 58, 66, 74, 82, 83, 84, 108


class Buf:
    __slots__ = ("w", "r")

    def __init__(self):
        self.w = None
        self.r = {}


ENGS = ("pe", "act", "dve", "pool", "sp")
EPOCH = 20000
DMA_EPOCH = 1000


class Sched:
    def __init__(self, nc, n_dma_ch=8):
        self.nc = nc
        self.streams = {e: [] for e in ENGS}
        self.cnt = {e: 0 for e in ENGS}
        self.epoch = {e: 0 for e in ENGS}
        self.waited = {e: {} for e in ENGS}
        self.n_dma_ch = n_dma_ch
        self.dcnt = [0] * n_dma_ch
        self.depoch = [0] * n_dma_ch
        self.dnext = 0
        self.semkeys = []
        self._seen = set()
        self.latest = {}
        self.ninst = 0

    def _key(self, k):
        if k not in self._seen:
            self._seen.add(k)
            self.semkeys.append(k)
        return k

    def _filter(self, eng, need):
        out = []
        wd = self.waited[eng]
        for s, v in need.items():
            if eng == "pe" and s[0] == "pe":
                continue
            if wd.get(s, 0) < v:
                wd[s] = v
                out.append((s, v))
        return out

    def _deps(self, eng, reads, writes):
        need = {}
        for b in reads:
            if b.w is not None:
                s, v = b.w
                if need.get(s, 0) < v:
                    need[s] = v
        for b in writes:
            if b.w is not None:
                s, v = b.w
                if need.get(s, 0) < v:
                    need[s] = v
            for s, v in b.r.items():
                if need.get(s, 0) < v:
                    need[s] = v
        return self._filter(eng, need)

    def _mark(self, ev, reads, writes):
        s, v = ev
        self.latest[s] = v
        for b in reads:
            if b.r.get(s, 0) < v:
                b.r[s] = v
        for b in writes:
            b.w = ev
            b.r = {}

    def op(self, eng, fn, reads=(), writes=()):
        deps = self._deps(eng, reads, writes)
        if self.cnt[eng] >= EPOCH:
            self.epoch[eng] += 1
            self.cnt[eng] = 0
        self.cnt[eng] += 1
        key = self._key((eng, self.epoch[eng]))
        self.streams[eng].append((deps, fn, key, 1))
        self._mark((key, self.cnt[eng]), reads, writes)
        self.ninst += 1

    def dma(self, queue, fn, reads=(), writes=()):
        deps = self._deps(queue, reads, writes)
        ch = self.dnext
        self.dnext = (self.dnext + 1) % self.n_dma_ch
        if self.dcnt[ch] >= DMA_EPOCH:
            self.depoch[ch] += 1
            self.dcnt[ch] = 0
        self.dcnt[ch] += 1
        key = self._key(("dma%d" % ch, self.depoch[ch]))
        self.streams[queue].append((deps, fn, key, 16))
        self._mark((key, 16 * self.dcnt[ch]), reads, writes)
        self.ninst += 1

    def barrier(self):
        for eng in ENGS:
            deps = self._filter(eng, dict(self.latest))
            if deps:
                self.streams[eng].append((deps, None, None, 0))

    def emit(self, stack):
        nc = self.nc
        sems = {}
        for k in self.semkeys:
            sems[k] = stack.enter_context(nc.semaphore("s_%s_%d" % k))
        block = stack.enter_context(nc.Block())
        streams = self.streams

        def run(engobj, lst):
            for deps, fn, key, inc in lst:
                for s, v in deps:
                    engobj.wait_ge(sems[s], v)
                if fn is not None:
                    fn(engobj).then_inc(sems[key], inc)

        @block.tensor
        def _(e):
            run(e, streams["pe"])

        @block.scalar
        def _(e):
            run(e, streams["act"])

        @block.vector
        def _(e):
            run(e, streams["dve"])

        @block.gpsimd
        def _(e):
            run(e, streams["pool"])

        @block.sync
        def _(e):
            run(e, streams["sp"])


class Arena:
    def __init__(self, ap):
        self.ap = ap
        self.n = ap.shape[1]
        self.off = 0

    def f32(self, n):
        a = self.ap[:, self.off:self.off + n]
        self.off += n
        assert self.off <= self.n, ("arena overflow", self.off, self.n)
        return a

    def bf16(self, n):
        nn = (n + 1) // 2
        a = self.ap[:, self.off:self.off + nn].bitcast(BF16)
        self.off += nn
        assert self.off <= self.n, ("arena overflow", self.off, self.n)
        return a

    def reset(self):
        self.off = 0


class Ring:
    def __init__(self, aps):
        self.items = [(a, Buf()) for a in aps]
        self.i = 0

    def get(self):
        it = self.items[self.i]
        self.i = (self.i + 1) % len(self.items)
        return it


def tsl(tt):
    return slice(tt * TW, (tt + 1) * TW)


def build(NL, dbg=False):
    nc = bass.Bass("TRN2", target_bir_lowering=False)

    def din(name, shape):
        return nc.dram_tensor(name, shape, F32, kind="ExternalInput").ap()

    xT = din("xT", [1024, T])
    pp = din("pp", [NL, 128, NPP])
    w_in = din("w_in", [NL, 1024, IN_COLS])
    gla_w_up = din("gla_w_up", [NL, 16, 512])
    gla_b_alpha = din("gla_b_alpha", [NL, 512])
    lru_w_a = din("lru_w_a", [NL, 4, 256, 256])
    lru_w_x = din("lru_w_x", [NL, 4, 256, 256])
    w_br = [din("w_branch_a", [NL, 1024, 1024]), din("w_branch_b", [NL, 1024, 1024]), din("w_branch_c", [NL, 1024, 1024])]
    w_out = din("w_out", [NL, 1024, 1024])
    w_up = din("w_mlp_up", [NL, 1024, 4096])
    w_dn = din("w_mlp_down", [NL, 4096, 1024])
    yT = nc.dram_tensor("yT", [1024, T], F32, kind="ExternalOutput").ap()
    dbg_t = None
    if dbg:
        dbg_t = nc.dram_tensor("dbg", [6, 1024, T], F32, kind="ExternalOutput").ap()

    with ExitStack() as st:
        def sb(name, shape, dt):
            return st.enter_context(nc.sbuf_tensor(name, shape, dt))

        def psum(name, shape, dt):
            return st.enter_context(nc.psum_tensor(name, shape, dt))

        S = Sched(nc)
        xbuf_t = sb("xbuf", [128, KC * T], F32)
        xb = xbuf_t[:].rearrange("p (k t) -> p k t", k=KC)
        XB = [[Buf() for _ in range(TT)] for _ in range(KC)]
        xn_t = sb("xn", [128, KC * T], BF16)
        xn = xn_t[:].rearrange("p (k t) -> p k t", k=KC)
        XN = [[Buf() for _ in range(TT)] for _ in range(KC)]
        ob_t = sb("ob", [128, KC * T], BF16)
        ob = ob_t[:].rearrange("p (k t) -> p k t", k=KC)
        OB = [[Buf() for _ in range(TT)] for _ in range(KC)]
        wsl_t = sb("wsl", [128, NW * KC * 256], BF16)
        wsl = [wsl_t[:, i * KC * 256:(i + 1) * KC * 256].rearrange("p (k n) -> p k n", k=KC) for i in range(NW)]
        WB = [Buf() for _ in range(NW)]
        ppt = sb("ppt", [128, NL * NPP], F32)
        PPB = Buf()
        cst_bf = sb("cst_bf", [128, 128 * 5 + 256 + 2048 + 4 * 512 * 2], BF16)
        CB = Buf()
        cst_f = sb("cst_f", [128, 4], F32)
        ar_t = sb("arena", [128, 12200], F32)
        AR = Arena(ar_t[:])
        AR2 = Arena(xbuf_t[:])
        YT = [Buf() for _ in range(KC)]
        DBG = Buf()

        pbanks = [psum("pb%d" % i, [128, 512], F32) for i in range(7)]
        prot = Ring([pbanks[i][:] for i in range(4)])
        P4, P5, P6 = pbanks[4][:], pbanks[5][:], pbanks[6][:]
        PB4, PB5, PB6 = Buf(), Buf(), Buf()
        ptr_t = psum("ptr", [128, 1024], BF16)
        ptr = ptr_t[:]
        PTR = Buf()

        def getps():
            return prot.get()

        def mm(out, lhsT, rhs, start, stop, r, w):
            S.op("pe", lambda e: e.matmul(out, lhsT, rhs, start=start, stop=stop), r, w)

        def act(out, in_, func, r, w, bias=None, scale=None):
            kw = {}
            if bias is not None:
                kw["bias"] = bias
            if scale is not None:
                kw["scale"] = scale
            S.op("act", lambda e: e.activation(out=out, in_=in_, func=func, **kw), r, w)

        def tto(eng, out, in0, in1, op, r, w):
            S.op(eng, lambda e: e.tensor_tensor(out=out, in0=in0, in1=in1, op=op), r, w)

        def stt(eng, out, in0, scalar, in1, op0, op1, r, w):
            S.op(eng, lambda e: e.scalar_tensor_tensor(out=out, in0=in0, scalar=scalar, in1=in1, op0=op0, op1=op1), r, w)

        def tsc(eng, out, in0, s1, s2, op0, op1, r, w):
            S.op(eng, lambda e: e.tensor_scalar(out=out, in0=in0, scalar1=s1, scalar2=s2, op0=op0, op1=op1), r, w)

        def tsm(eng, out, in0, s1, r, w):
            S.op(eng, lambda e: e.tensor_scalar_mul(out=out, in0=in0, scalar1=s1), r, w)

        def cp(eng, out, in_, r, w):
            if eng == "act":
                S.op("act", lambda e: e.activation(out=out, in_=in_, func=AF.Copy), r, w)
            else:
                S.op(eng, lambda e: e.tensor_copy(out=out, in_=in_), r, w)

        def memset(eng, ap, val, w):
            S.op(eng, lambda e: e.memset(ap, val), (), w)

        def recip(out, in_, r, w):
            S.op("dve", lambda e: e.reciprocal(out=out, in_=in_), r, w)

        def dma(q, out, in_, r, w):
            S.dma(q, lambda e: e.dma_start(out=out, in_=in_), r, w)

        def asel(ap, pattern, cmp, base, cm, w):
            S.op("pool", lambda e: e.affine_select(out=ap, in_=ap, pattern=pattern, compare_op=cmp, fill=0.0, base=base, channel_multiplier=cm), w, w)

        o = 0
        ident_bf = cst_bf[:, o:o + 128]; o += 128
        ones_bf = cst_bf[:, o:o + 128]; o += 128
        maskG = cst_bf[:, o:o + 128]; o += 128
        TriS = cst_bf[:, o:o + 128]; o += 128
        NU = cst_bf[:, o:o + 128]; o += 128
        Eb = cst_bf[:, o:o + 256].rearrange("p (b m) -> p b m", b=16); o += 256
        Xb = cst_bf[:, o:o + 2048].rearrange("p (a s) -> p a s", a=16); o += 2048
        DM = []
        NM = []
        for r_ in range(4):
            DM.append(cst_bf[:, o:o + 512]); o += 512
        for r_ in range(4):
            NM.append(cst_bf[:, o:o + 512]); o += 512
        eps_t = cst_f[:, 0:1]
        one_t = cst_f[:, 1:2]
        memset("dve", cst_f[:, 0:1], EPS, [CB])
        memset("dve", cst_f[:, 1:2], 1.0, [CB])
        tmpc = AR.f32(2048)
        TB = Buf()

        def mk(dst, np_, nf, val, pattern, cmp, base, cm, post=None):
            t = tmpc[0:np_, 0:nf]
            memset("pool", t, val, [TB])
            if pattern is not None:
                tv = t if len(pattern) == 1 else t.rearrange("p (a b) -> p a b", a=pattern[0][1])
                asel(tv, pattern, cmp, base, cm, [TB])
            if post is not None:
                post(t)
            cp("dve", dst, t, [TB], [CB])

        mk(ident_bf, 128, 128, 1.0, [[-1, 128]], ALU.is_equal, 0, 1)
        mk(ones_bf, 128, 128, 1.0, None, None, 0, 0)
        mk(maskG, 128, 128, 1.0, [[1, 128]], ALU.is_ge, 0, -1, post=lambda t: memset("pool", t[0:64, 64:128], 0.0, [TB]))
        tsm("dve", TriS, maskG, -1.0 / 16.0, [CB], [CB])
        mk(NU, 128, 128, -1.0, [[-1, 128]], ALU.is_ge, 0, 1)
        mk(cst_bf[:, 640:896], 128, 256, 1.0, [[1, 16], [-1, 16]], ALU.is_equal, 0, 0)
        mk(cst_bf[0:16, 896:896 + 2048], 16, 2048, -1.0, [[-1, 16], [0, 128]], ALU.is_gt, 0, 1)
        for r_ in range(4):
            mk(DM[r_], 128, 512, 1.0, [[1, 512]], ALU.is_gt, -128 * r_, -1)
            tsc("dve", NM[r_], DM[r_], 30000.0, -30000.0, ALU.mult, ALU.add, [CB], [CB])
        for l in range(NL):
            dma("sp", ppt[:, l * NPP:(l + 1) * NPP], pp[l], [], [PPB])

        def par(l, col):
            return ppt[:, l * NPP + col:l * NPP + col + 1]

        class WQ:
            def __init__(self):
                self.plan = []
                self.issued = 0
                self.consumed = 0

            def extend(self, items):
                self.plan.extend(items)

            def get(self, D=2):
                while self.issued < min(len(self.plan), self.consumed + 1 + D):
                    src, n = self.plan[self.issued]
                    slot = self.issued % NW
                    dma("pool", wsl[slot][:, :, 0:n], src.rearrange("(k p) n -> p k n", p=128), [], [WB[slot]])
                    self.issued += 1
                slot = self.consumed % NW
                self.consumed += 1
                return slot

        wq = WQ()

        def proj(slot, c0, n, tt, src=None, RB=None):
            if src is None:
                src, RB = xn, XN
            ps, pb = getps()
            for k in range(KC):
                mm(ps[0:n, :], wsl[slot][:, k, c0:c0 + n], src[:, k, tsl(tt)], k == 0, k == KC - 1, [WB[slot], RB[k][tt]], [pb])
            return ps, pb

        def dump(idx, src, RBs, bf):
            if not dbg:
                return
            for k in range(KC):
                dma("pool" if bf else "sp", dbg_t[idx, k * 128:(k + 1) * 128, :], src[:, k, :], RBs[k], [DBG])

        def rmsnorm(l, gcol):
            AR.reset()
            sqr = Ring([AR.bf16(TW) for _ in range(3)])
            srr = Ring([AR.f32(TW) for _ in range(2)])
            for tt in range(TT):
                ps, pb = getps()
                for k in range(KC):
                    sq, sqb = sqr.get()
                    act(sq, xb[:, k, tsl(tt)], AF.Square, [XB[k][tt]], [sqb])
                    mm(ps, ones_bf, sq, k == 0, k == KC - 1, [sqb, CB], [pb])
                sr, srb = srr.get()
                act(sr, ps, AF.Sqrt, [pb, CB], [srb], bias=eps_t, scale=1.0 / 1024.0)
                recip(sr, sr, [srb], [srb])
                for k in range(KC):
                    stt("dve", xn[:, k, tsl(tt)], xb[:, k, tsl(tt)], par(l, gcol + k), sr, ALU.mult, ALU.mult,
                        [XB[k][tt], srb, PPB], [XN[k][tt]])

        def spill_x():
            for k in range(KC):
                dma("sp", yT[k * 128:(k + 1) * 128, :], xb[:, k, :], XB[k], [YT[k]])

        def reload_x():
            for k in range(KC):
                dma("sp", xb[:, k, :], yT[k * 128:(k + 1) * 128, :], [YT[k]], XB[k])

        def gla(l):
            AR.reset()
            AR2.reset()
            A = AR2
            adT = A.bf16(T); ADT = [Buf() for _ in range(TT)]
            wupa = A.bf16(512); WUPA = Buf()
            e1r = Ring([A.f32(TW) for _ in range(2)])
            Pr = Ring([A.bf16(TW) for _ in range(2)])
            ecr = Ring([A.f32(TW) for _ in range(2)])
            eir = Ring([A.f32(TW) for _ in range(2)])
            qd = A.bf16(T); QD = [Buf() for _ in range(TT)]
            ki = A.bf16(T); KI = [Buf() for _ in range(TT)]
            ki_tm = A.bf16(T); KITM = [Buf() for _ in range(TT)]
            v_tm = A.bf16(16 * 256); VTM = [Buf() for _ in range(8)]
            sg = [A.bf16(T), A.bf16(T)]; SG = [[Buf() for _ in range(TT)] for _ in range(2)]
            scr = Ring([A.bf16(128) for _ in range(3)])
            dec = A.f32(32); DEC = Buf()
            sqr = Ring([A.bf16(TW) for _ in range(2)])
            srr = Ring([A.f32(TW) for _ in range(2)])
            posb = [Ring([A.f32(TW)]), Ring([A.f32(TW)])]
            kvs = AR.f32(16 * 256); KVS = [Buf() for _ in range(16)]
            S_all = AR.f32(17 * 256); SAL = [Buf() for _ in range(17)]
            S_bfa = AR.bf16(16 * 256); SBA = [Buf() for _ in range(4)]
            for tt in range(TT):
                memset("dve", adT[0:33, tsl(tt)], 1.0, [ADT[tt]])
            memset("dve", wupa[0:33, :], 0.0, [WUPA])
            dma("pool", wupa[0:16, :], gla_w_up[l], [], [WUPA])
            dma("pool", wupa[32:33, :], gla_b_alpha[l:l + 1, :], [], [WUPA])
            plan = [(w_in[l][:, C_GD:C_GD + 16], 16)]
            for h in range(4):
                plan += [(w_in[l][:, C_GQ + h * 128:C_GQ + (h + 1) * 128], 128),
                         (w_in[l][:, C_GK + h * 128:C_GK + (h + 1) * 128], 128),
                         (w_in[l][:, C_GV + h * 256:C_GV + (h + 1) * 256], 256),
                         (w_in[l][:, C_GG + h * 256:C_GG + (h + 1) * 256], 256)]
            wq.extend(plan)
            s_ = wq.get(D=1)
            for tt in range(TT):
                ps, pb = proj(s_, 0, 16, tt)
                cp("act", adT[0:16, tsl(tt)], ps[0:16, :], [pb], [ADT[tt]])
            for h in range(4):
                s_q = wq.get(D=2)
                s_k = wq.get(D=2)
                for tt in range(TT):
                    psy, pyb = getps()
                    for mi in range(4):
                        m = tt * 4 + mi
                        mm(psy[:, mi * 128:(mi + 1) * 128], adT[0:33, m * 128:(m + 1) * 128], wupa[0:33, h * 128:(h + 1) * 128],
                           True, True, [ADT[tt], WUPA], [pyb])
                    e1, e1b = e1r.get()
                    act(e1, psy, AF.Exp, [pyb], [e1b], scale=-1.0)
                    Pt, Ptb = Pr.get()
                    act(Pt, e1, AF.Ln, [e1b, CB], [Ptb], bias=one_t)
                    psc, pcb = getps()
                    for mi in range(4):
                        mm(psc[:, mi * 128:(mi + 1) * 128], Pt[:, mi * 128:(mi + 1) * 128], TriS, True, True, [Ptb, CB], [pcb])
                    ec, ecb = ecr.get()
                    ei, eib = eir.get()
                    act(ec, psc, AF.Exp, [pcb], [ecb])
                    act(ei, psc, AF.Exp, [pcb], [eib], scale=-1.0)
                    cp("dve", dec[:, tt * 8:(tt + 1) * 8], ec[:, 63::64], [ecb], [DEC])
                    psq, pqb = proj(s_q, 0, 128, tt)
                    stt("dve", qd[:, tsl(tt)], psq, 128.0 ** -0.5, ec, ALU.mult, ALU.mult, [pqb, ecb], [QD[tt]])
                    psk, pkb = proj(s_k, 0, 128, tt)
                    tto("dve", ki[:, tsl(tt)], psk, ei, ALU.mult, [pkb, eib], [KI[tt]])
                    for mi in range(4):
                        m = tt * 4 + mi
                        S.op("pe", (lambda o_, i_: (lambda e: e.transpose(o_, i_, ident_bf)))(ptr[:, mi * 128:(mi + 1) * 128], ki[:, m * 128:(m + 1) * 128]),
                             [KI[tt], CB], [PTR])
                    cp("act", ki_tm[:, tsl(tt)], ptr[:, 0:512], [PTR], [KITM[tt]])
                s_v = wq.get(D=2)
                for m2 in range(8):
                    ps, pb = getps()
                    for mj in range(2):
                        m = m2 * 2 + mj
                        for k in range(KC):
                            mm(ps[:, mj * 256:(mj + 1) * 256], xn[:, k, m * 128:(m + 1) * 128], wsl[s_v][:, k, 0:256],
                               k == 0, k == KC - 1, [XN[k][m // 4], WB[s_v]], [pb])
                    cp("act", v_tm[:, m2 * 512:(m2 + 1) * 512], ps, [pb], [VTM[m2]])
                s_g = wq.get(D=2)
                for jb in range(2):
                    for tt in range(TT):
                        ps, pb = proj(s_g, jb * 128, 128, tt)
                        act(sg[jb][:, tsl(tt)], ps, AF.Silu, [pb], [SG[jb][tt]])
                for hs in range(2):
                    for ci in range(16):
                        c = hs * 16 + ci
                        m = c // 2
                        half = c % 2
                        pkv, pkb = getps()
                        mm(pkv[:, 0:256], ki_tm[half * 64:(half + 1) * 64, m * 128:(m + 1) * 128],
                           v_tm[half * 64:(half + 1) * 64, m * 256:(m + 1) * 256], True, True, [KITM[m // 4], VTM[m // 2]], [pkb])
                        act(kvs[:, ci * 256:(ci + 1) * 256], pkv[:, 0:256], AF.Identity, [pkb, DEC], [KVS[ci]], scale=dec[:, c:c + 1])
                    if hs == 0:
                        memset("dve", S_all[:, 0:256], 0.0, [SAL[0]])
                    else:
                        cp("dve", S_all[:, 0:256], S_all[:, 16 * 256:17 * 256], [SAL[16]], [SAL[0]])
                    for ci in range(16):
                        c = hs * 16 + ci
                        stt("dve", S_all[:, (ci + 1) * 256:(ci + 2) * 256], S_all[:, ci * 256:(ci + 1) * 256], dec[:, c:c + 1],
                            kvs[:, ci * 256:(ci + 1) * 256], ALU.mult, ALU.add, [SAL[ci], DEC, KVS[ci]], [SAL[ci + 1]])
                        if ci % 4 == 3:
                            g4 = ci // 4
                            cp("act", S_bfa[:, g4 * 1024:(g4 + 1) * 1024], S_all[:, g4 * 1024:(g4 + 1) * 1024],
                               [SAL[g4 * 4 + j] for j in range(4)], [SBA[g4]])
                    for tl in range(2):
                        tt = hs * 2 + tl
                        po = [P4, P5]
                        POB = [PB4, PB5]
                        scl = {}
                        for it in range(5):
                            if it < 4:
                                mi = it
                                m = tt * 4 + mi
                                pss, psb = getps()
                                mm(pss[:, 0:128], ki[:, m * 128:(m + 1) * 128], qd[:, m * 128:(m + 1) * 128], True, True, [KI[tt], QD[tt]], [psb])
                                sc, scb = scr.get()
                                tto("dve", sc, pss[:, 0:128], maskG, ALU.mult, [psb, CB], [scb])
                                scl[mi] = (sc, scb)
                            if it >= 1:
                                mi = it - 1
                                m = tt * 4 + mi
                                sc, scb = scl.pop(mi)
                                for half in range(2):
                                    c = 2 * m + half
                                    ci = c - hs * 16
                                    for jb in range(2):
                                        col = mi * 128 + half * 64
                                        mm(po[jb][:, col:col + 64], S_bfa[:, ci * 256 + jb * 128:ci * 256 + (jb + 1) * 128], qd[:, c * 64:(c + 1) * 64],
                                           True, False, [SBA[ci // 4], QD[tt]], [POB[jb]])
                                        mm(po[jb][:, col:col + 64], v_tm[:, m * 256 + jb * 128:m * 256 + (jb + 1) * 128], sc[:, half * 64:(half + 1) * 64],
                                           False, True, [VTM[m // 2], scb], [POB[jb]])
                        pcs = []
                        for jb in range(2):
                            pc, pcb_ = posb[jb].get()
                            cp("act", pc, po[jb], [POB[jb]], [pcb_])
                            pcs.append((pc, pcb_))
                        pn, pnb = getps()
                        for jb in range(2):
                            sq, sqb = sqr.get()
                            act(sq, pcs[jb][0], AF.Square, [pcs[jb][1]], [sqb])
                            mm(pn, ones_bf, sq, jb == 0, jb == 1, [sqb, CB], [pnb])
                        sr, srb = srr.get()
                        act(sr, pn, AF.Sqrt, [pnb, CB], [srb], bias=eps_t, scale=1.0 / 256.0)
                        recip(sr, sr, [srb], [srb])
                        for jb in range(2):
                            pc, pcb_ = pcs[jb]
                            tto("dve", pc, pc, sr, ALU.mult, [pcb_, srb], [pcb_])
                            stt("dve", ob[:, h * 2 + jb, tsl(tt)], pc, par(l, P_GLAN + jb), sg[jb][:, tsl(tt)], ALU.mult, ALU.mult,
                                [pcb_, PPB, SG[jb][tt]], [OB[h * 2 + jb][tt]])

        def branch(l, b):
            AR.reset()
            gr = Ring([AR.f32(TW) for _ in range(2)])
            tr = Ring([AR.f32(TW) for _ in range(2)])
            plan = []
            for nbp in range(4):
                plan += [(w_br[b][l][:, nbp * 256:(nbp + 1) * 256], 256),
                         (w_in[l][:, C_GATE + b * 1024 + nbp * 256:C_GATE + b * 1024 + (nbp + 1) * 256], 256)]
            wq.extend(plan)
            for nbp in range(4):
                s1 = wq.get(D=2)
                s2 = wq.get(D=2)
                for nbi in range(2):
                    nb = nbp * 2 + nbi
                    for tt in range(TT):
                        ps1, p1b = proj(s1, nbi * 128, 128, tt, ob, OB)
                        ps2, p2b = proj(s2, nbi * 128, 128, tt)
                        g, gb = gr.get()
                        act(g, ps2, AF.Sigmoid, [p2b, PPB], [gb], bias=par(l, P_BG + b * 8 + nb))
                        if b == 0:
                            tto("dve", xb[:, nb, tsl(tt)], ps1, g, ALU.mult, [p1b, gb], [XB[nb][tt]])
                        else:
                            t_, tb = tr.get()
                            tto("dve", t_, ps1, g, ALU.mult, [p1b, gb], [tb])
                            tto("dve", xb[:, nb, tsl(tt)], xb[:, nb, tsl(tt)], t_, ALU.add, [tb, XB[nb][tt]], [XB[nb][tt]])

        def lru(l):
            AR.reset()
            A = AR
            cl = A.f32(8); CL = Buf()
            tmp8 = A.f32(8)
            lxbf = [A.bf16(T + 4), A.bf16(T + 4)]; LXB = [[Buf() for _ in range(TT)] for _ in range(2)]
            dg = [[A.bf16(128) for _ in range(4)] for _ in range(2)]; DG = Buf()
            wab = A.bf16(512).rearrange("p (i j) -> p i j", i=2); wxb = A.bf16(512).rearrange("p (i j) -> p i j", i=2); WAB = Buf(); WXB = Buf()
            xcf = [Ring([A.f32(TW) for _ in range(2)]) for _ in range(2)]
            xcb = [Ring([A.bf16(TW) for _ in range(1)]) for _ in range(2)]
            rr = Ring([A.f32(TW) for _ in range(2)])
            igr = Ring([A.f32(TW) for _ in range(2)])
            n2r = Ring([A.f32(TW) for _ in range(1)])
            hr = [Ring([A.f32(TW) for _ in range(2)]) for _ in range(2)]
            glr = Ring([A.f32(TW) for _ in range(2)])
            g2r = Ring([A.f32(TW) for _ in range(1)])
            act(tmp8, ppt[:, l * NPP + P_LAM:l * NPP + P_LAM + 8], AF.Exp, [PPB], [CL], scale=-1.0)
            act(tmp8, tmp8, AF.Ln, [CL, CB], [CL], bias=one_t)
            tsm("dve", cl, tmp8, -8.0, [CL], [CL])
            plan = []
            for hb in range(4):
                plan += [(w_in[l][:, C_LX + hb * 256:C_LX + (hb + 1) * 256], 256), (w_in[l][:, C_LG + hb * 256:C_LG + (hb + 1) * 256], 256)]
            wq.extend(plan)
            for hb in range(4):
                slx = wq.get(D=2)
                slg = wq.get(D=2)
                dma("pool", wab, lru_w_a[l, hb].rearrange("(i p) j -> p i j", p=128), [], [WAB])
                dma("pool", wxb, lru_w_x[l, hb].rearrange("(i p) j -> p i j", p=128), [], [WXB])
                for cbl in range(2):
                    memset("dve", lxbf[cbl][:, 0:3], 0.0, [LXB[cbl][0]])
                    for w_ in range(4):
                        tsm("dve", dg[cbl][w_], ident_bf, par(l, P_CW + (hb * 2 + cbl) * 4 + w_), [CB, PPB], [DG])
                hprev = [None, None]
                for tt in range(TT):
                    for cbl in range(2):
                        ps, pb = proj(slx, cbl * 128, 128, tt)
                        cp("act", lxbf[cbl][:, 3 + tt * TW:3 + (tt + 1) * TW], ps, [pb], [LXB[cbl][tt]])
                    xf = []
                    xh = []
                    for cbl in range(2):
                        ps, pb = getps()
                        rd = [LXB[cbl][tt], DG] + ([LXB[cbl][tt - 1]] if tt > 0 else [])
                        for w_ in range(4):
                            mm(ps, dg[cbl][w_], lxbf[cbl][:, tt * TW + w_:tt * TW + w_ + TW], w_ == 0, w_ == 3, rd, [pb])
                        f_, fb = xcf[cbl].get()
                        act(f_, ps, AF.Identity, [pb, PPB], [fb], bias=par(l, P_CB + hb * 2 + cbl))
                        h_, hb_ = xcb[cbl].get()
                        cp("dve", h_, f_, [fb], [hb_])
                        xf.append((f_, fb))
                        xh.append((h_, hb_))
                    for jb in range(2):
                        c = hb * 2 + jb
                        psr, prb = getps()
                        for ib in range(2):
                            mm(psr, wab[:, ib, jb * 128:(jb + 1) * 128], xh[ib][0], ib == 0, ib == 1, [WAB, xh[ib][1]], [prb])
                        psi, pib = getps()
                        for ib in range(2):
                            mm(psi, wxb[:, ib, jb * 128:(jb + 1) * 128], xh[ib][0], ib == 0, ib == 1, [WXB, xh[ib][1]], [pib])
                        r_, rb = rr.get()
                        act(r_, psr, AF.Sigmoid, [prb, PPB], [rb], bias=par(l, P_BA + c))
                        a_, ab = r_, rb
                        act(a_, r_, AF.Exp, [rb, CL], [ab], scale=cl[:, c:c + 1])
                        ig, igb = igr.get()
                        act(ig, psi, AF.Sigmoid, [pib, PPB], [igb], bias=par(l, P_BX + c))
                        n2, n2b = n2r.get()
                        stt("dve", n2, a_, -1.0, a_, ALU.mult, ALU.mult, [ab], [n2b])
                        act(n2, n2, AF.Sqrt, [n2b, CB], [n2b], bias=one_t)
                        u_, ub = ig, igb
                        tto("dve", u_, n2, ig, ALU.mult, [n2b, igb], [ub])
                        tto("dve", u_, u_, xf[jb][0], ALU.mult, [ub, xf[jb][1]], [ub])
                        hh, hhb = hr[jb].get()
                        if hprev[jb] is None:
                            S.op("dve", (lambda o_, a0, u0: (lambda e: e.tensor_tensor_scan(out=o_, data0=a0, data1=u0, initial=0.0, op0=ALU.mult, op1=ALU.add)))(hh, a_, u_),
                                 [ab, ub], [hhb])
                        else:
                            hp, hpb = hprev[jb]
                            S.op("dve", (lambda o_, a0, u0, i0: (lambda e: e.tensor_tensor_scan(out=o_, data0=a0, data1=u0, initial=i0, op0=ALU.mult, op1=ALU.add)))(hh, a_, u_, hp[:, TW - 1:TW]),
                                 [ab, ub, hpb], [hhb])
                        hprev[jb] = (hh, hhb)
                        psg, pgb = proj(slg, jb * 128, 128, tt)
                        gl, glb = glr.get()
                        g2, g2b = g2r.get()
                        act(gl, psg, AF.Identity, [pgb], [glb])
                        tto("dve", g2, gl, gl, ALU.mult, [glb], [g2b])
                        tsc("dve", g2, g2, 0.044715, 1.0, ALU.mult, ALU.add, [g2b], [g2b])
                        tto("dve", g2, g2, gl, ALU.mult, [g2b, glb], [g2b])
                        act(g2, g2, AF.Sigmoid, [g2b], [g2b], scale=1.5957691216057308)
                        tto("dve", gl, gl, hh, ALU.mult, [glb, hhb], [glb])
                        tto("dve", ob[:, c, tsl(tt)], gl, g2, ALU.mult, [glb, g2b], [OB[c][tt]])

        def sbattn(l):
            AR.reset()
            A = AR
            Lk = A.bf16(16 * TW); LK = [Buf() for _ in range(16)]
            qn = A.bf16(T); QN = [Buf() for _ in range(TT)]
            kn = A.bf16(T); KN = [Buf() for _ in range(TT)]
            v_tm = A.bf16(T); VT = [Buf() for _ in range(4)]
            Er = Ring([A.f32(TW) for _ in range(2)])
            wr = Ring([A.bf16(TW) for _ in range(3)])
            sqr = Ring([A.bf16(TW) for _ in range(2)])
            srr = Ring([A.f32(TW) for _ in range(2)])
            csr = Ring([A.bf16(TW) for _ in range(2)])
            gq2 = A.f32(2); GQ = Buf()
            tsm("dve", gq2[:, 0:1], par(l, P_SQG), 128.0 ** -0.5, [PPB], [GQ])
            cp("dve", gq2[:, 1:2], par(l, P_SKG), [PPB], [GQ])
            plan = []
            for h in range(8):
                plan += [(w_in[l][:, C_SQ + h * 128:C_SQ + (h + 1) * 128], 128), (w_in[l][:, C_SK + h * 128:C_SK + (h + 1) * 128], 128),
                         (w_in[l][:, C_SV + h * 128:C_SV + (h + 1) * 128], 128)]
            wq.extend(plan)
            for h in range(8):
                slots_qk = [wq.get(D=2), wq.get(D=2)]
                items = [(which, tt) for which in range(2) for tt in range(TT)]
                stA = {}

                def stageA(i):
                    which, tt = items[i]
                    ps, pb = proj(slots_qk[which], 0, 128, tt)
                    sq, sqb = sqr.get()
                    act(sq, ps, AF.Square, [pb], [sqb])
                    stA[i] = (ps, pb, sq, sqb)

                def stageB(i):
                    which, tt = items[i]
                    dst, DBs = ((qn, QN), (kn, KN))[which]
                    ps, pb, sq, sqb = stA.pop(i)
                    pn, pnb = getps()
                    mm(pn, ones_bf, sq, True, True, [sqb, CB], [pnb])
                    sr, srb = srr.get()
                    act(sr, pn, AF.Sqrt, [pnb, CB], [srb], bias=eps_t, scale=1.0 / 128.0)
                    recip(sr, sr, [srb], [srb])
                    stt("dve", dst[:, tsl(tt)], ps, gq2[:, which:which + 1], sr, ALU.mult, ALU.mult, [pb, srb, GQ], [DBs[tt]])

                for i in range(len(items) + 1):
                    if i < len(items):
                        stageA(i)
                    if i >= 1:
                        stageB(i - 1)
                s_v = wq.get(D=2)
                for m4 in range(4):
                    ps, pb = getps()
                    for mi in range(4):
                        m = m4 * 4 + mi
                        for k in range(KC):
                            mm(ps[:, mi * 128:(mi + 1) * 128], xn[:, k, m * 128:(m + 1) * 128], wsl[s_v][:, k, 0:128],
                               k == 0, k == KC - 1, [XN[k][m4], WB[s_v]], [pb])
                    cp("act", v_tm[:, m4 * 512:(m4 + 1) * 512], ps, [pb], [VT[m4]])
                LAG = 2
                csl = {}
                wl = {}

                def p1(qt, a):
                    psz, pzb = getps()
                    mm(psz, kn[:, a * 128:(a + 1) * 128], qn[:, tsl(qt)], True, True, [KN[a // 4], QN[qt]], [pzb])
                    E, Eb_ = Er.get()
                    act(E, psz, AF.Exp, [pzb], [Eb_])
                    La = Lk[:, a * TW:(a + 1) * TW]
                    act(La, E, AF.Ln, [Eb_, CB], [LK[a]], bias=one_t)
                    if a >= 4 * qt:
                        tto("dve", La, La, DM[a - 4 * qt], ALU.mult, [LK[a], CB], [LK[a]])

                def p1cs(qt, a):
                    nk = 4 * (qt + 1)
                    La = Lk[:, a * TW:(a + 1) * TW]
                    mm(P4[0:16, :], Eb[:, a, :], La, a == 0, a == nk - 1, [LK[a], CB], [PB4])

                def p1fin(qt):
                    cs, csb = csr.get()
                    cp("dve", cs[0:16, :], P4[0:16, :], [PB4], [csb])
                    csl[qt] = (cs, csb)

                def p2(qt, a):
                    cs, csb = csl[qt]
                    diag = a >= 4 * qt
                    psw, pwb = getps()
                    La = Lk[:, a * TW:(a + 1) * TW]
                    mm(psw, kn[:, a * 128:(a + 1) * 128], qn[:, tsl(qt)], True, False, [KN[a // 4], QN[qt]], [pwb])
                    mm(psw, NU, La, False, False, [CB, LK[a]], [pwb])
                    mm(psw, Xb[0:16, a, :], cs[0:16, :], False, not diag, [CB, csb], [pwb])
                    if diag:
                        mm(psw, ident_bf, NM[a - 4 * qt], False, True, [CB], [pwb])
                    w_, wb_ = wr.get()
                    act(w_, psw, AF.Exp, [pwb], [wb_])
                    wl[(qt, a)] = (w_, wb_)

                def p2pv(qt, a):
                    nk = 4 * (qt + 1)
                    po, pob = (P5, PB5) if qt % 2 == 0 else (P6, PB6)
                    w_, wb_ = wl.pop((qt, a))
                    mm(po, v_tm[:, a * 128:(a + 1) * 128], w_, a == 0, a == nk - 1, [VT[a // 4], wb_], [pob])

                def p2fin(qt):
                    po, pob = (P5, PB5) if qt % 2 == 0 else (P6, PB6)
                    cp("dve", ob[:, h, tsl(qt)], po, [pob], [OB[h][qt]])

                for it in range(4 + LAG):
                    if it < 4:
                        p1(0, it)
                    if it >= LAG:
                        p1cs(0, it - LAG)
                p1fin(0)
                for qt in range(TT):
                    nk = 4 * (qt + 1)
                    nxt = qt + 1 < TT
                    nk2 = nk + 4 if nxt else 0
                    for it in range(max(nk, nk2) + LAG):
                        if it < nk:
                            p2(qt, it)
                        if nxt and it < nk2:
                            p1(qt + 1, it)
                        if LAG <= it < nk + LAG:
                            p2pv(qt, it - LAG)
                        if nxt and LAG <= it < nk2 + LAG:
                            p1cs(qt + 1, it - LAG)
                    if nxt:
                        p1fin(qt + 1)
                    p2fin(qt)

        def wout(l):
            AR.reset()
            for nb in range(KC):
                for tt in range(TT):
                    cp("act" if (nb + tt) % 2 == 0 else "dve", ob[:, nb, tsl(tt)], xb[:, nb, tsl(tt)], [XB[nb][tt]], [OB[nb][tt]])
            dump(4, ob, OB, True)
            reload_x()
            wq.extend([(w_out[l][:, i * 256:(i + 1) * 256], 256) for i in range(4)])
            for i in range(4):
                s_ = wq.get(D=2)
                for nbi in range(2):
                    nb = i * 2 + nbi
                    for tt in range(TT):
                        ps, pb = proj(s_, nbi * 128, 128, tt, ob, OB)
                        tto("dve", xb[:, nb, tsl(tt)], ps, xb[:, nb, tsl(tt)], ALU.add, [pb, XB[nb][tt]], [XB[nb][tt]])

        def mlp(l):
            rmsnorm(l, P_GMLP)
            rr = Ring([AR.f32(TW) for _ in range(3)])
            plan = []
            for fg in range(4):
                plan += [(w_up[l][:, fg * 1024 + i * 256:fg * 1024 + (i + 1) * 256], 256) for i in range(4)]
                plan += [(w_dn[l][fg * 1024:(fg + 1) * 1024, i * 256:(i + 1) * 256], 256) for i in range(4)]
            wq.extend(plan)
            for fg in range(4):
                for i in range(4):
                    s_ = wq.get(D=3)
                    for fbi in range(2):
                        fb = i * 2 + fbi
                        for tt in range(TT):
                            ps, pb = proj(s_, fbi * 128, 128, tt)
                            r_, rb = rr.get()
                            act(r_, ps, AF.Relu, [pb], [rb])
                            tto("dve", ob[:, fb, tsl(tt)], r_, r_, ALU.mult, [rb], [OB[fb][tt]])
                for i in range(4):
                    s_ = wq.get(D=3)
                    for nbi in range(2):
                        nb = i * 2 + nbi
                        for tt in range(TT):
                            ps, pb = proj(s_, nbi * 128, 128, tt, ob, OB)
                            tto("dve", xb[:, nb, tsl(tt)], ps, xb[:, nb, tsl(tt)], ALU.add, [pb, XB[nb][tt]], [XB[nb][tt]])

        S.barrier()
        for k in range(KC):
            dma("sp", xb[:, k, :], xT[k * 128:(k + 1) * 128, :], [], XB[k])
        for l in range(NL):
            rmsnorm(l, P_GMIX)
            if l == 0:
                dump(0, xn, XN, True)
            spill_x()
            S.barrier()
            gla(l)
            if l == 0:
                dump(1, ob, OB, True)
            S.barrier()
            branch(l, 0)
            S.barrier()
            lru(l)
            if l == 0:
                dump(2, ob, OB, True)
            S.barrier()
            branch(l, 1)
            S.barrier()
            sbattn(l)
            if l == 0:
                dump(3, ob, OB, True)
            S.barrier()
            branch(l, 2)
            S.barrier()
            wout(l)
            if l == 0:
                dump(5, xb, XB, False)
            S.barrier()
            mlp(l)
            S.barrier()
        for k in range(KC):
            dma("sp", yT[k * 128:(k + 1) * 128, :], xb[:, k, :], XB[k], [YT[k]])
        S.barrier()
        S.emit(st)
        print("instructions:", S.ninst, {e: len(S.streams[e]) for e in ENGS})
    return nc


def pack_params(inp, layers):
    def fm(v, nb):
        return np.ascontiguousarray(np.asarray(v, np.float32).reshape(nb, 128).T)

    out = np.zeros((len(layers), 128, NPP), np.float32)
    for i, l in enumerate(layers):
        out[i, :, P_GMIX:P_GMIX + 8] = fm(inp["norm_mix_g"][l], 8)
        out[i, :, P_GMLP:P_GMLP + 8] = fm(inp["norm_mlp_g"][l], 8)
        out[i, :, P_GLAN:P_GLAN + 2] = fm(inp["gla_norm_g"][l], 2)
        cw = np.asarray(inp["lru_conv_w"][l], np.float32)
        for cb in range(8):
            for w_ in range(4):
                out[i, :, P_CW + cb * 4 + w_] = cw[w_, cb * 128:(cb + 1) * 128]
        out[i, :, P_CB:P_CB + 8] = fm(inp["lru_conv_b"][l], 8)
        out[i, :, P_BA:P_BA + 8] = fm(inp["lru_b_a"][l], 8)
        out[i, :, P_BX:P_BX + 8] = fm(inp["lru_b_x"][l], 8)
        out[i, :, P_LAM:P_LAM + 8] = fm(inp["lru_lambda"][l], 8)
        out[i, :, P_SQG] = np.asarray(inp["sb_q_norm_g"][l], np.float32)
        out[i, :, P_SKG] = np.asarray(inp["sb_k_norm_g"][l], np.float32)
        out[i, :, P_BG:P_BG + 24] = fm(inp["b_gate"][l], 24)
    return out


_NC_CACHE = {}
WNAMES = ["w_in", "gla_w_up", "gla_b_alpha", "lru_w_a", "lru_w_x", "w_branch_a", "w_branch_b", "w_branch_c", "w_out", "w_mlp_up", "w_mlp_down"]


def _get_nc(NL, dbg=False):
    key = (NL, dbg)
    if key not in _NC_CACHE:
        _NC_CACHE[key] = build(NL, dbg)
    return _NC_CACHE[key]


def run_layers(inp, xT_list, layers, dbg=False, cores=8):
    nc = _get_nc(len(layers), dbg)
    ppk = pack_params(inp, layers)
    shared = {"pp": ppk}
    for n in WNAMES:
        shared[n] = np.ascontiguousarray(np.asarray(inp[n], np.float32)[layers[0]:layers[-1] + 1])
    in_maps = []
    for c in range(cores):
        d = dict(shared)
        d["xT"] = xT_list[c]
        in_maps.append(d)
    res = run_bass_kernel_spmd(nc, in_maps, core_ids=list(range(cores)))
    return res.results


def kernel(**inputs):
    x = np.asarray(inputs["x"], np.float32)
    B = x.shape[0]
    xT = [np.ascontiguousarray(x[b].T) for b in range(B)]
    if FUSED:
        res = run_layers(inputs, xT, list(range(DEPTH)))
        xT = [r["yT"] for r in res]
    else:
        for l in range(DEPTH):
            res = run_layers(inputs, xT, [l])
            xT = [np.ascontiguousarray(r["yT"]) for r in res]
    return np.stack([np.asarray(t).T for t in xT], axis=0).astype(np.float32)
```

```python
from contextlib import ExitStack
import numpy as np
import concourse.bass as bass
import concourse.mybir as mybir
from concourse.bass_utils import run_bass_kernel_spmd

F32 = mybir.dt.float32
BF16 = mybir.dt.bfloat16
AF = mybir.ActivationFunctionType
ALU = mybir.AluOpType

FUSED = True
DEPTH = 4
T = 2048
TT = 4
TW = 512
KC = 8
NW = 4
EPS = 1e-6
C_GQ, C_GK, C_GV, C_GG, C_GD, C_LX, C_LG, C_SQ, C_SK, C_SV, C_GATE = 0, 512, 1024, 2048, 3072, 3088, 4112, 5136, 6160, 7184, 8208
IN_COLS = 11280
P_GMIX, P_GMLP, P_GLAN, P_CW, P_CB, P_BA, P_BX, P_LAM, P_SQG, P_SKG, P_BG, NPP = 0, 8, 16, 18, 50, 58, 66, 74, 82, 83, 84, 108


class Buf:
    __slots__ = ("w", "r")

    def __init__(self):
        self.w = None
        self.r = {}


ENGS = ("pe", "act", "dve", "pool", "sp")
EPOCH = 20000
DMA_EPOCH = 1000


class Sched:
    def __init__(self, nc, n_dma_ch=8):
        self.nc = nc
        self.streams = {e: [] for e in ENGS}
        self.cnt = {e: 0 for e in ENGS}
        self.epoch = {e: 0 for e in ENGS}
        self.waited = {e: {} for e in ENGS}
        self.n_dma_ch = n_dma_ch
        self.dcnt = [0] * n_dma_ch
        self.depoch = [0] * n_dma_ch
        self.dnext = 0
        self.semkeys = []
        self._seen = set()
        self.latest = {}
        self.ninst = 0

    def _key(self, k):
        if k not in self._seen:
            self._seen.add(k)
            self.semkeys.append(k)
        return k

    def _filter(self, eng, need):
        out = []
        wd = self.waited[eng]
        for s, v in need.items():
            if eng == "pe" and s[0] == "pe":
                continue
            if wd.get(s, 0) < v:
                wd[s] = v
                out.append((s, v))
        return out

    def _deps(self, eng, reads, writes):
        need = {}
        for b in reads:
            if b.w is not None:
                s, v = b.w
                if need.get(s, 0) < v:
                    need[s] = v
        for b in writes:
            if b.w is not None:
                s, v = b.w
                if need.get(s, 0) < v:
                    need[s] = v
            for s, v in b.r.items():
                if need.get(s, 0) < v:
                    need[s] = v
        return self._filter(eng, need)

    def _mark(self, ev, reads, writes):
        s, v = ev
        self.latest[s] = v
        for b in reads:
            if b.r.get(s, 0) < v:
                b.r[s] = v
        for b in writes:
            b.w = ev
            b.r = {}

    def op(self, eng, fn, reads=(), writes=()):
        deps = self._deps(eng, reads, writes)
        if self.cnt[eng] >= EPOCH:
            self.epoch[eng] += 1
            self.cnt[eng] = 0
        self.cnt[eng] += 1
        key = self._key((eng, self.epoch[eng]))
        self.streams[eng].append((deps, fn, key, 1))
        self._mark((key, self.cnt[eng]), reads, writes)
        self.ninst += 1

    def dma(self, queue, fn, reads=(), writes=()):
        deps = self._deps(queue, reads, writes)
        ch = self.dnext
        self.dnext = (self.dnext + 1) % self.n_dma_ch
        if self.dcnt[ch] >= DMA_EPOCH:
            self.depoch[ch] += 1
            self.dcnt[ch] = 0
        self.dcnt[ch] += 1
        key = self._key(("dma%d" % ch, self.depoch[ch]))
        self.streams[queue].append((deps, fn, key, 16))
        self._mark((key, 16 * self.dcnt[ch]), reads, writes)
        self.ninst += 1

    def barrier(self):
        for eng in ENGS:
            deps = self._filter(eng, dict(self.latest))
            if deps:
                self.streams[eng].append((deps, None, None, 0))

    def emit(self, stack):
        nc = self.nc
        sems = {}
        for k in self.semkeys:
            sems[k] = stack.enter_context(nc.semaphore("s_%s_%d" % k))
        block = stack.enter_context(nc.Block())
        streams = self.streams

        def run(engobj, lst):
            for deps, fn, key, inc in lst:
                for s, v in deps:
                    engobj.wait_ge(sems[s], v)
                if fn is not None:
                    fn(engobj).then_inc(sems[key], inc)

        @block.tensor
        def _(e):
            run(e, streams["pe"])

        @block.scalar
        def _(e):
            run(e, streams["act"])

        @block.vector
        def _(e):
            run(e, streams["dve"])

        @block.gpsimd
        def _(e):
            run(e, streams["pool"])

        @block.sync
        def _(e):
            run(e, streams["sp"])


class Arena:
    def __init__(self, ap):
        self.ap = ap
        self.n = ap.shape[1]
        self.off = 0

    def f32(self, n):
        a = self.ap[:, self.off:self.off + n]
        self.off += n
        assert self.off <= self.n, ("arena overflow", self.off, self.n)
        return a

    def bf16(self, n):
        nn = (n + 1) // 2
        a = self.ap[:, self.off:self.off + nn].bitcast(BF16)
        self.off += nn
        assert self.off <= self.n, ("arena overflow", self.off, self.n)
        return a

    def reset(self):
        self.off = 0


class Ring:
    def __init__(self, aps):
        self.items = [(a, Buf()) for a in aps]
        self.i = 0

    def get(self):
        it = self.items[self.i]
        self.i = (self.i + 1) % len(self.items)
        return it


def tsl(tt):
    return slice(tt * TW, (tt + 1) * TW)


def build(NL, dbg=False):
    nc = bass.Bass("TRN2", target_bir_lowering=False)

    def din(name, shape):
        return nc.dram_tensor(name, shape, F32, kind="ExternalInput").ap()

    xT = din("xT", [1024, T])
    pp = din("pp", [NL, 128, NPP])
    w_in = din("w_in", [NL, 1024, IN_COLS])
    gla_w_up = din("gla_w_up", [NL, 16, 512])
    gla_b_alpha = din("gla_b_alpha", [NL, 512])
    lru_w_a = din("lru_w_a", [NL, 4, 256, 256])
    lru_w_x = din("lru_w_x", [NL, 4, 256, 256])
    w_br = [din("w_branch_a", [NL, 1024, 1024]), din("w_branch_b", [NL, 1024, 1024]), din("w_branch_c", [NL, 1024, 1024])]
    w_out = din("w_out", [NL, 1024, 1024])
    w_up = din("w_mlp_up", [NL, 1024, 4096])
    w_dn = din("w_mlp_down", [NL, 4096, 1024])
    yT = nc.dram_tensor("yT", [1024, T], F32, kind="ExternalOutput").ap()
    dbg_t = None
    if dbg:
        dbg_t = nc.dram_tensor("dbg", [6, 1024, T], F32, kind="ExternalOutput").ap()

    with ExitStack() as st:
        def sb(name, shape, dt):
            return st.enter_context(nc.sbuf_tensor(name, shape, dt))

        def psum(name, shape, dt):
            return st.enter_context(nc.psum_tensor(name, shape, dt))

        S = Sched(nc)
        xbuf_t = sb("xbuf", [128, KC * T], F32)
        xb = xbuf_t[:].rearrange("p (k t) -> p k t", k=KC)
        XB = [[Buf() for _ in range(TT)] for _ in range(KC)]
        xn_t = sb("xn", [128, KC * T], BF16)
        xn = xn_t[:].rearrange("p (k t) -> p k t", k=KC)
        XN = [[Buf() for _ in range(TT)] for _ in range(KC)]
        ob_t = sb("ob", [128, KC * T], BF16)
        ob = ob_t[:].rearrange("p (k t) -> p k t", k=KC)
        OB = [[Buf() for _ in range(TT)] for _ in range(KC)]
        wsl_t = sb("wsl", [128, NW * KC * 256], BF16)
        wsl = [wsl_t[:, i * KC * 256:(i + 1) * KC * 256].rearrange("p (k n) -> p k n", k=KC) for i in range(NW)]
        WB = [Buf() for _ in range(NW)]
        ppt = sb("ppt", [128, NL * NPP], F32)
        PPB = Buf()
        cst_bf = sb("cst_bf", [128, 128 * 5 + 256 + 2048 + 4 * 512 * 2], BF16)
        CB = Buf()
        cst_f = sb("cst_f", [128, 4], F32)
        ar_t = sb("arena", [128, 12200], F32)
        AR = Arena(ar_t[:])
        AR2 = Arena(xbuf_t[:])
        YT = [Buf() for _ in range(KC)]
        DBG = Buf()

        pbanks = [psum("pb%d" % i, [128, 512], F32) for i in range(7)]
        prot = Ring([pbanks[i][:] for i in range(4)])
        P4, P5, P6 = pbanks[4][:], pbanks[5][:], pbanks[6][:]
        PB4, PB5, PB6 = Buf(), Buf(), Buf()
        ptr_t = psum("ptr", [128, 1024], BF16)
        ptr = ptr_t[:]
        PTR = Buf()

        def getps():
            return prot.get()

        def mm(out, lhsT, rhs, start, stop, r, w):
            S.op("pe", lambda e: e.matmul(out, lhsT, rhs, start=start, stop=stop), r, w)

        def act(out, in_, func, r, w, bias=None, scale=None):
            kw = {}
            if bias is not None:
                kw["bias"] = bias
            if scale is not None:
                kw["scale"] = scale
            S.op("act", lambda e: e.activation(out=out, in_=in_, func=func, **kw), r, w)

        def tto(eng, out, in0, in1, op, r, w):
            S.op(eng, lambda e: e.tensor_tensor(out=out, in0=in0, in1=in1, op=op), r, w)

        def stt(eng, out, in0, scalar, in1, op0, op1, r, w):
            S.op(eng, lambda e: e.scalar_tensor_tensor(out=out, in0=in0, scalar=scalar, in1=in1, op0=op0, op1=op1), r, w)

        def tsc(eng, out, in0, s1, s2, op0, op1, r, w):
            S.op(eng, lambda e: e.tensor_scalar(out=out, in0=in0, scalar1=s1, scalar2=s2, op0=op0, op1=op1), r, w)

        def tsm(eng, out, in0, s1, r, w):
            S.op(eng, lambda e: e.tensor_scalar_mul(out=out, in0=in0, scalar1=s1), r, w)

        def cp(eng, out, in_, r, w):
            if eng == "act":
                S.op("act", lambda e: e.activation(out=out, in_=in_, func=AF.Copy), r, w)
            else:
                S.op(eng, lambda e: e.tensor_copy(out=out, in_=in_), r, w)

        def memset(eng, ap, val, w):
            S.op(eng, lambda e: e.memset(ap, val), (), w)

        def recip(out, in_, r, w):
            S.op("dve", lambda e: e.reciprocal(out=out, in_=in_), r, w)

        def dma(q, out, in_, r, w):
            S.dma(q, lambda e: e.dma_start(out=out, in_=in_), r, w)

        def asel(ap, pattern, cmp, base, cm, w):
            S.op("pool", lambda e: e.affine_select(out=ap, in_=ap, pattern=pattern, compare_op=cmp, fill=0.0, base=base, channel_multiplier=cm), w, w)

        o = 0
        ident_bf = cst_bf[:, o:o + 128]; o += 128
        ones_bf = cst_bf[:, o:o + 128]; o += 128
        maskG = cst_bf[:, o:o + 128]; o += 128
        TriS = cst_bf[:, o:o + 128]; o += 128
        NU = cst_bf[:, o:o + 128]; o += 128
        Eb = cst_bf[:, o:o + 256].rearrange("p (b m) -> p b m", b=16); o += 256
        Xb = cst_bf[:, o:o + 2048].rearrange("p (a s) -> p a s", a=16); o += 2048
        DM = []
        NM = []
        for r_ in range(4):
            DM.append(cst_bf[:, o:o + 512]); o += 512
        for r_ in range(4):
            NM.append(cst_bf[:, o:o + 512]); o += 512
        eps_t = cst_f[:, 0:1]
        one_t = cst_f[:, 1:2]
        memset("dve", cst_f[:, 0:1], EPS, [CB])
        memset("dve", cst_f[:, 1:2], 1.0, [CB])
        tmpc = AR.f32(2048)
        TB = Buf()

        def mk(dst, np_, nf, val, pattern, cmp, base, cm, post=None):
            t = tmpc[0:np_, 0:nf]
            memset("pool", t, val, [TB])
            if pattern is not None:
                tv = t if len(pattern) == 1 else t.rearrange("p (a b) -> p a b", a=pattern[0][1])
                asel(tv, pattern, cmp, base, cm, [TB])
            if post is not None:
                post(t)
            cp("dve", dst, t, [TB], [CB])

        mk(ident_bf, 128, 128, 1.0, [[-1, 128]], ALU.is_equal, 0, 1)
        mk(ones_bf, 128, 128, 1.0, None, None, 0, 0)
        mk(maskG, 128, 128, 1.0, [[1, 128]], ALU.is_ge, 0, -1, post=lambda t: memset("pool", t[0:64, 64:128], 0.0, [TB]))
        tsm("dve", TriS, maskG, -1.0 / 16.0, [CB], [CB])
        mk(NU, 128, 128, -1.0, [[-1, 128]], ALU.is_ge, 0, 1)
        mk(cst_bf[:, 640:896], 128, 256, 1.0, [[1, 16], [-1, 16]], ALU.is_equal, 0, 0)
        mk(cst_bf[0:16, 896:896 + 2048], 16, 2048, -1.0, [[-1, 16], [0, 128]], ALU.is_gt, 0, 1)
        for r_ in range(4):
            mk(DM[r_], 128, 512, 1.0, [[1, 512]], ALU.is_gt, -128 * r_, -1)
            tsc("dve", NM[r_], DM[r_], 30000.0, -30000.0, ALU.mult, ALU.add, [CB], [CB])
        for l in range(NL):
            dma("sp", ppt[:, l * NPP:(l + 1) * NPP], pp[l], [], [PPB])

        def par(l, col):
            return ppt[:, l * NPP + col:l * NPP + col + 1]

        class WQ:
            def __init__(self):
                self.plan = []
                self.issued = 0
                self.consumed = 0

            def extend(self, items):
                self.plan.extend(items)

            def get(self, D=2):
                while self.issued < min(len(self.plan), self.consumed + 1 + D):
                    src, n = self.plan[self.issued]
                    slot = self.issued % NW
                    dma("pool", wsl[slot][:, :, 0:n], src.rearrange("(k p) n -> p k n", p=128), [], [WB[slot]])
                    self.issued += 1
                slot = self.consumed % NW
                self.consumed += 1
                return slot

        wq = WQ()

        def proj(slot, c0, n, tt, src=None, RB=None):
            if src is None:
                src, RB = xn, XN
            ps, pb = getps()
            for k in range(KC):
                mm(ps[0:n, :], wsl[slot][:, k, c0:c0 + n], src[:, k, tsl(tt)], k == 0, k == KC - 1, [WB[slot], RB[k][tt]], [pb])
            return ps, pb

        def dump(idx, src, RBs, bf):
            if not dbg:
                return
            for k in range(KC):
                dma("pool" if bf else "sp", dbg_t[idx, k * 128:(k + 1) * 128, :], src[:, k, :], RBs[k], [DBG])

        def rmsnorm(l, gcol):
            AR.reset()
            sqr = Ring([AR.bf16(TW) for _ in range(3)])
            srr = Ring([AR.f32(TW) for _ in range(2)])
            for tt in range(TT):
                ps, pb = getps()
                for k in range(KC):
                    sq, sqb = sqr.get()
                    act(sq, xb[:, k, tsl(tt)], AF.Square, [XB[k][tt]], [sqb])
                    mm(ps, ones_bf, sq, k == 0, k == KC - 1, [sqb, CB], [pb])
                sr, srb = srr.get()
                act(sr, ps, AF.Ln, [pb, CB], [srb], bias=eps_t, scale=1.0 / 1024.0)
                act(sr, sr, AF.Exp, [srb], [srb], scale=-0.5)
                for k in range(KC):
                    stt("dve", xn[:, k, tsl(tt)], xb[:, k, tsl(tt)], par(l, gcol + k), sr, ALU.mult, ALU.mult,
                        [XB[k][tt], srb, PPB], [XN[k][tt]])

        def spill_x():
            for k in range(KC):
                dma("sp", yT[k * 128:(k + 1) * 128, :], xb[:, k, :], XB[k], [YT[k]])

        def reload_x():
            for k in range(KC):
                dma("sp", xb[:, k, :], yT[k * 128:(k + 1) * 128, :], [YT[k]], XB[k])

        def gla(l):
            AR.reset()
            AR2.reset()
            A = AR2
            adT = A.bf16(T); ADT = [Buf() for _ in range(TT)]
            wupa = A.bf16(512); WUPA = Buf()
            e1r = Ring([A.f32(TW) for _ in range(2)])
            Pr = Ring([A.bf16(TW) for _ in range(2)])
            ecr = Ring([A.f32(TW) for _ in range(2)])
            eir = Ring([A.f32(TW) for _ in range(2)])
            qd = A.bf16(T); QD = [Buf() for _ in range(TT)]
            ki = A.bf16(T); KI = [Buf() for _ in range(TT)]
            ki_tm = A.bf16(T); KITM = [Buf() for _ in range(TT)]
            v_tm = A.bf16(16 * 256); VTM = [Buf() for _ in range(8)]
            sg = [A.bf16(T), A.bf16(T)]; SG = [[Buf() for _ in range(TT)] for _ in range(2)]
            scr = Ring([A.bf16(128) for _ in range(3)])
            dec = A.f32(32); DEC = Buf()
            sqr = Ring([A.bf16(TW) for _ in range(2)])
            srr = Ring([A.f32(TW) for _ in range(2)])
            posb = [Ring([A.f32(TW), A.f32(TW)]), Ring([A.f32(TW), A.f32(TW)])]
            kvs = AR.f32(16 * 256); KVS = [Buf() for _ in range(16)]
            S_all = AR.f32(17 * 256); SAL = [Buf() for _ in range(17)]
            S_bfa = AR.bf16(16 * 256); SBA = [Buf() for _ in range(4)]
            for tt in range(TT):
                memset("dve", adT[0:33, tsl(tt)], 1.0, [ADT[tt]])
            memset("dve", wupa[0:33, :], 0.0, [WUPA])
            dma("pool", wupa[0:16, :], gla_w_up[l], [], [WUPA])
            dma("pool", wupa[32:33, :], gla_b_alpha[l:l + 1, :], [], [WUPA])
            plan = [(w_in[l][:, C_GD:C_GD + 16], 16)]
            for h in range(4):
                plan += [(w_in[l][:, C_GQ + h * 128:C_GQ + (h + 1) * 128], 128),
                         (w_in[l][:, C_GK + h * 128:C_GK + (h + 1) * 128], 128),
                         (w_in[l][:, C_GV + h * 256:C_GV + (h + 1) * 256], 256),
                         (w_in[l][:, C_GG + h * 256:C_GG + (h + 1) * 256], 256)]
            wq.extend(plan)
            s_ = wq.get(D=1)
            for tt in range(TT):
                ps, pb = proj(s_, 0, 16, tt)
                cp("act", adT[0:16, tsl(tt)], ps[0:16, :], [pb], [ADT[tt]])
            for h in range(4):
                s_q = wq.get(D=2)
                s_k = wq.get(D=2)
                for tt in range(TT + 1):
                    if tt < TT:
                        psy, pyb = getps()
                        for mi in range(4):
                            m = tt * 4 + mi
                            mm(psy[:, mi * 128:(mi + 1) * 128], adT[0:33, m * 128:(m + 1) * 128], wupa[0:33, h * 128:(h + 1) * 128],
                               True, True, [ADT[tt], WUPA], [pyb])
                        e1, e1b = e1r.get()
                        act(e1, psy, AF.Exp, [pyb], [e1b], scale=-1.0)
                        Pt, Ptb = Pr.get()
                        act(Pt, e1, AF.Ln, [e1b, CB], [Ptb], bias=one_t)
                        psq, pqb = proj(s_q, 0, 128, tt)
                        psk, pkb = proj(s_k, 0, 128, tt)
                        psc, pcb = getps()
                        for mi in range(4):
                            mm(psc[:, mi * 128:(mi + 1) * 128], Pt[:, mi * 128:(mi + 1) * 128], TriS, True, True, [Ptb, CB], [pcb])
                        ec, ecb = ecr.get()
                        ei, eib = eir.get()
                        act(ec, psc, AF.Exp, [pcb], [ecb])
                        act(ei, psc, AF.Exp, [pcb], [eib], scale=-1.0)
                        cp("dve", dec[:, tt * 8:(tt + 1) * 8], ec[:, 63::64], [ecb], [DEC])
                        stt("dve", qd[:, tsl(tt)], psq, 128.0 ** -0.5, ec, ALU.mult, ALU.mult, [pqb, ecb], [QD[tt]])
                        tto("dve", ki[:, tsl(tt)], psk, ei, ALU.mult, [pkb, eib], [KI[tt]])
                    if tt >= 1:
                        t2 = tt - 1
                        for mi in range(4):
                            m = t2 * 4 + mi
                            S.op("pe", (lambda o_, i_: (lambda e: e.transpose(o_, i_, ident_bf)))(ptr[:, mi * 128:(mi + 1) * 128], ki[:, m * 128:(m + 1) * 128]),
                                 [KI[t2], CB], [PTR])
                        cp("act", ki_tm[:, tsl(t2)], ptr[:, 0:512], [PTR], [KITM[t2]])
                s_v = wq.get(D=2)
                for m2 in range(8):
                    ps, pb = getps()
                    for mj in range(2):
                        m = m2 * 2 + mj
                        for k in range(KC):
                            mm(ps[:, mj * 256:(mj + 1) * 256], xn[:, k, m * 128:(m + 1) * 128], wsl[s_v][:, k, 0:256],
                               k == 0, k == KC - 1, [XN[k][m // 4], WB[s_v]], [pb])
                    cp("act", v_tm[:, m2 * 512:(m2 + 1) * 512], ps, [pb], [VTM[m2]])
                s_g = wq.get(D=2)
                for jb in range(2):
                    for tt in range(TT):
                        ps, pb = proj(s_g, jb * 128, 128, tt)
                        act(sg[jb][:, tsl(tt)], ps, AF.Silu, [pb], [SG[jb][tt]])
                pend = []

                def normrest(tt, pcs, h=h):
                    pn, pnb = getps()
                    for jb in range(2):
                        sq, sqb = sqr.get()
                        act(sq, pcs[jb][0], AF.Square, [pcs[jb][1]], [sqb])
                        mm(pn, ones_bf, sq, jb == 0, jb == 1, [sqb, CB], [pnb])
                    sr, srb = srr.get()
                    act(sr, pn, AF.Ln, [pnb, CB], [srb], bias=eps_t, scale=1.0 / 256.0)
                    act(sr, sr, AF.Exp, [srb], [srb], scale=-0.5)
                    for jb in range(2):
                        pc, pcb_ = pcs[jb]
                        tto("dve", pc, pc, sr, ALU.mult, [pcb_, srb], [pcb_])
                        stt("dve", ob[:, h * 2 + jb, tsl(tt)], pc, par(l, P_GLAN + jb), sg[jb][:, tsl(tt)], ALU.mult, ALU.mult,
                            [pcb_, PPB, SG[jb][tt]], [OB[h * 2 + jb][tt]])

                for hs in range(2):
                    for ci in range(16):
                        c = hs * 16 + ci
                        m = c // 2
                        half = c % 2
                        pkv, pkb = getps()
                        mm(pkv[:, 0:256], ki_tm[half * 64:(half + 1) * 64, m * 128:(m + 1) * 128],
                           v_tm[half * 64:(half + 1) * 64, m * 256:(m + 1) * 256], True, True, [KITM[m // 4], VTM[m // 2]], [pkb])
                        act(kvs[:, ci * 256:(ci + 1) * 256], pkv[:, 0:256], AF.Identity, [pkb, DEC], [KVS[ci]], scale=dec[:, c:c + 1])
                    if hs == 0:
                        memset("dve", S_all[:, 0:256], 0.0, [SAL[0]])
                    else:
                        cp("dve", S_all[:, 0:256], S_all[:, 16 * 256:17 * 256], [SAL[16]], [SAL[0]])
                    for ci in range(16):
                        c = hs * 16 + ci
                        stt("dve", S_all[:, (ci + 1) * 256:(ci + 2) * 256], S_all[:, ci * 256:(ci + 1) * 256], dec[:, c:c + 1],
                            kvs[:, ci * 256:(ci + 1) * 256], ALU.mult, ALU.add, [SAL[ci], DEC, KVS[ci]], [SAL[ci + 1]])
                        if ci % 4 == 3:
                            g4 = ci // 4
                            cp("act", S_bfa[:, g4 * 1024:(g4 + 1) * 1024], S_all[:, g4 * 1024:(g4 + 1) * 1024],
                               [SAL[g4 * 4 + j] for j in range(4)], [SBA[g4]])
                    for tl in range(2):
                        tt = hs * 2 + tl
                        po = [P4, P5]
                        POB = [PB4, PB5]
                        scl = {}
                        for it in range(5):
                            if it < 4:
                                mi = it
                                m = tt * 4 + mi
                                pss, psb = getps()
                                mm(pss[:, 0:128], ki[:, m * 128:(m + 1) * 128], qd[:, m * 128:(m + 1) * 128], True, True, [KI[tt], QD[tt]], [psb])
                                sc, scb = scr.get()
                                tto("dve", sc, pss[:, 0:128], maskG, ALU.mult, [psb, CB], [scb])
                                scl[mi] = (sc, scb)
                            if it >= 1:
                                mi = it - 1
                                m = tt * 4 + mi
                                sc, scb = scl.pop(mi)
                                for half in range(2):
                                    c = 2 * m + half
                                    ci = c - hs * 16
                                    for jb in range(2):
                                        col = mi * 128 + half * 64
                                        mm(po[jb][:, col:col + 64], S_bfa[:, ci * 256 + jb * 128:ci * 256 + (jb + 1) * 128], qd[:, c * 64:(c + 1) * 64],
                                           True, False, [SBA[ci // 4], QD[tt]], [POB[jb]])
                                        mm(po[jb][:, col:col + 64], v_tm[:, m * 256 + jb * 128:m * 256 + (jb + 1) * 128], sc[:, half * 64:(half + 1) * 64],
                                           False, True, [VTM[m // 2], scb], [POB[jb]])
                        pcs = []
                        for jb in range(2):
                            pc, pcb_ = posb[jb].get()
                            cp("act", pc, po[jb], [POB[jb]], [pcb_])
                            pcs.append((pc, pcb_))
                        if pend:
                            normrest(*pend.pop())
                        pend.append((tt, pcs))
                while pend:
                    normrest(*pend.pop())

        def branch(l, b):
            AR.reset()
            gr = Ring([AR.f32(TW) for _ in range(2)])
            tr = Ring([AR.f32(TW) for _ in range(2)])
            plan = []
            for nbp in range(4):
                plan += [(w_br[b][l][:, nbp * 256:(nbp + 1) * 256], 256),
                         (w_in[l][:, C_GATE + b * 1024 + nbp * 256:C_GATE + b * 1024 + (nbp + 1) * 256], 256)]
            wq.extend(plan)
            for nbp in range(4):
                s1 = wq.get(D=2)
                s2 = wq.get(D=2)
                for nbi in range(2):
                    nb = nbp * 2 + nbi
                    for tt in range(TT):
                        ps1, p1b = proj(s1, nbi * 128, 128, tt, ob, OB)
                        ps2, p2b = proj(s2, nbi * 128, 128, tt)
                        g, gb = gr.get()
                        act(g, ps2, AF.Sigmoid, [p2b, PPB], [gb], bias=par(l, P_BG + b * 8 + nb))
                        if b == 0:
                            tto("dve", xb[:, nb, tsl(tt)], ps1, g, ALU.mult, [p1b, gb], [XB[nb][tt]])
                        else:
                            t_, tb = tr.get()
                            tto("dve", t_, ps1, g, ALU.mult, [p1b, gb], [tb])
                            tto("dve", xb[:, nb, tsl(tt)], xb[:, nb, tsl(tt)], t_, ALU.add, [tb, XB[nb][tt]], [XB[nb][tt]])

        def lru(l):
            AR.reset()
            A = AR
            cl = A.f32(8); CL = Buf()
            tmp8 = A.f32(8)
            lxbf = [A.bf16(T + 4), A.bf16(T + 4)]; LXB = [[Buf() for _ in range(TT)] for _ in range(2)]
            dg = [[A.bf16(128) for _ in range(4)] for _ in range(2)]; DG = Buf()
            wab = A.bf16(512).rearrange("p (i j) -> p i j", i=2); wxb = A.bf16(512).rearrange("p (i j) -> p i j", i=2); WAB = Buf(); WXB = Buf()
            xcf = [Ring([A.f32(TW) for _ in range(1)]) for _ in range(2)]
            xcb = [Ring([A.bf16(TW) for _ in range(1)]) for _ in range(2)]
            rr = Ring([A.f32(TW) for _ in range(2)])
            igr = Ring([A.f32(TW) for _ in range(2)])
            n2r = Ring([A.f32(TW) for _ in range(2)])
            hr = [Ring([A.f32(TW) for _ in range(2)]) for _ in range(2)]
            glr = Ring([A.f32(TW) for _ in range(2)])
            g2r = Ring([A.f32(TW) for _ in range(2)])
            act(tmp8, ppt[:, l * NPP + P_LAM:l * NPP + P_LAM + 8], AF.Exp, [PPB], [CL], scale=-1.0)
            act(tmp8, tmp8, AF.Ln, [CL, CB], [CL], bias=one_t)
            tsm("dve", cl, tmp8, -8.0, [CL], [CL])
            plan = []
            for hb in range(4):
                plan += [(w_in[l][:, C_LX + hb * 256:C_LX + (hb + 1) * 256], 256), (w_in[l][:, C_LG + hb * 256:C_LG + (hb + 1) * 256], 256)]
            wq.extend(plan)
            for hb in range(4):
                slx = wq.get(D=2)
                slg = wq.get(D=2)
                dma("pool", wab, lru_w_a[l, hb].rearrange("(i p) j -> p i j", p=128), [], [WAB])
                dma("pool", wxb, lru_w_x[l, hb].rearrange("(i p) j -> p i j", p=128), [], [WXB])
                for cbl in range(2):
                    memset("dve", lxbf[cbl][:, 0:3], 0.0, [LXB[cbl][0]])
                    for w_ in range(4):
                        tsm("dve", dg[cbl][w_], ident_bf, par(l, P_CW + (hb * 2 + cbl) * 4 + w_), [CB, PPB], [DG])
                hprev = [None, None]
                for tt in range(TT):
                    for cbl in range(2):
                        ps, pb = proj(slx, cbl * 128, 128, tt)
                        cp("act", lxbf[cbl][:, 3 + tt * TW:3 + (tt + 1) * TW], ps, [pb], [LXB[cbl][tt]])
                    xf = []
                    xh = []
                    for cbl in range(2):
                        ps, pb = getps()
                        rd = [LXB[cbl][tt], DG] + ([LXB[cbl][tt - 1]] if tt > 0 else [])
                        for w_ in range(4):
                            mm(ps, dg[cbl][w_], lxbf[cbl][:, tt * TW + w_:tt * TW + w_ + TW], w_ == 0, w_ == 3, rd, [pb])
                        f_, fb = xcf[cbl].get()
                        act(f_, ps, AF.Identity, [pb, PPB], [fb], bias=par(l, P_CB + hb * 2 + cbl))
                        h_, hb_ = xcb[cbl].get()
                        cp("dve", h_, f_, [fb], [hb_])
                        xf.append((f_, fb))
                        xh.append((h_, hb_))
                    rs, igs, gls, g2s, n2s, hhs = [], [], [], [], [], []
                    for jb in range(2):
                        c = hb * 2 + jb
                        psr, prb = getps()
                        for ib in range(2):
                            mm(psr, wab[:, ib, jb * 128:(jb + 1) * 128], xh[ib][0], ib == 0, ib == 1, [WAB, xh[ib][1]], [prb])
                        psi, pib = getps()
                        for ib in range(2):
                            mm(psi, wxb[:, ib, jb * 128:(jb + 1) * 128], xh[ib][0], ib == 0, ib == 1, [WXB, xh[ib][1]], [pib])
                        r_, rb = rr.get()
                        act(r_, psr, AF.Sigmoid, [prb, PPB], [rb], bias=par(l, P_BA + c))
                        ig, igb = igr.get()
                        act(ig, psi, AF.Sigmoid, [pib, PPB], [igb], bias=par(l, P_BX + c))
                        rs.append((r_, rb))
                        igs.append((ig, igb))
                    for jb in range(2):
                        c = hb * 2 + jb
                        a_, ab = rs[jb]
                        act(a_, a_, AF.Exp, [ab, CL], [ab], scale=cl[:, c:c + 1])
                    for jb in range(2):
                        psg, pgb = proj(slg, jb * 128, 128, tt)
                        gl, glb = glr.get()
                        act(gl, psg, AF.Identity, [pgb], [glb])
                        gls.append((gl, glb))
                    for jb in range(2):
                        a_, ab = rs[jb]
                        n2, n2b = n2r.get()
                        stt("dve", n2, a_, -1.0, a_, ALU.mult, ALU.mult, [ab], [n2b])
                        n2s.append((n2, n2b))
                        gl, glb = gls[jb]
                        g2, g2b = g2r.get()
                        tto("dve", g2, gl, gl, ALU.mult, [glb], [g2b])
                        tsc("dve", g2, g2, 0.044715, 1.0, ALU.mult, ALU.add, [g2b], [g2b])
                        tto("dve", g2, g2, gl, ALU.mult, [g2b, glb], [g2b])
                        g2s.append((g2, g2b))
                    for jb in range(2):
                        n2, n2b = n2s[jb]
                        act(n2, n2, AF.Sqrt, [n2b, CB], [n2b], bias=one_t)
                    for jb in range(2):
                        g2, g2b = g2s[jb]
                        act(g2, g2, AF.Sigmoid, [g2b], [g2b], scale=1.5957691216057308)
                    for jb in range(2):
                        c = hb * 2 + jb
                        a_, ab = rs[jb]
                        ig, igb = igs[jb]
                        n2, n2b = n2s[jb]
                        gl, glb = gls[jb]
                        g2, g2b = g2s[jb]
                        u_, ub = ig, igb
                        tto("dve", u_, n2, ig, ALU.mult, [n2b, igb], [ub])
                        tto("dve", u_, u_, xf[jb][0], ALU.mult, [ub, xf[jb][1]], [ub])
                        hh, hhb = hr[jb].get()
                        if hprev[jb] is None:
                            S.op("dve", (lambda o_, a0, u0: (lambda e: e.tensor_tensor_scan(out=o_, data0=a0, data1=u0, initial=0.0, op0=ALU.mult, op1=ALU.add)))(hh, a_, u_),
                                 [ab, ub], [hhb])
                        else:
                            hp, hpb = hprev[jb]
                            S.op("dve", (lambda o_, a0, u0, i0: (lambda e: e.tensor_tensor_scan(out=o_, data0=a0, data1=u0, initial=i0, op0=ALU.mult, op1=ALU.add)))(hh, a_, u_, hp[:, TW - 1:TW]),
                                 [ab, ub, hpb], [hhb])
                        hprev[jb] = (hh, hhb)
                        tto("dve", gl, gl, hh, ALU.mult, [glb, hhb], [glb])
                        tto("dve", ob[:, c, tsl(tt)], gl, g2, ALU.mult, [glb, g2b], [OB[c][tt]])

        def sbattn(l):
            AR.reset()
            A = AR
            Lk = A.bf16(16 * TW); LK = [Buf() for _ in range(16)]
            qn = A.bf16(T); QN = [Buf() for _ in range(TT)]
            kn = A.bf16(T); KN = [Buf() for _ in range(TT)]
            v_tm = A.bf16(T); VT = [Buf() for _ in range(4)]
            Er = Ring([A.f32(TW) for _ in range(2)])
            wr = Ring([A.bf16(TW) for _ in range(3)])
            sqr = Ring([A.bf16(TW) for _ in range(2)])
            srr = Ring([A.f32(TW) for _ in range(2)])
            csr = Ring([A.bf16(TW) for _ in range(2)])
            gq2 = A.f32(2); GQ = Buf()
            tsm("dve", gq2[:, 0:1], par(l, P_SQG), 128.0 ** -0.5, [PPB], [GQ])
            cp("dve", gq2[:, 1:2], par(l, P_SKG), [PPB], [GQ])
            plan = []
            for h in range(8):
                plan += [(w_in[l][:, C_SQ + h * 128:C_SQ + (h + 1) * 128], 128), (w_in[l][:, C_SK + h * 128:C_SK + (h + 1) * 128], 128),
                         (w_in[l][:, C_SV + h * 128:C_SV + (h + 1) * 128], 128)]
            wq.extend(plan)
            for h in range(8):
                slots_qk = [wq.get(D=2), wq.get(D=2)]
                items = [(which, tt) for which in range(2) for tt in range(TT)]
                stA = {}

                def stageA(i):
                    which, tt = items[i]
                    ps, pb = proj(slots_qk[which], 0, 128, tt)
                    sq, sqb = sqr.get()
                    act(sq, ps, AF.Square, [pb], [sqb])
                    stA[i] = (ps, pb, sq, sqb)

                def stageB(i):
                    which, tt = items[i]
                    dst, DBs = ((qn, QN), (kn, KN))[which]
                    ps, pb, sq, sqb = stA.pop(i)
                    pn, pnb = getps()
                    mm(pn, ones_bf, sq, True, True, [sqb, CB], [pnb])
                    sr, srb = srr.get()
                    act(sr, pn, AF.Ln, [pnb, CB], [srb], bias=eps_t, scale=1.0 / 128.0)
                    act(sr, sr, AF.Exp, [srb], [srb], scale=-0.5)
                    stt("dve", dst[:, tsl(tt)], ps, gq2[:, which:which + 1], sr, ALU.mult, ALU.mult, [pb, srb, GQ], [DBs[tt]])

                for i in range(len(items) + 1):
                    if i < len(items):
                        stageA(i)
                    if i >= 1:
                        stageB(i - 1)
                s_v = wq.get(D=2)
                for m4 in range(4):
                    ps, pb = getps()
                    for mi in range(4):
                        m = m4 * 4 + mi
                        for k in range(KC):
                            mm(ps[:, mi * 128:(mi + 1) * 128], xn[:, k, m * 128:(m + 1) * 128], wsl[s_v][:, k, 0:128],
                               k == 0, k == KC - 1, [XN[k][m4], WB[s_v]], [pb])
                    cp("act", v_tm[:, m4 * 512:(m4 + 1) * 512], ps, [pb], [VT[m4]])
                LAG = 2
                csl = {}
                wl = {}

                def p1(qt, a):
                    psz, pzb = getps()
                    mm(psz, kn[:, a * 128:(a + 1) * 128], qn[:, tsl(qt)], True, True, [KN[a // 4], QN[qt]], [pzb])
                    E, Eb_ = Er.get()
                    act(E, psz, AF.Exp, [pzb], [Eb_])
                    La = Lk[:, a * TW:(a + 1) * TW]
                    act(La, E, AF.Ln, [Eb_, CB], [LK[a]], bias=one_t)
                    if a >= 4 * qt:
                        tto("dve", La, La, DM[a - 4 * qt], ALU.mult, [LK[a], CB], [LK[a]])

                def p1cs(qt, a):
                    nk = 4 * (qt + 1)
                    La = Lk[:, a * TW:(a + 1) * TW]
                    mm(P4[0:16, :], Eb[:, a, :], La, a == 0, a == nk - 1, [LK[a], CB], [PB4])

                def p1fin(qt):
                    cs, csb = csr.get()
                    cp("dve", cs[0:16, :], P4[0:16, :], [PB4], [csb])
                    csl[qt] = (cs, csb)

                def p2(qt, a):
                    cs, csb = csl[qt]
                    diag = a >= 4 * qt
                    psw, pwb = getps()
                    La = Lk[:, a * TW:(a + 1) * TW]
                    mm(psw, kn[:, a * 128:(a + 1) * 128], qn[:, tsl(qt)], True, False, [KN[a // 4], QN[qt]], [pwb])
                    mm(psw, NU, La, False, False, [CB, LK[a]], [pwb])
                    mm(psw, Xb[0:16, a, :], cs[0:16, :], False, not diag, [CB, csb], [pwb])
                    if diag:
                        mm(psw, ident_bf, NM[a - 4 * qt], False, True, [CB], [pwb])
                    w_, wb_ = wr.get()
                    act(w_, psw, AF.Exp, [pwb], [wb_])
                    wl[(qt, a)] = (w_, wb_)

                def p2pv(qt, a):
                    nk = 4 * (qt + 1)
                    po, pob = (P5, PB5) if qt % 2 == 0 else (P6, PB6)
                    w_, wb_ = wl.pop((qt, a))
                    mm(po, v_tm[:, a * 128:(a + 1) * 128], w_, a == 0, a == nk - 1, [VT[a // 4], wb_], [pob])

                def p2fin(qt):
                    po, pob = (P5, PB5) if qt % 2 == 0 else (P6, PB6)
                    cp("dve", ob[:, h, tsl(qt)], po, [pob], [OB[h][qt]])

                for it in range(4 + LAG):
                    if it < 4:
                        p1(0, it)
                    if it >= LAG:
                        p1cs(0, it - LAG)
                p1fin(0)
                for qt in range(TT):
                    nk = 4 * (qt + 1)
                    nxt = qt + 1 < TT
                    nk2 = nk + 4 if nxt else 0
                    for it in range(max(nk, nk2) + LAG):
                        if it < nk:
                            p2(qt, it)
                        if nxt and it < nk2:
                            p1(qt + 1, it)
                        if LAG <= it < nk + LAG:
                            p2pv(qt, it - LAG)
                        if nxt and LAG <= it < nk2 + LAG:
                            p1cs(qt + 1, it - LAG)
                    if nxt:
                        p1fin(qt + 1)
                    p2fin(qt)

        def wout(l):
            AR.reset()
            for nb in range(KC):
                for tt in range(TT):
                    cp("act" if (nb + tt) % 2 == 0 else "dve", ob[:, nb, tsl(tt)], xb[:, nb, tsl(tt)], [XB[nb][tt]], [OB[nb][tt]])
            dump(4, ob, OB, True)
            reload_x()
            wq.extend([(w_out[l][:, i * 256:(i + 1) * 256], 256) for i in range(4)])
            for i in range(4):
                s_ = wq.get(D=2)
                for nbi in range(2):
                    nb = i * 2 + nbi
                    for tt in range(TT):
                        ps, pb = proj(s_, nbi * 128, 128, tt, ob, OB)
                        tto("dve", xb[:, nb, tsl(tt)], ps, xb[:, nb, tsl(tt)], ALU.add, [pb, XB[nb][tt]], [XB[nb][tt]])

        def mlp(l):
            rmsnorm(l, P_GMLP)
            rr = Ring([AR.f32(TW) for _ in range(3)])
            plan = []
            for fg in range(4):
                plan += [(w_up[l][:, fg * 1024 + i * 256:fg * 1024 + (i + 1) * 256], 256) for i in range(4)]
                plan += [(w_dn[l][fg * 1024:(fg + 1) * 1024, i * 256:(i + 1) * 256], 256) for i in range(4)]
            wq.extend(plan)
            for fg in range(4):
                for i in range(4):
                    s_ = wq.get(D=3)
                    for fbi in range(2):
                        fb = i * 2 + fbi
                        for tt in range(TT):
                            ps, pb = proj(s_, fbi * 128, 128, tt)
                            r_, rb = rr.get()
                            act(r_, ps, AF.Relu, [pb], [rb])
                            tto("dve", ob[:, fb, tsl(tt)], r_, r_, ALU.mult, [rb], [OB[fb][tt]])
                for i in range(4):
                    s_ = wq.get(D=3)
                    for nbi in range(2):
                        nb = i * 2 + nbi
                        for tt in range(TT):
                            ps, pb = proj(s_, nbi * 128, 128, tt, ob, OB)
                            tto("dve", xb[:, nb, tsl(tt)], ps, xb[:, nb, tsl(tt)], ALU.add, [pb, XB[nb][tt]], [XB[nb][tt]])

        S.barrier()
        for k in range(KC):
            dma("sp", xb[:, k, :], xT[k * 128:(k + 1) * 128, :], [], XB[k])
        for l in range(NL):
            rmsnorm(l, P_GMIX)
            if l == 0:
                dump(0, xn, XN, True)
            spill_x()
            S.barrier()
            gla(l)
            if l == 0:
                dump(1, ob, OB, True)
            S.barrier()
            branch(l, 0)
            S.barrier()
            lru(l)
            if l == 0:
                dump(2, ob, OB, True)
            S.barrier()
            branch(l, 1)
            S.barrier()
            sbattn(l)
            if l == 0:
                dump(3, ob, OB, True)
            S.barrier()
            branch(l, 2)
            S.barrier()
            wout(l)
            if l == 0:
                dump(5, xb, XB, False)
            S.barrier()
            mlp(l)
            S.barrier()
        for k in range(KC):
            dma("sp", yT[k * 128:(k + 1) * 128, :], xb[:, k, :], XB[k], [YT[k]])
        S.barrier()
        S.emit(st)
        print("instructions:", S.ninst, {e: len(S.streams[e]) for e in ENGS})
    return nc


def pack_params(inp, layers):
    def fm(v, nb):
        return np.ascontiguousarray(np.asarray(v, np.float32).reshape(nb, 128).T)

    out = np.zeros((len(layers), 128, NPP), np.float32)
    for i, l in enumerate(layers):
        out[i, :, P_GMIX:P_GMIX + 8] = fm(inp["norm_mix_g"][l], 8)
        out[i, :, P_GMLP:P_GMLP + 8] = fm(inp["norm_mlp_g"][l], 8)
        out[i, :, P_GLAN:P_GLAN + 2] = fm(inp["gla_norm_g"][l], 2)
        cw = np.asarray(inp["lru_conv_w"][l], np.float32)
        for cb in range(8):
            for w_ in range(4):
                out[i, :, P_CW + cb * 4 + w_] = cw[w_, cb * 128:(cb + 1) * 128]
        out[i, :, P_CB:P_CB + 8] = fm(inp["lru_conv_b"][l], 8)
        out[i, :, P_BA:P_BA + 8] = fm(inp["lru_b_a"][l], 8)
        out[i, :, P_BX:P_BX + 8] = fm(inp["lru_b_x"][l], 8)
        out[i, :, P_LAM:P_LAM + 8] = fm(inp["lru_lambda"][l], 8)
        out[i, :, P_SQG] = np.asarray(inp["sb_q_norm_g"][l], np.float32)
        out[i, :, P_SKG] = np.asarray(inp["sb_k_norm_g"][l], np.float32)
        out[i, :, P_BG:P_BG + 24] = fm(inp["b_gate"][l], 24)
    return out


_NC_CACHE = {}
WNAMES = ["w_in", "gla_w_up", "gla_b_alpha", "lru_w_a", "lru_w_x", "w_branch_a", "w_branch_b", "w_branch_c", "w_out", "w_mlp_up", "w_mlp_down"]


def _get_nc(NL, dbg=False):
    key = (NL, dbg)
    if key not in _NC_CACHE:
        _NC_CACHE[key] = build(NL, dbg)
    return _NC_CACHE[key]


def run_layers(inp, xT_list, layers, dbg=False, cores=8):
    nc = _get_nc(len(layers), dbg)
    ppk = pack_params(inp, layers)
    shared = {"pp": ppk}
    for n in WNAMES:
        shared[n] = np.ascontiguousarray(np.asarray(inp[n], np.float32)[layers[0]:layers[-1] + 1])
    in_maps = []
    for c in range(cores):
        d = dict(shared)
        d["xT"] = xT_list[c]
        in_maps.append(d)
    res = run_bass_kernel_spmd(nc, in_maps, core_ids=list(range(cores)))
    return res.results


def kernel(**inputs):
    x = np.asarray(inputs["x"], np.float32)
    B = x.shape[0]
    xT = [np.ascontiguousarray(x[b].T) for b in range(B)]
    if FUSED:
        res = run_layers(inputs, xT, list(range(DEPTH)))
        xT = [r["yT"] for r in res]
    else:
        for l in range(DEPTH):
            res = run_layers(inputs, xT, [l])
            xT = [np.ascontiguousarray(r["yT"]) for r in res]
    return np.stack([np.asarray(t).T for t in xT], axis=0).astype(np.float32)
```

```python
from contextlib import ExitStack
import numpy as np
import concourse.bass as bass
import concourse.mybir as mybir
from concourse.bass_utils import run_bass_kernel_spmd

F32 = mybir.dt.float32
BF16 = mybir.dt.bfloat16
AF = mybir.ActivationFunctionType
ALU = mybir.AluOpType

FUSED = True
DEPTH = 4
T = 2048
TT = 4
TW = 512
KC = 8
NW = 4
EPS = 1e-6
C_GQ, C_GK, C_GV, C_GG, C_GD, C_LX, C_LG, C_SQ, C_SK, C_SV, C_GATE = 0, 512, 1024, 2048, 3072, 3088, 4112, 5136, 6160, 7184, 8208
IN_COLS = 11280
P_GMIX, P_GMLP, P_GLAN, P_CW, P_CB, P_BA, P_BX, P_LAM, P_SQG, P_SKG, P_BG, NPP = 0, 8, 16, 18, 50, 58, 66, 74, 82, 83, 84, 108


class Buf:
    __slots__ = ("w", "r")

    def __init__(self):
        self.w = None
        self.r = {}


ENGS = ("pe", "act", "dve", "pool", "sp")
EPOCH = 20000
DMA_EPOCH = 1000


class Sched:
    def __init__(self, nc, n_dma_ch=8):
        self.nc = nc
        self.streams = {e: [] for e in ENGS}
        self.cnt = {e: 0 for e in ENGS}
        self.epoch = {e: 0 for e in ENGS}
        self.waited = {e: {} for e in ENGS}
        self.n_dma_ch = n_dma_ch
        self.dcnt = [0] * n_dma_ch
        self.depoch = [0] * n_dma_ch
        self.dnext = 0
        self.semkeys = []
        self._seen = set()
        self.latest = {}
        self.ninst = 0

    def _key(self, k):
        if k not in self._seen:
            self._seen.add(k)
            self.semkeys.append(k)
        return k

    def _filter(self, eng, need):
        out = []
        wd = self.waited[eng]
        for s, v in need.items():
            if eng == "pe" and s[0] == "pe":
                continue
            if wd.get(s, 0) < v:
                wd[s] = v
                out.append((s, v))
        return out

    def _deps(self, eng, reads, writes):
        need = {}
        for b in reads:
            if b.w is not None:
                s, v = b.w
                if need.get(s, 0) < v:
                    need[s] = v
        for b in writes:
            if b.w is not None:
                s, v = b.w
                if need.get(s, 0) < v:
                    need[s] = v
            for s, v in b.r.items():
                if need.get(s, 0) < v:
                    need[s] = v
        return self._filter(eng, need)

    def _mark(self, ev, reads, writes):
        s, v = ev
        self.latest[s] = v
        for b in reads:
            if b.r.get(s, 0) < v:
                b.r[s] = v
        for b in writes:
            b.w = ev
            b.r = {}

    def op(self, eng, fn, reads=(), writes=()):
        deps = self._deps(eng, reads, writes)
        if self.cnt[eng] >= EPOCH:
            self.epoch[eng] += 1
            self.cnt[eng] = 0
        self.cnt[eng] += 1
        key = self._key((eng, self.epoch[eng]))
        self.streams[eng].append((deps, fn, key, 1))
        self._mark((key, self.cnt[eng]), reads, writes)
        self.ninst += 1

    def dma(self, queue, fn, reads=(), writes=()):
        deps = self._deps(queue, reads, writes)
        ch = self.dnext
        self.dnext = (self.dnext + 1) % self.n_dma_ch
        if self.dcnt[ch] >= DMA_EPOCH:
            self.depoch[ch] += 1
            self.dcnt[ch] = 0
        self.dcnt[ch] += 1
        key = self._key(("dma%d" % ch, self.depoch[ch]))
        self.streams[queue].append((deps, fn, key, 16))
        self._mark((key, 16 * self.dcnt[ch]), reads, writes)
        self.ninst += 1

    def barrier(self):
        for eng in ENGS:
            deps = self._filter(eng, dict(self.latest))
            if deps:
                self.streams[eng].append((deps, None, None, 0))

    def emit(self, stack):
        nc = self.nc
        sems = {}
        for k in self.semkeys:
            sems[k] = stack.enter_context(nc.semaphore("s_%s_%d" % k))
        block = stack.enter_context(nc.Block())
        streams = self.streams

        def run(engobj, lst):
            for deps, fn, key, inc in lst:
                for s, v in deps:
                    engobj.wait_ge(sems[s], v)
                if fn is not None:
                    fn(engobj).then_inc(sems[key], inc)

        @block.tensor
        def _(e):
            run(e, streams["pe"])

        @block.scalar
        def _(e):
            run(e, streams["act"])

        @block.vector
        def _(e):
            run(e, streams["dve"])

        @block.gpsimd
        def _(e):
            run(e, streams["pool"])

        @block.sync
        def _(e):
            run(e, streams["sp"])


class Arena:
    def __init__(self, ap):
        self.ap = ap
        self.n = ap.shape[1]
        self.off = 0

    def f32(self, n):
        a = self.ap[:, self.off:self.off + n]
        self.off += n
        assert self.off <= self.n, ("arena overflow", self.off, self.n)
        return a

    def bf16(self, n):
        nn = (n + 1) // 2
        a = self.ap[:, self.off:self.off + nn].bitcast(BF16)
        self.off += nn
        assert self.off <= self.n, ("arena overflow", self.off, self.n)
        return a

    def reset(self):
        self.off = 0


class Ring:
    def __init__(self, aps):
        self.items = [(a, Buf()) for a in aps]
        self.i = 0

    def get(self):
        it = self.items[self.i]
        self.i = (self.i + 1) % len(self.items)
        return it


def tsl(tt):
    return slice(tt * TW, (tt + 1) * TW)


def build(NL, dbg=False):
    nc = bass.Bass("TRN2", target_bir_lowering=False)

    def din(name, shape):
        return nc.dram_tensor(name, shape, F32, kind="ExternalInput").ap()

    xT = din("xT", [1024, T])
    pp = din("pp", [NL, 128, NPP])
    w_in = din("w_in", [NL, 1024, IN_COLS])
    gla_w_up = din("gla_w_up", [NL, 16, 512])
    gla_b_alpha = din("gla_b_alpha", [NL, 512])
    lru_w_a = din("lru_w_a", [NL, 4, 256, 256])
    lru_w_x = din("lru_w_x", [NL, 4, 256, 256])
    w_br = [din("w_branch_a", [NL, 1024, 1024]), din("w_branch_b", [NL, 1024, 1024]), din("w_branch_c", [NL, 1024, 1024])]
    w_out = din("w_out", [NL, 1024, 1024])
    w_up = din("w_mlp_up", [NL, 1024, 4096])
    w_dn = din("w_mlp_down", [NL, 4096, 1024])
    yT = nc.dram_tensor("yT", [1024, T], F32, kind="ExternalOutput").ap()
    dbg_t = None
    if dbg:
        dbg_t = nc.dram_tensor("dbg", [6, 1024, T], F32, kind="ExternalOutput").ap()

    with ExitStack() as st:
        def sb(name, shape, dt):
            return st.enter_context(nc.sbuf_tensor(name, shape, dt))

        def psum(name, shape, dt):
            return st.enter_context(nc.psum_tensor(name, shape, dt))

        S = Sched(nc)
        xbuf_t = sb("xbuf", [128, KC * T], F32)
        xb = xbuf_t[:].rearrange("p (k t) -> p k t", k=KC)
        XB = [[Buf() for _ in range(TT)] for _ in range(KC)]
        xn_t = sb("xn", [128, KC * T], BF16)
        xn = xn_t[:].rearrange("p (k t) -> p k t", k=KC)
        XN = [[Buf() for _ in range(TT)] for _ in range(KC)]
        ob_t = sb("ob", [128, KC * T], BF16)
        ob = ob_t[:].rearrange("p (k t) -> p k t", k=KC)
        OB = [[Buf() for _ in range(TT)] for _ in range(KC)]
        wsl_t = sb("wsl", [128, NW * KC * 256], BF16)
        wsl = [wsl_t[:, i * KC * 256:(i + 1) * KC * 256].rearrange("p (k n) -> p k n", k=KC) for i in range(NW)]
        WB = [Buf() for _ in range(NW)]
        ppt = sb("ppt", [128, NL * NPP], F32)
        PPB = Buf()
        cst_bf = sb("cst_bf", [128, 128 * 5 + 256 + 2048 + 4 * 512 * 2], BF16)
        CB = Buf()
        cst_f = sb("cst_f", [128, 4], F32)
        ar_t = sb("arena", [128, 12200], F32)
        AR = Arena(ar_t[:])
        AR2 = Arena(xbuf_t[:])
        YT = [Buf() for _ in range(KC)]
        DBG = Buf()

        pbanks = [psum("pb%d" % i, [128, 512], F32) for i in range(7)]
        ptr_t = psum("ptr", [128, 1024], BF16)
        ptr = ptr_t[:]
        PTR = Buf()
        banks = [(pbanks[i][:], Buf()) for i in range(7)] + [(ptr_t[:].bitcast(F32), PTR)]
        P4, P5, P6 = banks[4][0], banks[5][0], banks[6][0]
        PB4, PB5, PB6 = banks[4][1], banks[5][1], banks[6][1]
        prot = Ring([])
        ALLB = list(range(8))

        def set_ring(idx):
            prot.items = [banks[i] for i in idx]
            prot.i = 0

        set_ring(ALLB)

        def getps():
            return prot.get()

        def mm(out, lhsT, rhs, start, stop, r, w, sgc=False):
            if sgc:
                S.op("pe", lambda e: e.matmul(out, lhsT, rhs, start=start, stop=stop, skip_group_check=True), r, w)
            else:
                S.op("pe", lambda e: e.matmul(out, lhsT, rhs, start=start, stop=stop), r, w)

        def act(out, in_, func, r, w, bias=None, scale=None):
            kw = {}
            if bias is not None:
                kw["bias"] = bias
            if scale is not None:
                kw["scale"] = scale
            S.op("act", lambda e: e.activation(out=out, in_=in_, func=func, **kw), r, w)

        def tto(eng, out, in0, in1, op, r, w):
            S.op(eng, lambda e: e.tensor_tensor(out=out, in0=in0, in1=in1, op=op), r, w)

        def stt(eng, out, in0, scalar, in1, op0, op1, r, w):
            S.op(eng, lambda e: e.scalar_tensor_tensor(out=out, in0=in0, scalar=scalar, in1=in1, op0=op0, op1=op1), r, w)

        def tsc(eng, out, in0, s1, s2, op0, op1, r, w):
            S.op(eng, lambda e: e.tensor_scalar(out=out, in0=in0, scalar1=s1, scalar2=s2, op0=op0, op1=op1), r, w)

        def tsm(eng, out, in0, s1, r, w):
            S.op(eng, lambda e: e.tensor_scalar_mul(out=out, in0=in0, scalar1=s1), r, w)

        def cp(eng, out, in_, r, w):
            if eng == "act":
                S.op("act", lambda e: e.activation(out=out, in_=in_, func=AF.Copy), r, w)
            else:
                S.op(eng, lambda e: e.tensor_copy(out=out, in_=in_), r, w)

        def memset(eng, ap, val, w):
            S.op(eng, lambda e: e.memset(ap, val), (), w)

        def recip(out, in_, r, w):
            S.op("dve", lambda e: e.reciprocal(out=out, in_=in_), r, w)

        def dma(q, out, in_, r, w):
            S.dma(q, lambda e: e.dma_start(out=out, in_=in_), r, w)

        def asel(ap, pattern, cmp, base, cm, w):
            S.op("pool", lambda e: e.affine_select(out=ap, in_=ap, pattern=pattern, compare_op=cmp, fill=0.0, base=base, channel_multiplier=cm), w, w)

        o = 0
        ident_bf = cst_bf[:, o:o + 128]; o += 128
        ones_bf = cst_bf[:, o:o + 128]; o += 128
        maskG = cst_bf[:, o:o + 128]; o += 128
        TriS = cst_bf[:, o:o + 128]; o += 128
        NU = cst_bf[:, o:o + 128]; o += 128
        Eb = cst_bf[:, o:o + 256].rearrange("p (b m) -> p b m", b=16); o += 256
        Xb = cst_bf[:, o:o + 2048].rearrange("p (a s) -> p a s", a=16); o += 2048
        DM = []
        NM = []
        for r_ in range(4):
            DM.append(cst_bf[:, o:o + 512]); o += 512
        for r_ in range(4):
            NM.append(cst_bf[:, o:o + 512]); o += 512
        nm_f = NM[0].tensor if False else None
        br_area = cst_bf[:, 7040 - 2048:7040].bitcast(F32)
        br_gr = Ring([br_area[:, 0:TW], br_area[:, TW:2 * TW]])
        eps_t = cst_f[:, 0:1]
        one_t = cst_f[:, 1:2]
        memset("dve", cst_f[:, 0:1], EPS, [CB])
        memset("dve", cst_f[:, 1:2], 1.0, [CB])
        tmpc = AR.f32(2048)
        TB = Buf()

        def mk(dst, np_, nf, val, pattern, cmp, base, cm, post=None):
            t = tmpc[0:np_, 0:nf]
            memset("pool", t, val, [TB])
            if pattern is not None:
                tv = t if len(pattern) == 1 else t.rearrange("p (a b) -> p a b", a=pattern[0][1])
                asel(tv, pattern, cmp, base, cm, [TB])
            if post is not None:
                post(t)
            cp("dve", dst, t, [TB], [CB])

        mk(ident_bf, 128, 128, 1.0, [[-1, 128]], ALU.is_equal, 0, 1)
        mk(ones_bf, 128, 128, 1.0, None, None, 0, 0)
        mk(maskG, 128, 128, 1.0, [[1, 128]], ALU.is_ge, 0, -1, post=lambda t: memset("pool", t[0:64, 64:128], 0.0, [TB]))
        tsm("dve", TriS, maskG, -1.0 / 16.0, [CB], [CB])
        mk(NU, 128, 128, -1.0, [[-1, 128]], ALU.is_ge, 0, 1)
        nones_bf = cst_bf[:, 640:768]
        mk(nones_bf, 128, 128, -1.0, None, None, 0, 0)
        mk(cst_bf[0:16, 896:896 + 2048], 16, 2048, -1.0, [[-1, 16], [0, 128]], ALU.is_gt, 0, 1)
        for r_ in range(4):
            mk(DM[r_], 128, 512, 1.0, [[1, 512]], ALU.is_gt, -128 * r_, -1)
        for l in range(NL):
            dma("sp", ppt[:, l * NPP:(l + 1) * NPP], pp[l], [], [PPB])

        def par(l, col):
            return ppt[:, l * NPP + col:l * NPP + col + 1]

        class WQ:
            def __init__(self):
                self.plan = []
                self.issued = 0
                self.consumed = 0

            def extend(self, items):
                self.plan.extend(items)

            def get(self, D=2):
                while self.issued < min(len(self.plan), self.consumed + 1 + D):
                    src, n = self.plan[self.issued]
                    slot = self.issued % NW
                    dma("pool", wsl[slot][:, :, 0:n], src.rearrange("(k p) n -> p k n", p=128), [], [WB[slot]])
                    self.issued += 1
                slot = self.consumed % NW
                self.consumed += 1
                return slot

        wq = WQ()

        def proj(slot, c0, n, tt, src=None, RB=None):
            if src is None:
                src, RB = xn, XN
            ps, pb = getps()
            for k in range(KC):
                mm(ps[0:n, :], wsl[slot][:, k, c0:c0 + n], src[:, k, tsl(tt)], k == 0, k == KC - 1, [WB[slot], RB[k][tt]], [pb])
            return ps, pb

        def dump(idx, src, RBs, bf):
            if not dbg:
                return
            for k in range(KC):
                dma("pool" if bf else "sp", dbg_t[idx, k * 128:(k + 1) * 128, :], src[:, k, :], RBs[k], [DBG])

        def rmsnorm(l, gcol):
            set_ring(ALLB)
            AR.reset()
            sqr = Ring([AR.bf16(TW) for _ in range(3)])
            srr = Ring([AR.f32(TW) for _ in range(2)])
            for tt in range(TT):
                ps, pb = getps()
                for k in range(KC):
                    sq, sqb = sqr.get()
                    act(sq, xb[:, k, tsl(tt)], AF.Square, [XB[k][tt]], [sqb])
                    mm(ps, ones_bf, sq, k == 0, k == KC - 1, [sqb, CB], [pb])
                sr, srb = srr.get()
                act(sr, ps, AF.Ln, [pb, CB], [srb], bias=eps_t, scale=1.0 / 1024.0)
                act(sr, sr, AF.Exp, [srb], [srb], scale=-0.5)
                for k in range(KC):
                    stt("dve", xn[:, k, tsl(tt)], xb[:, k, tsl(tt)], par(l, gcol + k), sr, ALU.mult, ALU.mult,
                        [XB[k][tt], srb, PPB], [XN[k][tt]])

        def spill_x():
            for k in range(KC):
                dma("sp", yT[k * 128:(k + 1) * 128, :], xb[:, k, :], XB[k], [YT[k]])

        def reload_x():
            for k in range(KC):
                dma("sp", xb[:, k, :], yT[k * 128:(k + 1) * 128, :], [YT[k]], XB[k])

        def gla(l):
            set_ring([0, 1, 2, 3, 6])
            AR.reset()
            AR2.reset()
            A = AR2
            adT = A.bf16(T); ADT = [Buf() for _ in range(TT)]
            wupa = A.bf16(512); WUPA = Buf()
            e1r = Ring([A.f32(TW) for _ in range(2)])
            Pr = Ring([A.bf16(TW) for _ in range(2)])
            ecr = Ring([A.f32(TW) for _ in range(2)])
            eir = Ring([A.f32(TW) for _ in range(2)])
            qd = A.bf16(T); QD = [Buf() for _ in range(TT)]
            ki = A.bf16(T); KI = [Buf() for _ in range(TT)]
            ki_tm = A.bf16(T); KITM = [Buf() for _ in range(TT)]
            v_tm = A.bf16(16 * 256); VTM = [Buf() for _ in range(8)]
            sg = [A.bf16(T), A.bf16(T)]; SG = [[Buf() for _ in range(TT)] for _ in range(2)]
            scr = Ring([A.bf16(128) for _ in range(3)])
            dec = A.f32(32); DEC = Buf()
            sqr = Ring([A.bf16(TW) for _ in range(2)])
            srr = Ring([A.f32(TW) for _ in range(2)])
            posb = [Ring([A.f32(TW), A.f32(TW)]), Ring([A.f32(TW), A.f32(TW)])]
            kvs = AR.f32(16 * 256); KVS = [Buf() for _ in range(16)]
            S_all = AR.f32(17 * 256); SAL = [Buf() for _ in range(17)]
            S_bfa = AR.bf16(16 * 256); SBA = [Buf() for _ in range(4)]
            for tt in range(TT):
                memset("dve", adT[0:33, tsl(tt)], 1.0, [ADT[tt]])
            memset("dve", wupa[0:33, :], 0.0, [WUPA])
            dma("pool", wupa[0:16, :], gla_w_up[l], [], [WUPA])
            dma("pool", wupa[32:33, :], gla_b_alpha[l:l + 1, :], [], [WUPA])
            plan = [(w_in[l][:, C_GD:C_GD + 16], 16)]
            for h in range(4):
                plan += [(w_in[l][:, C_GQ + h * 128:C_GQ + (h + 1) * 128], 128),
                         (w_in[l][:, C_GK + h * 128:C_GK + (h + 1) * 128], 128),
                         (w_in[l][:, C_GV + h * 256:C_GV + (h + 1) * 256], 256),
                         (w_in[l][:, C_GG + h * 256:C_GG + (h + 1) * 256], 256)]
            wq.extend(plan)
            s_ = wq.get(D=1)
            for tt in range(TT):
                ps, pb = proj(s_, 0, 16, tt)
                cp("act", adT[0:16, tsl(tt)], ps[0:16, :], [pb], [ADT[tt]])
            for h in range(4):
                s_q = wq.get(D=2)
                s_k = wq.get(D=2)
                for tt in range(TT + 1):
                    if tt < TT:
                        psy, pyb = getps()
                        for mi in range(4):
                            m = tt * 4 + mi
                            mm(psy[:, mi * 128:(mi + 1) * 128], adT[0:33, m * 128:(m + 1) * 128], wupa[0:33, h * 128:(h + 1) * 128],
                               True, True, [ADT[tt], WUPA], [pyb])
                        e1, e1b = e1r.get()
                        act(e1, psy, AF.Exp, [pyb], [e1b], scale=-1.0)
                        Pt, Ptb = Pr.get()
                        act(Pt, e1, AF.Ln, [e1b, CB], [Ptb], bias=one_t)
                        psq, pqb = proj(s_q, 0, 128, tt)
                        psk, pkb = proj(s_k, 0, 128, tt)
                        psc, pcb = getps()
                        for mi in range(4):
                            mm(psc[:, mi * 128:(mi + 1) * 128], Pt[:, mi * 128:(mi + 1) * 128], TriS, True, True, [Ptb, CB], [pcb])
                        ec, ecb = ecr.get()
                        ei, eib = eir.get()
                        act(ec, psc, AF.Exp, [pcb], [ecb])
                        act(ei, psc, AF.Exp, [pcb], [eib], scale=-1.0)
                        cp("dve", dec[:, tt * 8:(tt + 1) * 8], ec[:, 63::64], [ecb], [DEC])
                        stt("dve", qd[:, tsl(tt)], psq, 128.0 ** -0.5, ec, ALU.mult, ALU.mult, [pqb, ecb], [QD[tt]])
                        tto("dve", ki[:, tsl(tt)], psk, ei, ALU.mult, [pkb, eib], [KI[tt]])
                    if tt >= 1:
                        t2 = tt - 1
                        for mi in range(4):
                            m = t2 * 4 + mi
                            S.op("pe", (lambda o_, i_: (lambda e: e.transpose(o_, i_, ident_bf)))(ptr[:, mi * 128:(mi + 1) * 128], ki[:, m * 128:(m + 1) * 128]),
                                 [KI[t2], CB], [PTR])
                        cp("act", ki_tm[:, tsl(t2)], ptr[:, 0:512], [PTR], [KITM[t2]])
                s_v = wq.get(D=2)
                for m2 in range(8):
                    ps, pb = getps()
                    for mj in range(2):
                        m = m2 * 2 + mj
                        for k in range(KC):
                            mm(ps[:, mj * 256:(mj + 1) * 256], xn[:, k, m * 128:(m + 1) * 128], wsl[s_v][:, k, 0:256],
                               k == 0, k == KC - 1, [XN[k][m // 4], WB[s_v]], [pb])
                    cp("act", v_tm[:, m2 * 512:(m2 + 1) * 512], ps, [pb], [VTM[m2]])
                s_g = wq.get(D=2)
                for jb in range(2):
                    for tt in range(TT):
                        ps, pb = proj(s_g, jb * 128, 128, tt)
                        act(sg[jb][:, tsl(tt)], ps, AF.Silu, [pb], [SG[jb][tt]])
                pend = []

                def normrest(tt, pcs, h=h):
                    pn, pnb = getps()
                    for jb in range(2):
                        sq, sqb = sqr.get()
                        act(sq, pcs[jb][0], AF.Square, [pcs[jb][1]], [sqb])
                        mm(pn, ones_bf, sq, jb == 0, jb == 1, [sqb, CB], [pnb])
                    sr, srb = srr.get()
                    act(sr, pn, AF.Ln, [pnb, CB], [srb], bias=eps_t, scale=1.0 / 256.0)
                    act(sr, sr, AF.Exp, [srb], [srb], scale=-0.5)
                    for jb in range(2):
                        pc, pcb_ = pcs[jb]
                        tto("dve", pc, pc, sr, ALU.mult, [pcb_, srb], [pcb_])
                        stt("dve", ob[:, h * 2 + jb, tsl(tt)], pc, par(l, P_GLAN + jb), sg[jb][:, tsl(tt)], ALU.mult, ALU.mult,
                            [pcb_, PPB, SG[jb][tt]], [OB[h * 2 + jb][tt]])

                for hs in range(2):
                    for ci in range(16):
                        c = hs * 16 + ci
                        m = c // 2
                        half = c % 2
                        pkv, pkb = getps()
                        mm(pkv[:, 0:256], ki_tm[half * 64:(half + 1) * 64, m * 128:(m + 1) * 128],
                           v_tm[half * 64:(half + 1) * 64, m * 256:(m + 1) * 256], True, True, [KITM[m // 4], VTM[m // 2]], [pkb])
                        act(kvs[:, ci * 256:(ci + 1) * 256], pkv[:, 0:256], AF.Identity, [pkb, DEC], [KVS[ci]], scale=dec[:, c:c + 1])
                    if hs == 0:
                        memset("dve", S_all[:, 0:256], 0.0, [SAL[0]])
                    else:
                        cp("dve", S_all[:, 0:256], S_all[:, 16 * 256:17 * 256], [SAL[16]], [SAL[0]])
                    for ci in range(16):
                        c = hs * 16 + ci
                        stt("dve", S_all[:, (ci + 1) * 256:(ci + 2) * 256], S_all[:, ci * 256:(ci + 1) * 256], dec[:, c:c + 1],
                            kvs[:, ci * 256:(ci + 1) * 256], ALU.mult, ALU.add, [SAL[ci], DEC, KVS[ci]], [SAL[ci + 1]])
                        if ci % 4 == 3:
                            g4 = ci // 4
                            cp("act", S_bfa[:, g4 * 1024:(g4 + 1) * 1024], S_all[:, g4 * 1024:(g4 + 1) * 1024],
                               [SAL[g4 * 4 + j] for j in range(4)], [SBA[g4]])
                    for tl in range(2):
                        tt = hs * 2 + tl
                        po = [P4, P5]
                        POB = [PB4, PB5]
                        scl = {}
                        for it in range(5):
                            if it < 4:
                                mi = it
                                m = tt * 4 + mi
                                pss, psb = getps()
                                mm(pss[:, 0:128], ki[:, m * 128:(m + 1) * 128], qd[:, m * 128:(m + 1) * 128], True, True, [KI[tt], QD[tt]], [psb])
                                sc, scb = scr.get()
                                tto("dve", sc, pss[:, 0:128], maskG, ALU.mult, [psb, CB], [scb])
                                scl[mi] = (sc, scb)
                            if it >= 1:
                                mi = it - 1
                                m = tt * 4 + mi
                                sc, scb = scl.pop(mi)
                                for half in range(2):
                                    c = 2 * m + half
                                    ci = c - hs * 16
                                    for jb in range(2):
                                        col = mi * 128 + half * 64
                                        mm(po[jb][:, col:col + 64], S_bfa[:, ci * 256 + jb * 128:ci * 256 + (jb + 1) * 128], qd[:, c * 64:(c + 1) * 64],
                                           True, False, [SBA[ci // 4], QD[tt]], [POB[jb]])
                                        mm(po[jb][:, col:col + 64], v_tm[:, m * 256 + jb * 128:m * 256 + (jb + 1) * 128], sc[:, half * 64:(half + 1) * 64],
                                           False, True, [VTM[m // 2], scb], [POB[jb]])
                        if pend:
                            normrest(*pend.pop())
                        pcs = []
                        for jb in range(2):
                            pc, pcb_ = posb[jb].get()
                            cp("act", pc, po[jb], [POB[jb]], [pcb_])
                            pcs.append((pc, pcb_))
                        pend.append((tt, pcs))
                while pend:
                    normrest(*pend.pop())

        def branch(l, b):
            set_ring(ALLB)
            gr = br_gr
            plan = []
            for nbp in range(4):
                plan += [(w_br[b][l][:, nbp * 256:(nbp + 1) * 256], 256),
                         (w_in[l][:, C_GATE + b * 1024 + nbp * 256:C_GATE + b * 1024 + (nbp + 1) * 256], 256)]
            wq.extend(plan)
            for nbp in range(4):
                s1 = wq.get(D=2)
                s2 = wq.get(D=2)
                for nbi in range(2):
                    nb = nbp * 2 + nbi
                    for tt in range(TT):
                        ps1, p1b = proj(s1, nbi * 128, 128, tt, ob, OB)
                        ps2, p2b = proj(s2, nbi * 128, 128, tt)
                        g, gb = gr.get()
                        act(g, ps2, AF.Sigmoid, [p2b, PPB], [gb], bias=par(l, P_BG + b * 8 + nb))
                        if b == 0:
                            tto("dve", xb[:, nb, tsl(tt)], ps1, g, ALU.mult, [p1b, gb], [XB[nb][tt]])
                        else:
                            tto("dve", g, ps1, g, ALU.mult, [p1b, gb], [gb])
                            tto("dve", xb[:, nb, tsl(tt)], xb[:, nb, tsl(tt)], g, ALU.add, [gb, XB[nb][tt]], [XB[nb][tt]])

        def lru(l):
            set_ring(ALLB)
            AR.reset()
            A = AR
            cl = A.f32(8); CL = Buf()
            tmp8 = A.f32(8)
            lxbf = [A.bf16(T + 4), A.bf16(T + 4)]; LXB = [[Buf() for _ in range(TT)] for _ in range(2)]
            dg = [[A.bf16(128) for _ in range(4)] for _ in range(2)]; DG = Buf()
            wab = A.bf16(512).rearrange("p (i j) -> p i j", i=2); wxb = A.bf16(512).rearrange("p (i j) -> p i j", i=2); WAB = Buf(); WXB = Buf()
            xcf = [Ring([A.f32(TW) for _ in range(1)]) for _ in range(2)]
            xcb = [Ring([A.bf16(TW) for _ in range(1)]) for _ in range(2)]
            rr = Ring([A.f32(TW) for _ in range(2)])
            igr = Ring([A.f32(TW) for _ in range(2)])
            n2r = Ring([A.f32(TW) for _ in range(2)])
            hr = [Ring([A.f32(TW) for _ in range(2)]) for _ in range(2)]
            glr = Ring([A.f32(TW) for _ in range(2)])
            g2r = Ring([A.f32(TW) for _ in range(2)])
            act(tmp8, ppt[:, l * NPP + P_LAM:l * NPP + P_LAM + 8], AF.Exp, [PPB], [CL], scale=-1.0)
            act(tmp8, tmp8, AF.Ln, [CL, CB], [CL], bias=one_t)
            tsm("dve", cl, tmp8, -8.0, [CL], [CL])
            plan = []
            for hb in range(4):
                plan += [(w_in[l][:, C_LX + hb * 256:C_LX + (hb + 1) * 256], 256), (w_in[l][:, C_LG + hb * 256:C_LG + (hb + 1) * 256], 256)]
            wq.extend(plan)
            for hb in range(4):
                slx = wq.get(D=2)
                slg = wq.get(D=2)
                dma("pool", wab, lru_w_a[l, hb].rearrange("(i p) j -> p i j", p=128), [], [WAB])
                dma("pool", wxb, lru_w_x[l, hb].rearrange("(i p) j -> p i j", p=128), [], [WXB])
                for cbl in range(2):
                    memset("dve", lxbf[cbl][:, 0:3], 0.0, [LXB[cbl][0]])
                    for w_ in range(4):
                        tsm("dve", dg[cbl][w_], ident_bf, par(l, P_CW + (hb * 2 + cbl) * 4 + w_), [CB, PPB], [DG])
                hprev = [None, None]
                for tt in range(TT):
                    for cbl in range(2):
                        ps, pb = proj(slx, cbl * 128, 128, tt)
                        cp("act", lxbf[cbl][:, 3 + tt * TW:3 + (tt + 1) * TW], ps, [pb], [LXB[cbl][tt]])
                    xf = []
                    xh = []
                    for cbl in range(2):
                        ps, pb = getps()
                        rd = [LXB[cbl][tt], DG] + ([LXB[cbl][tt - 1]] if tt > 0 else [])
                        for w_ in range(4):
                            mm(ps, dg[cbl][w_], lxbf[cbl][:, tt * TW + w_:tt * TW + w_ + TW], w_ == 0, w_ == 3, rd, [pb])
                        f_, fb = xcf[cbl].get()
                        act(f_, ps, AF.Identity, [pb, PPB], [fb], bias=par(l, P_CB + hb * 2 + cbl))
                        h_, hb_ = xcb[cbl].get()
                        cp("dve", h_, f_, [fb], [hb_])
                        xf.append((f_, fb))
                        xh.append((h_, hb_))
                    rs, igs, gls, g2s, n2s, hhs = [], [], [], [], [], []
                    for jb in range(2):
                        c = hb * 2 + jb
                        psr, prb = getps()
                        for ib in range(2):
                            mm(psr, wab[:, ib, jb * 128:(jb + 1) * 128], xh[ib][0], ib == 0, ib == 1, [WAB, xh[ib][1]], [prb])
                        psi, pib = getps()
                        for ib in range(2):
                            mm(psi, wxb[:, ib, jb * 128:(jb + 1) * 128], xh[ib][0], ib == 0, ib == 1, [WXB, xh[ib][1]], [pib])
                        r_, rb = rr.get()
                        act(r_, psr, AF.Sigmoid, [prb, PPB], [rb], bias=par(l, P_BA + c))
                        ig, igb = igr.get()
                        act(ig, psi, AF.Sigmoid, [pib, PPB], [igb], bias=par(l, P_BX + c))
                        rs.append((r_, rb))
                        igs.append((ig, igb))
                    for jb in range(2):
                        c = hb * 2 + jb
                        a_, ab = rs[jb]
                        act(a_, a_, AF.Exp, [ab, CL], [ab], scale=cl[:, c:c + 1])
                    for jb in range(2):
                        psg, pgb = proj(slg, jb * 128, 128, tt)
                        gl, glb = glr.get()
                        act(gl, psg, AF.Identity, [pgb], [glb])
                        gls.append((gl, glb))
                    for jb in range(2):
                        a_, ab = rs[jb]
                        n2, n2b = n2r.get()
                        stt("dve", n2, a_, -1.0, a_, ALU.mult, ALU.mult, [ab], [n2b])
                        n2s.append((n2, n2b))
                        gl, glb = gls[jb]
                        g2, g2b = g2r.get()
                        stt("dve", g2, gl, 0.044715, gl, ALU.mult, ALU.mult, [glb], [g2b])
                        stt("dve", g2, g2, 1.0, gl, ALU.add, ALU.mult, [g2b, glb], [g2b])
                        g2s.append((g2, g2b))
                    for jb in range(2):
                        n2, n2b = n2s[jb]
                        act(n2, n2, AF.Sqrt, [n2b, CB], [n2b], bias=one_t)
                    for jb in range(2):
                        g2, g2b = g2s[jb]
                        act(g2, g2, AF.Sigmoid, [g2b], [g2b], scale=1.5957691216057308)
                    for jb in range(2):
                        c = hb * 2 + jb
                        a_, ab = rs[jb]
                        ig, igb = igs[jb]
                        n2, n2b = n2s[jb]
                        gl, glb = gls[jb]
                        g2, g2b = g2s[jb]
                        u_, ub = ig, igb
                        tto("dve", u_, n2, ig, ALU.mult, [n2b, igb], [ub])
                        tto("dve", u_, u_, xf[jb][0], ALU.mult, [ub, xf[jb][1]], [ub])
                        hh, hhb = hr[jb].get()
                        if hprev[jb] is None:
                            S.op("dve", (lambda o_, a0, u0: (lambda e: e.tensor_tensor_scan(out=o_, data0=a0, data1=u0, initial=0.0, op0=ALU.mult, op1=ALU.add)))(hh, a_, u_),
                                 [ab, ub], [hhb])
                        else:
                            hp, hpb = hprev[jb]
                            S.op("dve", (lambda o_, a0, u0, i0: (lambda e: e.tensor_tensor_scan(out=o_, data0=a0, data1=u0, initial=i0, op0=ALU.mult, op1=ALU.add)))(hh, a_, u_, hp[:, TW - 1:TW]),
                                 [ab, ub, hpb], [hhb])
                        hprev[jb] = (hh, hhb)
                        tto("dve", gl, gl, hh, ALU.mult, [glb, hhb], [glb])
                        tto("dve", ob[:, c, tsl(tt)], gl, g2, ALU.mult, [glb, g2b], [OB[c][tt]])

        def sbattn(l):
            set_ring([0, 1, 2, 3, 4, 7])
            AR.reset()
            A = AR
            Lkr = Ring([A.bf16(TW) for _ in range(5)])
            Rbr = Ring([A.bf16(TW) for _ in range(5)])
            R32 = A.f32(TW); R32B = Buf()
            qn = A.bf16(T); QN = [Buf() for _ in range(TT)]
            kn = A.bf16(T); KN = [Buf() for _ in range(TT)]
            v_tm = A.bf16(T); VT = [Buf() for _ in range(4)]
            Er = Ring([A.f32(TW) for _ in range(2)])
            wr = Ring([A.bf16(TW) for _ in range(4)])
            sqr = Ring([A.bf16(TW) for _ in range(2)])
            srr = Ring([A.f32(TW) for _ in range(2)])
            gq2 = A.f32(2); GQ = Buf()
            tsm("dve", gq2[:, 0:1], par(l, P_SQG), 128.0 ** -0.5, [PPB], [GQ])
            cp("dve", gq2[:, 1:2], par(l, P_SKG), [PPB], [GQ])
            plan = []
            for h in range(8):
                plan += [(w_in[l][:, C_SQ + h * 128:C_SQ + (h + 1) * 128], 128), (w_in[l][:, C_SK + h * 128:C_SK + (h + 1) * 128], 128),
                         (w_in[l][:, C_SV + h * 128:C_SV + (h + 1) * 128], 128)]
            wq.extend(plan)
            for h in range(8):
                slots_qk = [wq.get(D=2), wq.get(D=2)]
                items = [(which, tt) for which in range(2) for tt in range(TT)]
                stA = {}

                def stageA(i):
                    which, tt = items[i]
                    ps, pb = proj(slots_qk[which], 0, 128, tt)
                    sq, sqb = sqr.get()
                    act(sq, ps, AF.Square, [pb], [sqb])
                    stA[i] = (ps, pb, sq, sqb)

                def stageB(i):
                    which, tt = items[i]
                    dst, DBs = ((qn, QN), (kn, KN))[which]
                    ps, pb, sq, sqb = stA.pop(i)
                    pn, pnb = getps()
                    mm(pn, ones_bf, sq, True, True, [sqb, CB], [pnb])
                    sr, srb = srr.get()
                    act(sr, pn, AF.Ln, [pnb, CB], [srb], bias=eps_t, scale=1.0 / 128.0)
                    act(sr, sr, AF.Exp, [srb], [srb], scale=-0.5)
                    stt("dve", dst[:, tsl(tt)], ps, gq2[:, which:which + 1], sr, ALU.mult, ALU.mult, [pb, srb, GQ], [DBs[tt]])

                for i in range(len(items) + 1):
                    if i < len(items):
                        stageA(i)
                    if i >= 1:
                        stageB(i - 1)
                s_v = wq.get(D=2)
                for m4 in range(4):
                    ps, pb = getps()
                    for mi in range(4):
                        m = m4 * 4 + mi
                        for k in range(KC):
                            mm(ps[:, mi * 128:(mi + 1) * 128], xn[:, k, m * 128:(m + 1) * 128], wsl[s_v][:, k, 0:128],
                               k == 0, k == KC - 1, [XN[k][m4], WB[s_v]], [pb])
                    cp("act", v_tm[:, m4 * 512:(m4 + 1) * 512], ps, [pb], [VT[m4]])
                L1, L2 = 2, 2
                blocks = [(qt, a) for qt in range(TT) for a in reversed(range(4 * (qt + 1)))]
                stt_ = {}
                wl = {}
                curR = [None]

                def S1(qt, a):
                    nk = 4 * (qt + 1)
                    psz, pzb = getps()
                    mm(psz, kn[:, a * 128:(a + 1) * 128], qn[:, tsl(qt)], True, True, [KN[a // 4], QN[qt]], [pzb])
                    E, Eb_ = Er.get()
                    act(E, psz, AF.Exp, [pzb], [Eb_])
                    La, Lab = Lkr.get()
                    act(La, E, AF.Ln, [Eb_, CB], [Lab], bias=one_t)
                    if a >= 4 * qt:
                        tto("dve", La, La, DM[a - 4 * qt], ALU.mult, [Lab, CB], [Lab])
                    myR = None if a == nk - 1 else curR[0]
                    if a > 0:
                        if a == nk - 1:
                            cp("dve", R32, La, [Lab], [R32B])
                        else:
                            tto("dve", R32, R32, La, ALU.add, [R32B, Lab], [R32B])
                        Rb, Rbb = Rbr.get()
                        cp("dve", Rb, R32, [R32B], [Rbb])
                        curR[0] = (Rb, Rbb)
                    stt_[(qt, a)] = (psz, pzb, La, Lab, myR)

                def S2(qt, a):
                    psz, pzb, La, Lab, myR = stt_.pop((qt, a))
                    mm(psz, NU, La, False, myR is None, [CB, Lab], [pzb], sgc=True)
                    if myR is not None:
                        mm(psz, nones_bf, myR[0], False, True, [CB, myR[1]], [pzb], sgc=True)
                    w_, wb_ = wr.get()
                    act(w_, psz, AF.Exp, [pzb], [wb_])
                    if a >= 4 * qt:
                        tto("dve", w_, w_, DM[a - 4 * qt], ALU.mult, [wb_, CB], [wb_])
                    wl[(qt, a)] = (w_, wb_)

                def S3(qt, a):
                    nk = 4 * (qt + 1)
                    po, pob = (P5, PB5) if qt % 2 == 0 else (P6, PB6)
                    w_, wb_ = wl.pop((qt, a))
                    mm(po, v_tm[:, a * 128:(a + 1) * 128], w_, a == nk - 1, a == 0, [VT[a // 4], wb_], [pob])
                    if a == 0:
                        cp("dve", ob[:, h, tsl(qt)], po, [pob], [OB[h][qt]])

                nb_ = len(blocks)
                for it in range(nb_ + L1 + L2):
                    if it < nb_:
                        S1(*blocks[it])
                    if L1 <= it < nb_ + L1:
                        S2(*blocks[it - L1])
                    if it >= L1 + L2:
                        S3(*blocks[it - L1 - L2])

        def wout(l):
            set_ring(ALLB)
            AR.reset()
            for nb in range(KC):
                for tt in range(TT):
                    cp("act" if (nb + tt) % 2 == 0 else "dve", ob[:, nb, tsl(tt)], xb[:, nb, tsl(tt)], [XB[nb][tt]], [OB[nb][tt]])
            dump(4, ob, OB, True)
            reload_x()
            wq.extend([(w_out[l][:, i * 256:(i + 1) * 256], 256) for i in range(4)])
            for i in range(4):
                s_ = wq.get(D=2)
                for nbi in range(2):
                    nb = i * 2 + nbi
                    for tt in range(TT):
                        ps, pb = proj(s_, nbi * 128, 128, tt, ob, OB)
                        tto("dve", xb[:, nb, tsl(tt)], ps, xb[:, nb, tsl(tt)], ALU.add, [pb, XB[nb][tt]], [XB[nb][tt]])

        def mlp(l):
            rmsnorm(l, P_GMLP)
            rr = Ring([AR.f32(TW) for _ in range(3)])
            plan = []
            for fg in range(4):
                plan += [(w_up[l][:, fg * 1024 + i * 256:fg * 1024 + (i + 1) * 256], 256) for i in range(4)]
                plan += [(w_dn[l][fg * 1024:(fg + 1) * 1024, i * 256:(i + 1) * 256], 256) for i in range(4)]
            wq.extend(plan)
            for fg in range(4):
                for i in range(4):
                    s_ = wq.get(D=3)
                    for fbi in range(2):
                        fb = i * 2 + fbi
                        for tt in range(TT):
                            ps, pb = proj(s_, fbi * 128, 128, tt)
                            r_, rb = rr.get()
                            act(r_, ps, AF.Relu, [pb], [rb])
                            tto("dve", ob[:, fb, tsl(tt)], r_, r_, ALU.mult, [rb], [OB[fb][tt]])
                for i in range(4):
                    s_ = wq.get(D=3)
                    for nbi in range(2):
                        nb = i * 2 + nbi
                        for tt in range(TT):
                            ps, pb = proj(s_, nbi * 128, 128, tt, ob, OB)
                            tto("dve", xb[:, nb, tsl(tt)], ps, xb[:, nb, tsl(tt)], ALU.add, [pb, XB[nb][tt]], [XB[nb][tt]])

        S.barrier()
        for k in range(KC):
            dma("sp", xb[:, k, :], xT[k * 128:(k + 1) * 128, :], [], XB[k])
        for l in range(NL):
            rmsnorm(l, P_GMIX)
            if l == 0:
                dump(0, xn, XN, True)
            spill_x()
            S.barrier()
            gla(l)
            if l == 0:
                dump(1, ob, OB, True)
            S.barrier()
            branch(l, 0)
            lru(l)
            if l == 0:
                dump(2, ob, OB, True)
            S.barrier()
            branch(l, 1)
            sbattn(l)
            if l == 0:
                dump(3, ob, OB, True)
            S.barrier()
            branch(l, 2)
            wout(l)
            if l == 0:
                dump(5, xb, XB, False)
            mlp(l)
            S.barrier()
        for k in range(KC):
            dma("sp", yT[k * 128:(k + 1) * 128, :], xb[:, k, :], XB[k], [YT[k]])
        S.barrier()
        S.emit(st)
        print("instructions:", S.ninst, {e: len(S.streams[e]) for e in ENGS})
    return nc


def pack_params(inp, layers):
    def fm(v, nb):
        return np.ascontiguousarray(np.asarray(v, np.float32).reshape(nb, 128).T)

    out = np.zeros((len(layers), 128, NPP), np.float32)
    for i, l in enumerate(layers):
        out[i, :, P_GMIX:P_GMIX + 8] = fm(inp["norm_mix_g"][l], 8)
        out[i, :, P_GMLP:P_GMLP + 8] = fm(inp["norm_mlp_g"][l], 8)
        out[i, :, P_GLAN:P_GLAN + 2] = fm(inp["gla_norm_g"][l], 2)
        cw = np.asarray(inp["lru_conv_w"][l], np.float32)
        for cb in range(8):
            for w_ in range(4):
                out[i, :, P_CW + cb * 4 + w_] = cw[w_, cb * 128:(cb + 1) * 128]
        out[i, :, P_CB:P_CB + 8] = fm(inp["lru_conv_b"][l], 8)
        out[i, :, P_BA:P_BA + 8] = fm(inp["lru_b_a"][l], 8)
        out[i, :, P_BX:P_BX + 8] = fm(inp["lru_b_x"][l], 8)
        out[i, :, P_LAM:P_LAM + 8] = fm(inp["lru_lambda"][l], 8)
        out[i, :, P_SQG] = np.asarray(inp["sb_q_norm_g"][l], np.float32)
        out[i, :, P_SKG] = np.asarray(inp["sb_k_norm_g"][l], np.float32)
        out[i, :, P_BG:P_BG + 24] = fm(inp["b_gate"][l], 24)
    return out


_NC_CACHE = {}
WNAMES = ["w_in", "gla_w_up", "gla_b_alpha", "lru_w_a", "lru_w_x", "w_branch_a", "w_branch_b", "w_branch_c", "w_out", "w_mlp_up", "w_mlp_down"]


def _get_nc(NL, dbg=False):
    key = (NL, dbg)
    if key not in _NC_CACHE:
        _NC_CACHE[key] = build(NL, dbg)
    return _NC_CACHE[key]


def run_layers(inp, xT_list, layers, dbg=False, cores=8):
    nc = _get_nc(len(layers), dbg)
    ppk = pack_params(inp, layers)
    shared = {"pp": ppk}
    for n in WNAMES:
        shared[n] = np.ascontiguousarray(np.asarray(inp[n], np.float32)[layers[0]:layers[-1] + 1])
    in_maps = []
    for c in range(cores):
        d = dict(shared)
        d["xT"] = xT_list[c]
        in_maps.append(d)
    res = run_bass_kernel_spmd(nc, in_maps, core_ids=list(range(cores)))
    return res.results


def kernel(**inputs):
    x = np.asarray(inputs["x"], np.float32)
    B = x.shape[0]
    xT = [np.ascontiguousarray(x[b].T) for b in range(B)]
    if FUSED:
        res = run_layers(inputs, xT, list(range(DEPTH)))
        xT = [r["yT"] for r in res]
    else:
        for l in range(DEPTH):
            res = run_layers(inputs, xT, [l])
            xT = [np.ascontiguousarray(r["yT"]) for r in res]
    return np.stack([np.asarray(t).T for t in xT], axis=0).astype(np.float32)
```

```python
from contextlib import ExitStack
import numpy as np
import concourse.bass as bass
import concourse.mybir as mybir
from concourse.bass_utils import run_bass_kernel_spmd

F32 = mybir.dt.float32
BF16 = mybir.dt.bfloat16
AF = mybir.ActivationFunctionType
ALU = mybir.AluOpType

FUSED = True
DEPTH = 4
T = 2048
TT = 4
TW = 512
KC = 8
NW = 4
EPS = 1e-6
C_GQ, C_GK, C_GV, C_GG, C_GD, C_LX, C_LG, C_SQ, C_SK, C_SV, C_GATE = 0, 512, 1024, 2048, 3072, 3088, 4112, 5136, 6160, 7184, 8208
IN_COLS = 11280
P_GMIX, P_GMLP, P_GLAN, P_CW, P_CB, P_BA, P_BX, P_LAM, P_SQG, P_SKG, P_BG, NPP = 0, 8, 16, 18, 50, 58, 66, 74, 82, 83, 84, 108


class Buf:
    __slots__ = ("w", "r")

    def __init__(self):
        self.w = None
        self.r = {}


ENGS = ("pe", "act", "dve", "pool", "sp")
EPOCH = 20000
DMA_EPOCH = 1000


class Sched:
    def __init__(self, nc, n_dma_ch=8):
        self.nc = nc
        self.streams = {e: [] for e in ENGS}
        self.cnt = {e: 0 for e in ENGS}
        self.epoch = {e: 0 for e in ENGS}
        self.waited = {e: {} for e in ENGS}
        self.n_dma_ch = n_dma_ch
        self.dcnt = [0] * n_dma_ch
        self.depoch = [0] * n_dma_ch
        self.dnext = 0
        self.semkeys = []
        self._seen = set()
        self.latest = {}
        self.ninst = 0

    def _key(self, k):
        if k not in self._seen:
            self._seen.add(k)
            self.semkeys.append(k)
        return k

    def _filter(self, eng, need):
        out = []
        wd = self.waited[eng]
        for s, v in need.items():
            if eng == "pe" and s[0] == "pe":
                continue
            if wd.get(s, 0) < v:
                wd[s] = v
                out.append((s, v))
        return out

    def _deps(self, eng, reads, writes):
        need = {}
        for b in reads:
            if b.w is not None:
                s, v = b.w
                if need.get(s, 0) < v:
                    need[s] = v
        for b in writes:
            if b.w is not None:
                s, v = b.w
                if need.get(s, 0) < v:
                    need[s] = v
            for s, v in b.r.items():
                if need.get(s, 0) < v:
                    need[s] = v
        return self._filter(eng, need)

    def _mark(self, ev, reads, writes):
        s, v = ev
        self.latest[s] = v
        for b in reads:
            if b.r.get(s, 0) < v:
                b.r[s] = v
        for b in writes:
            b.w = ev
            b.r = {}

    def op(self, eng, fn, reads=(), writes=()):
        deps = self._deps(eng, reads, writes)
        if self.cnt[eng] >= EPOCH:
            self.epoch[eng] += 1
            self.cnt[eng] = 0
        self.cnt[eng] += 1
        key = self._key((eng, self.epoch[eng]))
        self.streams[eng].append((deps, fn, key, 1))
        self._mark((key, self.cnt[eng]), reads, writes)
        self.ninst += 1

    def dma(self, queue, fn, reads=(), writes=()):
        deps = self._deps(queue, reads, writes)
        ch = self.dnext
        self.dnext = (self.dnext + 1) % self.n_dma_ch
        if self.dcnt[ch] >= DMA_EPOCH:
            self.depoch[ch] += 1
            self.dcnt[ch] = 0
        self.dcnt[ch] += 1
        key = self._key(("dma%d" % ch, self.depoch[ch]))
        self.streams[queue].append((deps, fn, key, 16))
        self._mark((key, 16 * self.dcnt[ch]), reads, writes)
        self.ninst += 1

    def barrier(self):
        for eng in ENGS:
            deps = self._filter(eng, dict(self.latest))
            if deps:
                self.streams[eng].append((deps, None, None, 0))

    def emit(self, stack):
        nc = self.nc
        sems = {}
        for k in self.semkeys:
            sems[k] = stack.enter_context(nc.semaphore("s_%s_%d" % k))
        block = stack.enter_context(nc.Block())
        streams = self.streams

        def run(engobj, lst):
            for deps, fn, key, inc in lst:
                for s, v in deps:
                    engobj.wait_ge(sems[s], v)
                if fn is not None:
                    fn(engobj).then_inc(sems[key], inc)

        @block.tensor
        def _(e):
            run(e, streams["pe"])

        @block.scalar
        def _(e):
            run(e, streams["act"])

        @block.vector
        def _(e):
            run(e, streams["dve"])

        @block.gpsimd
        def _(e):
            run(e, streams["pool"])

        @block.sync
        def _(e):
            run(e, streams["sp"])


class Arena:
    def __init__(self, ap):
        self.ap = ap
        self.n = ap.shape[1]
        self.off = 0

    def f32(self, n):
        a = self.ap[:, self.off:self.off + n]
        self.off += n
        assert self.off <= self.n, ("arena overflow", self.off, self.n)
        return a

    def bf16(self, n):
        nn = (n + 1) // 2
        a = self.ap[:, self.off:self.off + nn].bitcast(BF16)
        self.off += nn
        assert self.off <= self.n, ("arena overflow", self.off, self.n)
        return a

    def reset(self):
        self.off = 0


class Ring:
    def __init__(self, aps):
        self.items = [(a, Buf()) for a in aps]
        self.i = 0

    def get(self):
        it = self.items[self.i]
        self.i = (self.i + 1) % len(self.items)
        return it


def tsl(tt):
    return slice(tt * TW, (tt + 1) * TW)


def build(NL, dbg=False):
    nc = bass.Bass("TRN2", target_bir_lowering=False)

    def din(name, shape):
        return nc.dram_tensor(name, shape, F32, kind="ExternalInput").ap()

    xT = din("xT", [1024, T])
    pp = din("pp", [NL, 128, NPP])
    w_in = din("w_in", [NL, 1024, IN_COLS])
    gla_w_up = din("gla_w_up", [NL, 16, 512])
    gla_b_alpha = din("gla_b_alpha", [NL, 512])
    lru_w_a = din("lru_w_a", [NL, 4, 256, 256])
    lru_w_x = din("lru_w_x", [NL, 4, 256, 256])
    w_br = [din("w_branch_a", [NL, 1024, 1024]), din("w_branch_b", [NL, 1024, 1024]), din("w_branch_c", [NL, 1024, 1024])]
    w_out = din("w_out", [NL, 1024, 1024])
    w_up = din("w_mlp_up", [NL, 1024, 4096])
    w_dn = din("w_mlp_down", [NL, 4096, 1024])
    yT = nc.dram_tensor("yT", [1024, T], F32, kind="ExternalOutput").ap()
    dbg_t = None
    if dbg:
        dbg_t = nc.dram_tensor("dbg", [6, 1024, T], F32, kind="ExternalOutput").ap()

    with ExitStack() as st:
        def sb(name, shape, dt):
            return st.enter_context(nc.sbuf_tensor(name, shape, dt))

        def psum(name, shape, dt):
            return st.enter_context(nc.psum_tensor(name, shape, dt))

        S = Sched(nc)
        xbuf_t = sb("xbuf", [128, KC * T], F32)
        xb = xbuf_t[:].rearrange("p (k t) -> p k t", k=KC)
        XB = [[Buf() for _ in range(TT)] for _ in range(KC)]
        xn_t = sb("xn", [128, KC * T], BF16)
        xn = xn_t[:].rearrange("p (k t) -> p k t", k=KC)
        XN = [[Buf() for _ in range(TT)] for _ in range(KC)]
        ob_t = sb("ob", [128, KC * T], BF16)
        ob = ob_t[:].rearrange("p (k t) -> p k t", k=KC)
        OB = [[Buf() for _ in range(TT)] for _ in range(KC)]
        wsl_t = sb("wsl", [128, NW * KC * 256], BF16)
        wsl = [wsl_t[:, i * KC * 256:(i + 1) * KC * 256].rearrange("p (k n) -> p k n", k=KC) for i in range(NW)]
        WB = [Buf() for _ in range(NW)]
        ppt = sb("ppt", [128, NL * NPP], F32)
        PPB = Buf()
        cst_bf = sb("cst_bf", [128, 128 * 5 + 256 + 2048 + 4 * 512 * 2], BF16)
        CB = Buf()
        cst_f = sb("cst_f", [128, 4], F32)
        ar_t = sb("arena", [128, 12200], F32)
        AR = Arena(ar_t[:])
        AR2 = Arena(xbuf_t[:])
        YT = [Buf() for _ in range(KC)]
        DBG = Buf()

        pbanks = [psum("pb%d" % i, [128, 512], F32) for i in range(7)]
        ptr_t = psum("ptr", [128, 1024], BF16)
        ptr = ptr_t[:]
        PTR = Buf()
        banks = [(pbanks[i][:], Buf()) for i in range(7)] + [(ptr_t[:].bitcast(F32), PTR)]
        P4, P5, P6 = banks[4][0], banks[5][0], banks[6][0]
        PB4, PB5, PB6 = banks[4][1], banks[5][1], banks[6][1]
        prot = Ring([])
        ALLB = list(range(8))

        def set_ring(idx):
            prot.items = [banks[i] for i in idx]
            prot.i = 0

        set_ring(ALLB)

        def getps():
            return prot.get()

        def mm(out, lhsT, rhs, start, stop, r, w, sgc=False):
            if sgc:
                S.op("pe", lambda e: e.matmul(out, lhsT, rhs, start=start, stop=stop, skip_group_check=True), r, w)
            else:
                S.op("pe", lambda e: e.matmul(out, lhsT, rhs, start=start, stop=stop), r, w)

        def act(out, in_, func, r, w, bias=None, scale=None):
            kw = {}
            if bias is not None:
                kw["bias"] = bias
            if scale is not None:
                kw["scale"] = scale
            S.op("act", lambda e: e.activation(out=out, in_=in_, func=func, **kw), r, w)

        def tto(eng, out, in0, in1, op, r, w):
            S.op(eng, lambda e: e.tensor_tensor(out=out, in0=in0, in1=in1, op=op), r, w)

        def stt(eng, out, in0, scalar, in1, op0, op1, r, w):
            S.op(eng, lambda e: e.scalar_tensor_tensor(out=out, in0=in0, scalar=scalar, in1=in1, op0=op0, op1=op1), r, w)

        def tsc(eng, out, in0, s1, s2, op0, op1, r, w):
            S.op(eng, lambda e: e.tensor_scalar(out=out, in0=in0, scalar1=s1, scalar2=s2, op0=op0, op1=op1), r, w)

        def tsm(eng, out, in0, s1, r, w):
            S.op(eng, lambda e: e.tensor_scalar_mul(out=out, in0=in0, scalar1=s1), r, w)

        def cp(eng, out, in_, r, w):
            if eng == "act":
                S.op("act", lambda e: e.activation(out=out, in_=in_, func=AF.Copy), r, w)
            else:
                S.op(eng, lambda e: e.tensor_copy(out=out, in_=in_), r, w)

        def memset(eng, ap, val, w):
            S.op(eng, lambda e: e.memset(ap, val), (), w)

        def recip(out, in_, r, w):
            S.op("dve", lambda e: e.reciprocal(out=out, in_=in_), r, w)

        def dma(q, out, in_, r, w):
            S.dma(q, lambda e: e.dma_start(out=out, in_=in_), r, w)

        def asel(ap, pattern, cmp, base, cm, w):
            S.op("pool", lambda e: e.affine_select(out=ap, in_=ap, pattern=pattern, compare_op=cmp, fill=0.0, base=base, channel_multiplier=cm), w, w)

        o = 0
        ident_bf = cst_bf[:, o:o + 128]; o += 128
        ones_bf = cst_bf[:, o:o + 128]; o += 128
        maskG = cst_bf[:, o:o + 128]; o += 128
        TriS = cst_bf[:, o:o + 128]; o += 128
        NU = cst_bf[:, o:o + 128]; o += 128
        Eb = cst_bf[:, o:o + 256].rearrange("p (b m) -> p b m", b=16); o += 256
        Xb = cst_bf[:, o:o + 2048].rearrange("p (a s) -> p a s", a=16); o += 2048
        DM = []
        NM = []
        for r_ in range(4):
            DM.append(cst_bf[:, o:o + 512]); o += 512
        for r_ in range(4):
            NM.append(cst_bf[:, o:o + 512]); o += 512
        nm_f = NM[0].tensor if False else None
        br_area = cst_bf[:, 7040 - 2048:7040].bitcast(F32)
        br_gr = Ring([br_area[:, 0:TW], br_area[:, TW:2 * TW]])
        eps_t = cst_f[:, 0:1]
        one_t = cst_f[:, 1:2]
        memset("dve", cst_f[:, 0:1], EPS, [CB])
        memset("dve", cst_f[:, 1:2], 1.0, [CB])
        tmpc = AR.f32(2048)
        TB = Buf()

        def mk(dst, np_, nf, val, pattern, cmp, base, cm, post=None):
            t = tmpc[0:np_, 0:nf]
            memset("pool", t, val, [TB])
            if pattern is not None:
                tv = t if len(pattern) == 1 else t.rearrange("p (a b) -> p a b", a=pattern[0][1])
                asel(tv, pattern, cmp, base, cm, [TB])
            if post is not None:
                post(t)
            cp("dve", dst, t, [TB], [CB])

        mk(ident_bf, 128, 128, 1.0, [[-1, 128]], ALU.is_equal, 0, 1)
        mk(ones_bf, 128, 128, 1.0, None, None, 0, 0)
        mk(maskG, 128, 128, 1.0, [[1, 128]], ALU.is_ge, 0, -1, post=lambda t: memset("pool", t[0:64, 64:128], 0.0, [TB]))
        tsm("dve", TriS, maskG, -1.0 / 16.0, [CB], [CB])
        mk(NU, 128, 128, -1.0, [[-1, 128]], ALU.is_ge, 0, 1)
        nones_bf = cst_bf[:, 640:768]
        mk(nones_bf, 128, 128, -1.0, None, None, 0, 0)
        mk(cst_bf[0:16, 896:896 + 2048], 16, 2048, -1.0, [[-1, 16], [0, 128]], ALU.is_gt, 0, 1)
        for r_ in range(4):
            mk(DM[r_], 128, 512, 1.0, [[1, 512]], ALU.is_gt, -128 * r_, -1)
        for l in range(NL):
            dma("sp", ppt[:, l * NPP:(l + 1) * NPP], pp[l], [], [PPB])

        def par(l, col):
            return ppt[:, l * NPP + col:l * NPP + col + 1]

        class WQ:
            def __init__(self):
                self.plan = []
                self.issued = 0
                self.consumed = 0

            def extend(self, items):
                self.plan.extend(items)

            def get(self, D=2):
                while self.issued < min(len(self.plan), self.consumed + 1 + D):
                    src, n = self.plan[self.issued]
                    slot = self.issued % NW
                    dma("pool", wsl[slot][:, :, 0:n], src.rearrange("(k p) n -> p k n", p=128), [], [WB[slot]])
                    self.issued += 1
                slot = self.consumed % NW
                self.consumed += 1
                return slot

        wq = WQ()

        def proj(slot, c0, n, tt, src=None, RB=None):
            if src is None:
                src, RB = xn, XN
            ps, pb = getps()
            for k in range(KC):
                mm(ps[0:n, :], wsl[slot][:, k, c0:c0 + n], src[:, k, tsl(tt)], k == 0, k == KC - 1, [WB[slot], RB[k][tt]], [pb])
            return ps, pb

        def dump(idx, src, RBs, bf):
            if not dbg:
                return
            for k in range(KC):
                dma("pool" if bf else "sp", dbg_t[idx, k * 128:(k + 1) * 128, :], src[:, k, :], RBs[k], [DBG])

        def rmsnorm(l, gcol):
            set_ring(ALLB)
            AR.reset()
            sqr = Ring([AR.bf16(TW) for _ in range(3)])
            srr = Ring([AR.f32(TW) for _ in range(2)])
            for tt in range(TT):
                ps, pb = getps()
                for k in range(KC):
                    sq, sqb = sqr.get()
                    act(sq, xb[:, k, tsl(tt)], AF.Square, [XB[k][tt]], [sqb])
                    mm(ps, ones_bf, sq, k == 0, k == KC - 1, [sqb, CB], [pb])
                sr, srb = srr.get()
                act(sr, ps, AF.Ln, [pb, CB], [srb], bias=eps_t, scale=1.0 / 1024.0)
                act(sr, sr, AF.Exp, [srb], [srb], scale=-0.5)
                for k in range(KC):
                    stt("dve", xn[:, k, tsl(tt)], xb[:, k, tsl(tt)], par(l, gcol + k), sr, ALU.mult, ALU.mult,
                        [XB[k][tt], srb, PPB], [XN[k][tt]])

        def spill_x():
            for k in range(KC):
                dma("sp", yT[k * 128:(k + 1) * 128, :], xb[:, k, :], XB[k], [YT[k]])

        def reload_x():
            for k in range(KC):
                dma("sp", xb[:, k, :], yT[k * 128:(k + 1) * 128, :], [YT[k]], XB[k])

        def gla(l):
            set_ring([0, 1, 2, 3, 6])
            AR.reset()
            AR2.reset()
            A = AR2
            adT = A.bf16(T); ADT = [Buf() for _ in range(TT)]
            wupa = A.bf16(512); WUPA = Buf()
            e1r = Ring([A.f32(TW) for _ in range(2)])
            Pr = Ring([A.bf16(TW) for _ in range(2)])
            ecr = Ring([A.f32(TW) for _ in range(2)])
            eir = Ring([A.f32(TW) for _ in range(2)])
            qd = A.bf16(T); QD = [Buf() for _ in range(TT)]
            ki = A.bf16(T); KI = [Buf() for _ in range(TT)]
            ki_tm = A.bf16(T); KITM = [Buf() for _ in range(TT)]
            v_tm = A.bf16(16 * 256); VTM = [Buf() for _ in range(8)]
            sg = [A.bf16(T), A.bf16(T)]; SG = [[Buf() for _ in range(TT)] for _ in range(2)]
            scr = Ring([A.bf16(128) for _ in range(3)])
            dec = A.f32(32); DEC = Buf()
            sqr = Ring([A.bf16(TW) for _ in range(2)])
            srr = Ring([A.f32(TW) for _ in range(2)])
            posb = [Ring([A.f32(TW), A.f32(TW)]), Ring([A.f32(TW), A.f32(TW)])]
            kvs = AR.f32(16 * 256); KVS = [Buf() for _ in range(16)]
            S_all = AR.f32(17 * 256); SAL = [Buf() for _ in range(17)]
            S_bfa = AR.bf16(16 * 256); SBA = [Buf() for _ in range(4)]
            for tt in range(TT):
                memset("dve", adT[0:33, tsl(tt)], 1.0, [ADT[tt]])
            memset("dve", wupa[0:33, :], 0.0, [WUPA])
            dma("pool", wupa[0:16, :], gla_w_up[l], [], [WUPA])
            dma("pool", wupa[32:33, :], gla_b_alpha[l:l + 1, :], [], [WUPA])
            plan = [(w_in[l][:, C_GD:C_GD + 16], 16)]
            for h in range(4):
                plan += [(w_in[l][:, C_GQ + h * 128:C_GQ + (h + 1) * 128], 128),
                         (w_in[l][:, C_GK + h * 128:C_GK + (h + 1) * 128], 128),
                         (w_in[l][:, C_GV + h * 256:C_GV + (h + 1) * 256], 256),
                         (w_in[l][:, C_GG + h * 256:C_GG + (h + 1) * 256], 256)]
            wq.extend(plan)
            s_ = wq.get(D=1)
            for tt in range(TT):
                ps, pb = proj(s_, 0, 16, tt)
                cp("act", adT[0:16, tsl(tt)], ps[0:16, :], [pb], [ADT[tt]])
            for h in range(4):
                s_q = wq.get(D=2)
                s_k = wq.get(D=2)
                for tt in range(TT + 1):
                    if tt < TT:
                        psy, pyb = getps()
                        for mi in range(4):
                            m = tt * 4 + mi
                            mm(psy[:, mi * 128:(mi + 1) * 128], adT[0:33, m * 128:(m + 1) * 128], wupa[0:33, h * 128:(h + 1) * 128],
                               True, True, [ADT[tt], WUPA], [pyb])
                        e1, e1b = e1r.get()
                        act(e1, psy, AF.Exp, [pyb], [e1b], scale=-1.0)
                        Pt, Ptb = Pr.get()
                        act(Pt, e1, AF.Ln, [e1b, CB], [Ptb], bias=one_t)
                        psq, pqb = proj(s_q, 0, 128, tt)
                        psk, pkb = proj(s_k, 0, 128, tt)
                        psc, pcb = getps()
                        for mi in range(4):
                            mm(psc[:, mi * 128:(mi + 1) * 128], Pt[:, mi * 128:(mi + 1) * 128], TriS, True, True, [Ptb, CB], [pcb])
                        ec, ecb = ecr.get()
                        ei, eib = eir.get()
                        act(ec, psc, AF.Exp, [pcb], [ecb])
                        act(ei, psc, AF.Exp, [pcb], [eib], scale=-1.0)
                        cp("dve", dec[:, tt * 8:(tt + 1) * 8], ec[:, 63::64], [ecb], [DEC])
                        stt("dve", qd[:, tsl(tt)], psq, 128.0 ** -0.5, ec, ALU.mult, ALU.mult, [pqb, ecb], [QD[tt]])
                        tto("dve", ki[:, tsl(tt)], psk, ei, ALU.mult, [pkb, eib], [KI[tt]])
                    if tt >= 1:
                        t2 = tt - 1
                        for mi in range(4):
                            m = t2 * 4 + mi
                            S.op("pe", (lambda o_, i_: (lambda e: e.transpose(o_, i_, ident_bf)))(ptr[:, mi * 128:(mi + 1) * 128], ki[:, m * 128:(m + 1) * 128]),
                                 [KI[t2], CB], [PTR])
                        cp("act", ki_tm[:, tsl(t2)], ptr[:, 0:512], [PTR], [KITM[t2]])
                s_v = wq.get(D=2)
                for m2 in range(8):
                    ps, pb = getps()
                    for mj in range(2):
                        m = m2 * 2 + mj
                        for k in range(KC):
                            mm(ps[:, mj * 256:(mj + 1) * 256], xn[:, k, m * 128:(m + 1) * 128], wsl[s_v][:, k, 0:256],
                               k == 0, k == KC - 1, [XN[k][m // 4], WB[s_v]], [pb])
                    cp("act", v_tm[:, m2 * 512:(m2 + 1) * 512], ps, [pb], [VTM[m2]])
                s_g = wq.get(D=2)
                for jb in range(2):
                    for tt in range(TT):
                        ps, pb = proj(s_g, jb * 128, 128, tt)
                        act(sg[jb][:, tsl(tt)], ps, AF.Silu, [pb], [SG[jb][tt]])
                pend = []

                def normrest(tt, pcs, h=h):
                    pn, pnb = getps()
                    for jb in range(2):
                        sq, sqb = sqr.get()
                        act(sq, pcs[jb][0], AF.Square, [pcs[jb][1]], [sqb])
                        mm(pn, ones_bf, sq, jb == 0, jb == 1, [sqb, CB], [pnb])
                    sr, srb = srr.get()
                    act(sr, pn, AF.Ln, [pnb, CB], [srb], bias=eps_t, scale=1.0 / 256.0)
                    act(sr, sr, AF.Exp, [srb], [srb], scale=-0.5)
                    for jb in range(2):
                        pc, pcb_ = pcs[jb]
                        tto("dve", pc, pc, sr, ALU.mult, [pcb_, srb], [pcb_])
                        stt("dve", ob[:, h * 2 + jb, tsl(tt)], pc, par(l, P_GLAN + jb), sg[jb][:, tsl(tt)], ALU.mult, ALU.mult,
                            [pcb_, PPB, SG[jb][tt]], [OB[h * 2 + jb][tt]])

                for hs in range(2):
                    for ci in range(16):
                        c = hs * 16 + ci
                        m = c // 2
                        half = c % 2
                        pkv, pkb = getps()
                        mm(pkv[:, 0:256], ki_tm[half * 64:(half + 1) * 64, m * 128:(m + 1) * 128],
                           v_tm[half * 64:(half + 1) * 64, m * 256:(m + 1) * 256], True, True, [KITM[m // 4], VTM[m // 2]], [pkb])
                        tsm("dve", kvs[:, ci * 256:(ci + 1) * 256], pkv[:, 0:256], dec[:, c:c + 1], [pkb, DEC], [KVS[ci]])
                    if hs == 0:
                        memset("dve", S_all[:, 0:256], 0.0, [SAL[0]])
                    else:
                        cp("dve", S_all[:, 0:256], S_all[:, 16 * 256:17 * 256], [SAL[16]], [SAL[0]])
                    for ci in range(16):
                        c = hs * 16 + ci
                        stt("dve", S_all[:, (ci + 1) * 256:(ci + 2) * 256], S_all[:, ci * 256:(ci + 1) * 256], dec[:, c:c + 1],
                            kvs[:, ci * 256:(ci + 1) * 256], ALU.mult, ALU.add, [SAL[ci], DEC, KVS[ci]], [SAL[ci + 1]])
                        if ci % 4 == 3:
                            g4 = ci // 4
                            cp("act", S_bfa[:, g4 * 1024:(g4 + 1) * 1024], S_all[:, g4 * 1024:(g4 + 1) * 1024],
                               [SAL[g4 * 4 + j] for j in range(4)], [SBA[g4]])
                    for tl in range(2):
                        tt = hs * 2 + tl
                        po = [P4, P5]
                        POB = [PB4, PB5]
                        scl = {}
                        for it in range(5):
                            if it < 4:
                                mi = it
                                m = tt * 4 + mi
                                pss, psb = getps()
                                mm(pss[:, 0:128], ki[:, m * 128:(m + 1) * 128], qd[:, m * 128:(m + 1) * 128], True, True, [KI[tt], QD[tt]], [psb])
                                sc, scb = scr.get()
                                tto("dve", sc, pss[:, 0:128], maskG, ALU.mult, [psb, CB], [scb])
                                scl[mi] = (sc, scb)
                            if it >= 1:
                                mi = it - 1
                                m = tt * 4 + mi
                                sc, scb = scl.pop(mi)
                                for half in range(2):
                                    c = 2 * m + half
                                    ci = c - hs * 16
                                    for jb in range(2):
                                        col = mi * 128 + half * 64
                                        mm(po[jb][:, col:col + 64], S_bfa[:, ci * 256 + jb * 128:ci * 256 + (jb + 1) * 128], qd[:, c * 64:(c + 1) * 64],
                                           True, False, [SBA[ci // 4], QD[tt]], [POB[jb]])
                                        mm(po[jb][:, col:col + 64], v_tm[:, m * 256 + jb * 128:m * 256 + (jb + 1) * 128], sc[:, half * 64:(half + 1) * 64],
                                           False, True, [VTM[m // 2], scb], [POB[jb]])
                        if pend:
                            normrest(*pend.pop())
                        pcs = []
                        for jb in range(2):
                            pc, pcb_ = posb[jb].get()
                            cp("act", pc, po[jb], [POB[jb]], [pcb_])
                            pcs.append((pc, pcb_))
                        pend.append((tt, pcs))
                while pend:
                    normrest(*pend.pop())

        def branch(l, b):
            set_ring(ALLB)
            gr = br_gr
            plan = []
            for nbp in range(4):
                plan += [(w_br[b][l][:, nbp * 256:(nbp + 1) * 256], 256),
                         (w_in[l][:, C_GATE + b * 1024 + nbp * 256:C_GATE + b * 1024 + (nbp + 1) * 256], 256)]
            wq.extend(plan)
            for nbp in range(4):
                s1 = wq.get(D=2)
                s2 = wq.get(D=2)
                for nbi in range(2):
                    nb = nbp * 2 + nbi
                    for tt in range(TT):
                        ps1, p1b = proj(s1, nbi * 128, 128, tt, ob, OB)
                        ps2, p2b = proj(s2, nbi * 128, 128, tt)
                        g, gb = gr.get()
                        act(g, ps2, AF.Sigmoid, [p2b, PPB], [gb], bias=par(l, P_BG + b * 8 + nb))
                        if b == 0:
                            tto("dve", xb[:, nb, tsl(tt)], ps1, g, ALU.mult, [p1b, gb], [XB[nb][tt]])
                        else:
                            tto("dve", g, ps1, g, ALU.mult, [p1b, gb], [gb])
                            tto("dve", xb[:, nb, tsl(tt)], xb[:, nb, tsl(tt)], g, ALU.add, [gb, XB[nb][tt]], [XB[nb][tt]])

        def lru(l):
            set_ring(ALLB)
            AR.reset()
            A = AR
            cl = A.f32(8); CL = Buf()
            tmp8 = A.f32(8)
            lxbf = [A.bf16(T + 4), A.bf16(T + 4)]; LXB = [[Buf() for _ in range(TT)] for _ in range(2)]
            dg = [[A.bf16(128) for _ in range(4)] for _ in range(2)]; DG = Buf()
            wab = A.bf16(512).rearrange("p (i j) -> p i j", i=2); wxb = A.bf16(512).rearrange("p (i j) -> p i j", i=2); WAB = Buf(); WXB = Buf()
            xcf = [Ring([A.f32(TW) for _ in range(1)]) for _ in range(2)]
            xcb = [Ring([A.bf16(TW) for _ in range(1)]) for _ in range(2)]
            rr = Ring([A.f32(TW) for _ in range(2)])
            igr = Ring([A.f32(TW) for _ in range(2)])
            n2r = Ring([A.f32(TW) for _ in range(2)])
            hr = [Ring([A.f32(TW) for _ in range(2)]) for _ in range(2)]
            glr = Ring([A.f32(TW) for _ in range(2)])
            g2r = Ring([A.f32(TW) for _ in range(2)])
            act(tmp8, ppt[:, l * NPP + P_LAM:l * NPP + P_LAM + 8], AF.Exp, [PPB], [CL], scale=-1.0)
            act(tmp8, tmp8, AF.Ln, [CL, CB], [CL], bias=one_t)
            tsm("dve", cl, tmp8, -8.0, [CL], [CL])
            plan = []
            for hb in range(4):
                plan += [(w_in[l][:, C_LX + hb * 256:C_LX + (hb + 1) * 256], 256), (w_in[l][:, C_LG + hb * 256:C_LG + (hb + 1) * 256], 256)]
            wq.extend(plan)
            for hb in range(4):
                slx = wq.get(D=2)
                slg = wq.get(D=2)
                dma("pool", wab, lru_w_a[l, hb].rearrange("(i p) j -> p i j", p=128), [], [WAB])
                dma("pool", wxb, lru_w_x[l, hb].rearrange("(i p) j -> p i j", p=128), [], [WXB])
                for cbl in range(2):
                    memset("dve", lxbf[cbl][:, 0:3], 0.0, [LXB[cbl][0]])
                    for w_ in range(4):
                        tsm("dve", dg[cbl][w_], ident_bf, par(l, P_CW + (hb * 2 + cbl) * 4 + w_), [CB, PPB], [DG])
                hprev = [None, None]
                for tt in range(TT):
                    for cbl in range(2):
                        ps, pb = proj(slx, cbl * 128, 128, tt)
                        cp("act", lxbf[cbl][:, 3 + tt * TW:3 + (tt + 1) * TW], ps, [pb], [LXB[cbl][tt]])
                    xf = []
                    xh = []
                    for cbl in range(2):
                        ps, pb = getps()
                        rd = [LXB[cbl][tt], DG] + ([LXB[cbl][tt - 1]] if tt > 0 else [])
                        for w_ in range(4):
                            mm(ps, dg[cbl][w_], lxbf[cbl][:, tt * TW + w_:tt * TW + w_ + TW], w_ == 0, w_ == 3, rd, [pb])
                        f_, fb = xcf[cbl].get()
                        act(f_, ps, AF.Identity, [pb, PPB], [fb], bias=par(l, P_CB + hb * 2 + cbl))
                        h_, hb_ = xcb[cbl].get()
                        cp("dve", h_, f_, [fb], [hb_])
                        xf.append((f_, fb))
                        xh.append((h_, hb_))
                    rs, igs, gls, g2s, n2s, hhs = [], [], [], [], [], []
                    for jb in range(2):
                        c = hb * 2 + jb
                        psr, prb = getps()
                        for ib in range(2):
                            mm(psr, wab[:, ib, jb * 128:(jb + 1) * 128], xh[ib][0], ib == 0, ib == 1, [WAB, xh[ib][1]], [prb])
                        psi, pib = getps()
                        for ib in range(2):
                            mm(psi, wxb[:, ib, jb * 128:(jb + 1) * 128], xh[ib][0], ib == 0, ib == 1, [WXB, xh[ib][1]], [pib])
                        r_, rb = rr.get()
                        act(r_, psr, AF.Sigmoid, [prb, PPB], [rb], bias=par(l, P_BA + c))
                        ig, igb = igr.get()
                        act(ig, psi, AF.Sigmoid, [pib, PPB], [igb], bias=par(l, P_BX + c))
                        rs.append((r_, rb))
                        igs.append((ig, igb))
                    for jb in range(2):
                        c = hb * 2 + jb
                        a_, ab = rs[jb]
                        act(a_, a_, AF.Exp, [ab, CL], [ab], scale=cl[:, c:c + 1])
                    for jb in range(2):
                        psg, pgb = proj(slg, jb * 128, 128, tt)
                        gl, glb = glr.get()
                        act(gl, psg, AF.Identity, [pgb], [glb])
                        gls.append((gl, glb))
                    for jb in range(2):
                        a_, ab = rs[jb]
                        n2, n2b = n2r.get()
                        stt("dve", n2, a_, -1.0, a_, ALU.mult, ALU.mult, [ab], [n2b])
                        n2s.append((n2, n2b))
                        gl, glb = gls[jb]
                        g2, g2b = g2r.get()
                        stt("dve", g2, gl, 0.044715, gl, ALU.mult, ALU.mult, [glb], [g2b])
                        stt("dve", g2, g2, 1.0, gl, ALU.add, ALU.mult, [g2b, glb], [g2b])
                        g2s.append((g2, g2b))
                    for jb in range(2):
                        n2, n2b = n2s[jb]
                        act(n2, n2, AF.Sqrt, [n2b, CB], [n2b], bias=one_t)
                    for jb in range(2):
                        g2, g2b = g2s[jb]
                        act(g2, g2, AF.Sigmoid, [g2b], [g2b], scale=1.5957691216057308)
                    for jb in range(2):
                        c = hb * 2 + jb
                        a_, ab = rs[jb]
                        ig, igb = igs[jb]
                        n2, n2b = n2s[jb]
                        gl, glb = gls[jb]
                        g2, g2b = g2s[jb]
                        u_, ub = ig, igb
                        tto("dve", u_, n2, ig, ALU.mult, [n2b, igb], [ub])
                        tto("dve", u_, u_, xf[jb][0], ALU.mult, [ub, xf[jb][1]], [ub])
                        hh, hhb = hr[jb].get()
                        if hprev[jb] is None:
                            S.op("dve", (lambda o_, a0, u0: (lambda e: e.tensor_tensor_scan(out=o_, data0=a0, data1=u0, initial=0.0, op0=ALU.mult, op1=ALU.add)))(hh, a_, u_),
                                 [ab, ub], [hhb])
                        else:
                            hp, hpb = hprev[jb]
                            S.op("dve", (lambda o_, a0, u0, i0: (lambda e: e.tensor_tensor_scan(out=o_, data0=a0, data1=u0, initial=i0, op0=ALU.mult, op1=ALU.add)))(hh, a_, u_, hp[:, TW - 1:TW]),
                                 [ab, ub, hpb], [hhb])
                        hprev[jb] = (hh, hhb)
                        tto("dve", gl, gl, hh, ALU.mult, [glb, hhb], [glb])
                        tto("dve", ob[:, c, tsl(tt)], gl, g2, ALU.mult, [glb, g2b], [OB[c][tt]])

        def sbattn(l):
            set_ring([0, 1, 2, 3, 4, 7])
            AR.reset()
            A = AR
            Lkr = Ring([A.bf16(TW) for _ in range(5)])
            Rbr = Ring([A.bf16(TW) for _ in range(5)])
            R32 = A.f32(TW); R32B = Buf()
            qn = A.bf16(T); QN = [Buf() for _ in range(TT)]
            kn = A.bf16(T); KN = [Buf() for _ in range(TT)]
            v_tm = A.bf16(T); VT = [Buf() for _ in range(4)]
            Er = Ring([A.f32(TW) for _ in range(2)])
            wr = Ring([A.bf16(TW) for _ in range(4)])
            sqr = Ring([A.bf16(TW) for _ in range(2)])
            srr = Ring([A.f32(TW) for _ in range(2)])
            gq2 = A.f32(2); GQ = Buf()
            tsm("dve", gq2[:, 0:1], par(l, P_SQG), 128.0 ** -0.5, [PPB], [GQ])
            cp("dve", gq2[:, 1:2], par(l, P_SKG), [PPB], [GQ])
            plan = []
            for h in range(8):
                plan += [(w_in[l][:, C_SQ + h * 128:C_SQ + (h + 1) * 128], 128), (w_in[l][:, C_SK + h * 128:C_SK + (h + 1) * 128], 128),
                         (w_in[l][:, C_SV + h * 128:C_SV + (h + 1) * 128], 128)]
            wq.extend(plan)
            for h in range(8):
                slots_qk = [wq.get(D=2), wq.get(D=2)]
                items = [(which, tt) for which in range(2) for tt in range(TT)]
                stA = {}

                def stageA(i):
                    which, tt = items[i]
                    ps, pb = proj(slots_qk[which], 0, 128, tt)
                    sq, sqb = sqr.get()
                    act(sq, ps, AF.Square, [pb], [sqb])
                    stA[i] = (ps, pb, sq, sqb)

                def stageB(i):
                    which, tt = items[i]
                    dst, DBs = ((qn, QN), (kn, KN))[which]
                    ps, pb, sq, sqb = stA.pop(i)
                    pn, pnb = getps()
                    mm(pn, ones_bf, sq, True, True, [sqb, CB], [pnb])
                    sr, srb = srr.get()
                    act(sr, pn, AF.Ln, [pnb, CB], [srb], bias=eps_t, scale=1.0 / 128.0)
                    act(sr, sr, AF.Exp, [srb], [srb], scale=-0.5)
                    stt("dve", dst[:, tsl(tt)], ps, gq2[:, which:which + 1], sr, ALU.mult, ALU.mult, [pb, srb, GQ], [DBs[tt]])

                for i in range(len(items) + 1):
                    if i < len(items):
                        stageA(i)
                    if i >= 1:
                        stageB(i - 1)
                s_v = wq.get(D=2)
                for m4 in range(4):
                    ps, pb = getps()
                    for mi in range(4):
                        m = m4 * 4 + mi
                        for k in range(KC):
                            mm(ps[:, mi * 128:(mi + 1) * 128], xn[:, k, m * 128:(m + 1) * 128], wsl[s_v][:, k, 0:128],
                               k == 0, k == KC - 1, [XN[k][m4], WB[s_v]], [pb])
                    cp("act", v_tm[:, m4 * 512:(m4 + 1) * 512], ps, [pb], [VT[m4]])
                L1, L2 = 2, 2
                blocks = [(qt, a) for qt in range(TT) for a in reversed(range(4 * (qt + 1)))]
                stt_ = {}
                wl = {}
                curR = [None]

                def S1(qt, a):
                    nk = 4 * (qt + 1)
                    r_ = a - 4 * qt
                    c0 = 128 * r_ if r_ >= 0 else 0
                    cs_ = slice(c0, TW)
                    psz, pzb = getps()
                    mm(psz[:, cs_], kn[:, a * 128:(a + 1) * 128], qn[:, qt * TW + c0:(qt + 1) * TW], True, True, [KN[a // 4], QN[qt]], [pzb])
                    E, Eb_ = Er.get()
                    act(E[:, cs_], psz[:, cs_], AF.Exp, [pzb], [Eb_])
                    La, Lab = Lkr.get()
                    act(La[:, cs_], E[:, cs_], AF.Ln, [Eb_, CB], [Lab], bias=one_t)
                    if r_ >= 0:
                        tto("dve", La[:, c0:c0 + 128], La[:, c0:c0 + 128], DM[0][:, 0:128], ALU.mult, [Lab, CB], [Lab])
                    myR = None if a == nk - 1 else curR[0]
                    if a > 0:
                        if a == nk - 1:
                            if c0 > 0:
                                memset("dve", R32[:, 0:c0], 0.0, [R32B])
                            cp("dve", R32[:, cs_], La[:, cs_], [Lab], [R32B])
                        else:
                            tto("dve", R32[:, cs_], R32[:, cs_], La[:, cs_], ALU.add, [R32B, Lab], [R32B])
                        Rb, Rbb = Rbr.get()
                        cp("dve", Rb, R32, [R32B], [Rbb])
                        curR[0] = (Rb, Rbb)
                    stt_[(qt, a)] = (psz, pzb, La, Lab, myR, c0)

                def S2(qt, a):
                    psz, pzb, La, Lab, myR, c0 = stt_.pop((qt, a))
                    cs_ = slice(c0, TW)
                    mm(psz[:, cs_], NU, La[:, cs_], False, myR is None, [CB, Lab], [pzb], sgc=True)
                    if myR is not None:
                        mm(psz[:, cs_], nones_bf, myR[0][:, cs_], False, True, [CB, myR[1]], [pzb], sgc=True)
                    w_, wb_ = wr.get()
                    act(w_[:, cs_], psz[:, cs_], AF.Exp, [pzb], [wb_])
                    if a >= 4 * qt:
                        tto("dve", w_[:, c0:c0 + 128], w_[:, c0:c0 + 128], DM[0][:, 0:128], ALU.mult, [wb_, CB], [wb_])
                    if a == 4 * (qt + 1) - 1 and c0 > 0:
                        memset("dve", w_[:, 0:c0], 0.0, [wb_])
                    wl[(qt, a)] = (w_, wb_, c0)

                def S3(qt, a):
                    nk = 4 * (qt + 1)
                    po, pob = (P5, PB5) if qt % 2 == 0 else (P6, PB6)
                    w_, wb_, c0 = wl.pop((qt, a))
                    vv = v_tm[:, a * 128:(a + 1) * 128]
                    if a == nk - 1:
                        mm(po, vv, w_, True, a == 0, [VT[a // 4], wb_], [pob], sgc=True)
                    else:
                        mm(po[:, c0:TW], vv, w_[:, c0:TW], False, a == 0, [VT[a // 4], wb_], [pob], sgc=True)
                    if a == 0:
                        cp("dve", ob[:, h, tsl(qt)], po, [pob], [OB[h][qt]])

                nb_ = len(blocks)
                for it in range(nb_ + L1 + L2):
                    if it < nb_:
                        S1(*blocks[it])
                    if L1 <= it < nb_ + L1:
                        S2(*blocks[it - L1])
                    if it >= L1 + L2:
                        S3(*blocks[it - L1 - L2])

        def wout(l):
            set_ring(ALLB)
            AR.reset()
            for nb in range(KC):
                for tt in range(TT):
                    cp("act" if (nb + tt) % 2 == 0 else "dve", ob[:, nb, tsl(tt)], xb[:, nb, tsl(tt)], [XB[nb][tt]], [OB[nb][tt]])
            dump(4, ob, OB, True)
            reload_x()
            wq.extend([(w_out[l][:, i * 256:(i + 1) * 256], 256) for i in range(4)])
            for i in range(4):
                s_ = wq.get(D=2)
                for nbi in range(2):
                    nb = i * 2 + nbi
                    for tt in range(TT):
                        ps, pb = proj(s_, nbi * 128, 128, tt, ob, OB)
                        tto("dve", xb[:, nb, tsl(tt)], ps, xb[:, nb, tsl(tt)], ALU.add, [pb, XB[nb][tt]], [XB[nb][tt]])

        def mlp(l):
            rmsnorm(l, P_GMLP)
            rr = Ring([AR.f32(TW) for _ in range(3)])
            plan = []
            for fg in range(4):
                plan += [(w_up[l][:, fg * 1024 + i * 256:fg * 1024 + (i + 1) * 256], 256) for i in range(4)]
                plan += [(w_dn[l][fg * 1024:(fg + 1) * 1024, i * 256:(i + 1) * 256], 256) for i in range(4)]
            wq.extend(plan)
            for fg in range(4):
                for i in range(4):
                    s_ = wq.get(D=3)
                    for fbi in range(2):
                        fb = i * 2 + fbi
                        for tt in range(TT):
                            ps, pb = proj(s_, fbi * 128, 128, tt)
                            r_, rb = rr.get()
                            act(r_, ps, AF.Relu, [pb], [rb])
                            tto("dve", ob[:, fb, tsl(tt)], r_, r_, ALU.mult, [rb], [OB[fb][tt]])
                for i in range(4):
                    s_ = wq.get(D=3)
                    for nbi in range(2):
                        nb = i * 2 + nbi
                        for tt in range(TT):
                            ps, pb = proj(s_, nbi * 128, 128, tt, ob, OB)
                            tto("dve", xb[:, nb, tsl(tt)], ps, xb[:, nb, tsl(tt)], ALU.add, [pb, XB[nb][tt]], [XB[nb][tt]])

        S.barrier()
        for k in range(KC):
            dma("sp", xb[:, k, :], xT[k * 128:(k + 1) * 128, :], [], XB[k])
        for l in range(NL):
            rmsnorm(l, P_GMIX)
            if l == 0:
                dump(0, xn, XN, True)
            spill_x()
            S.barrier()
            gla(l)
            if l == 0:
                dump(1, ob, OB, True)
            S.barrier()
            branch(l, 0)
            lru(l)
            if l == 0:
                dump(2, ob, OB, True)
            S.barrier()
            branch(l, 1)
            sbattn(l)
            if l == 0:
                dump(3, ob, OB, True)
            S.barrier()
            branch(l, 2)
            wout(l)
            if l == 0:
                dump(5, xb, XB, False)
            mlp(l)
            S.barrier()
        for k in range(KC):
            dma("sp", yT[k * 128:(k + 1) * 128, :], xb[:, k, :], XB[k], [YT[k]])
        S.barrier()
        S.emit(st)
        print("instructions:", S.ninst, {e: len(S.streams[e]) for e in ENGS})
    return nc


def pack_params(inp, layers):
    def fm(v, nb):
        return np.ascontiguousarray(np.asarray(v, np.float32).reshape(nb, 128).T)

    out = np.zeros((len(layers), 128, NPP), np.float32)
    for i, l in enumerate(layers):
        out[i, :, P_GMIX:P_GMIX + 8] = fm(inp["norm_mix_g"][l], 8)
        out[i, :, P_GMLP:P_GMLP + 8] = fm(inp["norm_mlp_g"][l], 8)
        out[i, :, P_GLAN:P_GLAN + 2] = fm(inp["gla_norm_g"][l], 2)
        cw = np.asarray(inp["lru_conv_w"][l], np.float32)
        for cb in range(8):
            for w_ in range(4):
                out[i, :, P_CW + cb * 4 + w_] = cw[w_, cb * 128:(cb + 1) * 128]
        out[i, :, P_CB:P_CB + 8] = fm(inp["lru_conv_b"][l], 8)
        out[i, :, P_BA:P_BA + 8] = fm(inp["lru_b_a"][l], 8)
        out[i, :, P_BX:P_BX + 8] = fm(inp["lru_b_x"][l], 8)
        out[i, :, P_LAM:P_LAM + 8] = fm(inp["lru_lambda"][l], 8)
        out[i, :, P_SQG] = np.asarray(inp["sb_q_norm_g"][l], np.float32)
        out[i, :, P_SKG] = np.asarray(inp["sb_k_norm_g"][l], np.float32)
        out[i, :, P_BG:P_BG + 24] = fm(inp["b_gate"][l], 24)
    return out


_NC_CACHE = {}
WNAMES = ["w_in", "gla_w_up", "gla_b_alpha", "lru_w_a", "lru_w_x", "w_branch_a", "w_branch_b", "w_branch_c", "w_out", "w_mlp_up", "w_mlp_down"]


def _get_nc(NL, dbg=False):
    key = (NL, dbg)
    if key not in _NC_CACHE:
        _NC_CACHE[key] = build(NL, dbg)
    return _NC_CACHE[key]


def run_layers(inp, xT_list, layers, dbg=False, cores=8):
    nc = _get_nc(len(layers), dbg)
    ppk = pack_params(inp, layers)
    shared = {"pp": ppk}
    for n in WNAMES:
        shared[n] = np.ascontiguousarray(np.asarray(inp[n], np.float32)[layers[0]:layers[-1] + 1])
    in_maps = []
    for c in range(cores):
        d = dict(shared)
        d["xT"] = xT_list[c]
        in_maps.append(d)
    res = run_bass_kernel_spmd(nc, in_maps, core_ids=list(range(cores)))
    return res.results


def kernel(**inputs):
    x = np.asarray(inputs["x"], np.float32)
    B = x.shape[0]
    xT = [np.ascontiguousarray(x[b].T) for b in range(B)]
    if FUSED:
        res = run_layers(inputs, xT, list(range(DEPTH)))
        xT = [r["yT"] for r in res]
    else:
        for l in range(DEPTH):
            res = run_layers(inputs, xT, [l])
            xT = [np.ascontiguousarray(r["yT"]) for r in res]
    return np.stack([np.asarray(t).T for t in xT], axis=0).astype(np.float32)
```

```python
from contextlib import ExitStack
import numpy as np
import concourse.bass as bass
import concourse.mybir as mybir
from concourse.bass_utils import run_bass_kernel_spmd

F32 = mybir.dt.float32
BF16 = mybir.dt.bfloat16
AF = mybir.ActivationFunctionType
ALU = mybir.AluOpType

FUSED = True
DEPTH = 4
T = 2048
TT = 4
TW = 512
KC = 8
NW = 4
EPS = 1e-6
C_GQ, C_GK, C_GV, C_GG, C_GD, C_LX, C_LG, C_SQ, C_SK, C_SV, C_GATE = 0, 512, 1024, 2048, 3072, 3088, 4112, 5136, 6160, 7184, 8208
IN_COLS = 11280
P_GMIX, P_GMLP, P_GLAN, P_CW, P_CB, P_BA, P_BX, P_LAM, P_SQG, P_SKG, P_BG, NPP = 0, 8, 16, 18, 50, 58, 66, 74, 82, 83, 84, 108


class Buf:
    __slots__ = ("w", "r")

    def __init__(self):
        self.w = None
        self.r = {}


ENGS = ("pe", "act", "dve", "pool", "sp")
EPOCH = 20000
DMA_EPOCH = 1000


class Sched:
    def __init__(self, nc, n_dma_ch=8):
        self.nc = nc
        self.streams = {e: [] for e in ENGS}
        self.cnt = {e: 0 for e in ENGS}
        self.epoch = {e: 0 for e in ENGS}
        self.waited = {e: {} for e in ENGS}
        self.n_dma_ch = n_dma_ch
        self.dcnt = [0] * n_dma_ch
        self.depoch = [0] * n_dma_ch
        self.dnext = 0
        self.semkeys = []
        self._seen = set()
        self.latest = {}
        self.ninst = 0

    def _key(self, k):
        if k not in self._seen:
            self._seen.add(k)
            self.semkeys.append(k)
        return k

    def _filter(self, eng, need):
        out = []
        wd = self.waited[eng]
        for s, v in need.items():
            if eng == "pe" and s[0] == "pe":
                continue
            if wd.get(s, 0) < v:
                wd[s] = v
                out.append((s, v))
        return out

    def _deps(self, eng, reads, writes):
        need = {}
        for b in reads:
            if b.w is not None:
                s, v = b.w
                if need.get(s, 0) < v:
                    need[s] = v
        for b in writes:
            if b.w is not None:
                s, v = b.w
                if need.get(s, 0) < v:
                    need[s] = v
            for s, v in b.r.items():
                if need.get(s, 0) < v:
                    need[s] = v
        return self._filter(eng, need)

    def _mark(self, ev, reads, writes):
        s, v = ev
        self.latest[s] = v
        for b in reads:
            if b.r.get(s, 0) < v:
                b.r[s] = v
        for b in writes:
            b.w = ev
            b.r = {}

    def op(self, eng, fn, reads=(), writes=()):
        deps = self._deps(eng, reads, writes)
        if self.cnt[eng] >= EPOCH:
            self.epoch[eng] += 1
            self.cnt[eng] = 0
        self.cnt[eng] += 1
        key = self._key((eng, self.epoch[eng]))
        self.streams[eng].append((deps, fn, key, 1))
        self._mark((key, self.cnt[eng]), reads, writes)
        self.ninst += 1

    def dma(self, queue, fn, reads=(), writes=()):
        deps = self._deps(queue, reads, writes)
        ch = self.dnext
        self.dnext = (self.dnext + 1) % self.n_dma_ch
        if self.dcnt[ch] >= DMA_EPOCH:
            self.depoch[ch] += 1
            self.dcnt[ch] = 0
        self.dcnt[ch] += 1
        key = self._key(("dma%d" % ch, self.depoch[ch]))
        self.streams[queue].append((deps, fn, key, 16))
        self._mark((key, 16 * self.dcnt[ch]), reads, writes)
        self.ninst += 1

    def barrier(self):
        for eng in ENGS:
            deps = self._filter(eng, dict(self.latest))
            if deps:
                self.streams[eng].append((deps, None, None, 0))

    def emit(self, stack):
        nc = self.nc
        sems = {}
        for k in self.semkeys:
            sems[k] = stack.enter_context(nc.semaphore("s_%s_%d" % k))
        block = stack.enter_context(nc.Block())
        streams = self.streams

        def run(engobj, lst):
            for deps, fn, key, inc in lst:
                for s, v in deps:
                    engobj.wait_ge(sems[s], v)
                if fn is not None:
                    fn(engobj).then_inc(sems[key], inc)

        @block.tensor
        def _(e):
            run(e, streams["pe"])

        @block.scalar
        def _(e):
            run(e, streams["act"])

        @block.vector
        def _(e):
            run(e, streams["dve"])

        @block.gpsimd
        def _(e):
            run(e, streams["pool"])

        @block.sync
        def _(e):
            run(e, streams["sp"])


class Arena:
    def __init__(self, ap):
        self.ap = ap
        self.n = ap.shape[1]
        self.off = 0

    def f32(self, n):
        a = self.ap[:, self.off:self.off + n]
        self.off += n
        assert self.off <= self.n, ("arena overflow", self.off, self.n)
        return a

    def bf16(self, n):
        nn = (n + 1) // 2
        a = self.ap[:, self.off:self.off + nn].bitcast(BF16)
        self.off += nn
        assert self.off <= self.n, ("arena overflow", self.off, self.n)
        return a

    def reset(self):
        self.off = 0


class Ring:
    def __init__(self, aps):
        self.items = [(a, Buf()) for a in aps]
        self.i = 0

    def get(self):
        it = self.items[self.i]
        self.i = (self.i + 1) % len(self.items)
        return it


def tsl(tt):
    return slice(tt * TW, (tt + 1) * TW)


def build(NL, dbg=False):
    nc = bass.Bass("TRN2", target_bir_lowering=False)

    def din(name, shape):
        return nc.dram_tensor(name, shape, F32, kind="ExternalInput").ap()

    xT = din("xT", [1024, T])
    pp = din("pp", [NL, 128, NPP])
    w_in = din("w_in", [NL, 1024, IN_COLS])
    gla_w_up = din("gla_w_up", [NL, 16, 512])
    gla_b_alpha = din("gla_b_alpha", [NL, 512])
    lru_w_a = din("lru_w_a", [NL, 4, 256, 256])
    lru_w_x = din("lru_w_x", [NL, 4, 256, 256])
    w_br = [din("w_branch_a", [NL, 1024, 1024]), din("w_branch_b", [NL, 1024, 1024]), din("w_branch_c", [NL, 1024, 1024])]
    w_out = din("w_out", [NL, 1024, 1024])
    w_up = din("w_mlp_up", [NL, 1024, 4096])
    w_dn = din("w_mlp_down", [NL, 4096, 1024])
    yT = nc.dram_tensor("yT", [1024, T], F32, kind="ExternalOutput").ap()
    dbg_t = None
    if dbg:
        dbg_t = nc.dram_tensor("dbg", [6, 1024, T], F32, kind="ExternalOutput").ap()

    with ExitStack() as st:
        def sb(name, shape, dt):
            return st.enter_context(nc.sbuf_tensor(name, shape, dt))

        def psum(name, shape, dt):
            return st.enter_context(nc.psum_tensor(name, shape, dt))

        S = Sched(nc)
        xbuf_t = sb("xbuf", [128, KC * T], F32)
        xb = xbuf_t[:].rearrange("p (k t) -> p k t", k=KC)
        XB = [[Buf() for _ in range(TT)] for _ in range(KC)]
        xn_t = sb("xn", [128, KC * T], BF16)
        xn = xn_t[:].rearrange("p (k t) -> p k t", k=KC)
        XN = [[Buf() for _ in range(TT)] for _ in range(KC)]
        ob_t = sb("ob", [128, KC * T], BF16)
        ob = ob_t[:].rearrange("p (k t) -> p k t", k=KC)
        OB = [[Buf() for _ in range(TT)] for _ in range(KC)]
        wsl_t = sb("wsl", [128, NW * KC * 256], BF16)
        wsl = [wsl_t[:, i * KC * 256:(i + 1) * KC * 256].rearrange("p (k n) -> p k n", k=KC) for i in range(NW)]
        WB = [Buf() for _ in range(NW)]
        ppt = sb("ppt", [128, NL * NPP], F32)
        PPB = Buf()
        cst_bf = sb("cst_bf", [128, 128 * 5 + 256 + 2048 + 4 * 512 * 2], BF16)
        CB = Buf()
        cst_f = sb("cst_f", [128, 4], F32)
        ar_t = sb("arena", [128, 12200], F32)
        AR = Arena(ar_t[:])
        AR2 = Arena(xbuf_t[:])
        YT = [Buf() for _ in range(KC)]
        DBG = Buf()

        pbanks = [psum("pb%d" % i, [128, 512], F32) for i in range(7)]
        ptr_t = psum("ptr", [128, 1024], BF16)
        ptr = ptr_t[:]
        PTR = Buf()
        banks = [(pbanks[i][:], Buf()) for i in range(7)] + [(ptr_t[:].bitcast(F32), PTR)]
        P4, P5, P6 = banks[4][0], banks[5][0], banks[6][0]
        PB4, PB5, PB6 = banks[4][1], banks[5][1], banks[6][1]
        prot = Ring([])
        ALLB = list(range(8))

        def set_ring(idx):
            prot.items = [banks[i] for i in idx]
            prot.i = 0

        set_ring(ALLB)

        def getps():
            return prot.get()

        def mm(out, lhsT, rhs, start, stop, r, w, sgc=False):
            if sgc:
                S.op("pe", lambda e: e.matmul(out, lhsT, rhs, start=start, stop=stop, skip_group_check=True), r, w)
            else:
                S.op("pe", lambda e: e.matmul(out, lhsT, rhs, start=start, stop=stop), r, w)

        def act(out, in_, func, r, w, bias=None, scale=None):
            kw = {}
            if bias is not None:
                kw["bias"] = bias
            if scale is not None:
                kw["scale"] = scale
            S.op("act", lambda e: e.activation(out=out, in_=in_, func=func, **kw), r, w)

        def tto(eng, out, in0, in1, op, r, w):
            S.op(eng, lambda e: e.tensor_tensor(out=out, in0=in0, in1=in1, op=op), r, w)

        def stt(eng, out, in0, scalar, in1, op0, op1, r, w):
            S.op(eng, lambda e: e.scalar_tensor_tensor(out=out, in0=in0, scalar=scalar, in1=in1, op0=op0, op1=op1), r, w)

        def tsc(eng, out, in0, s1, s2, op0, op1, r, w):
            S.op(eng, lambda e: e.tensor_scalar(out=out, in0=in0, scalar1=s1, scalar2=s2, op0=op0, op1=op1), r, w)

        def tsm(eng, out, in0, s1, r, w):
            S.op(eng, lambda e: e.tensor_scalar_mul(out=out, in0=in0, scalar1=s1), r, w)

        def cp(eng, out, in_, r, w):
            if eng == "act":
                S.op("act", lambda e: e.activation(out=out, in_=in_, func=AF.Copy), r, w)
            else:
                S.op(eng, lambda e: e.tensor_copy(out=out, in_=in_), r, w)

        def memset(eng, ap, val, w):
            S.op(eng, lambda e: e.memset(ap, val), (), w)

        def recip(out, in_, r, w):
            S.op("dve", lambda e: e.reciprocal(out=out, in_=in_), r, w)

        def dma(q, out, in_, r, w):
            S.dma(q, lambda e: e.dma_start(out=out, in_=in_), r, w)

        def asel(ap, pattern, cmp, base, cm, w):
            S.op("pool", lambda e: e.affine_select(out=ap, in_=ap, pattern=pattern, compare_op=cmp, fill=0.0, base=base, channel_multiplier=cm), w, w)

        o = 0
        ident_bf = cst_bf[:, o:o + 128]; o += 128
        ones_bf = cst_bf[:, o:o + 128]; o += 128
        maskG = cst_bf[:, o:o + 128]; o += 128
        TriS = cst_bf[:, o:o + 128]; o += 128
        NU = cst_bf[:, o:o + 128]; o += 128
        Eb = cst_bf[:, o:o + 256].rearrange("p (b m) -> p b m", b=16); o += 256
        Xb = cst_bf[:, o:o + 2048].rearrange("p (a s) -> p a s", a=16); o += 2048
        DM = []
        NM = []
        for r_ in range(4):
            DM.append(cst_bf[:, o:o + 512]); o += 512
        for r_ in range(4):
            NM.append(cst_bf[:, o:o + 512]); o += 512
        nm_f = NM[0].tensor if False else None
        br_area = cst_bf[:, 7040 - 2048:7040].bitcast(F32)
        br_gr = Ring([br_area[:, 0:TW], br_area[:, TW:2 * TW]])
        eps_t = cst_f[:, 0:1]
        one_t = cst_f[:, 1:2]
        memset("dve", cst_f[:, 0:1], EPS, [CB])
        memset("dve", cst_f[:, 1:2], 1.0, [CB])
        tmpc = AR.f32(2048)
        TB = Buf()

        def mk(dst, np_, nf, val, pattern, cmp, base, cm, post=None):
            t = tmpc[0:np_, 0:nf]
            memset("pool", t, val, [TB])
            if pattern is not None:
                tv = t if len(pattern) == 1 else t.rearrange("p (a b) -> p a b", a=pattern[0][1])
                asel(tv, pattern, cmp, base, cm, [TB])
            if post is not None:
                post(t)
            cp("dve", dst, t, [TB], [CB])

        mk(ident_bf, 128, 128, 1.0, [[-1, 128]], ALU.is_equal, 0, 1)
        mk(ones_bf, 128, 128, 1.0, None, None, 0, 0)
        mk(maskG, 128, 128, 1.0, [[1, 128]], ALU.is_ge, 0, -1, post=lambda t: memset("pool", t[0:64, 64:128], 0.0, [TB]))
        tsm("dve", TriS, maskG, -1.0 / 16.0, [CB], [CB])
        mk(NU, 128, 128, -1.0, [[-1, 128]], ALU.is_ge, 0, 1)
        nones_bf = cst_bf[:, 640:768]
        mk(nones_bf, 128, 128, -1.0, None, None, 0, 0)
        mk(cst_bf[0:16, 896:896 + 2048], 16, 2048, -1.0, [[-1, 16], [0, 128]], ALU.is_gt, 0, 1)
        for r_ in range(4):
            mk(DM[r_], 128, 512, 1.0, [[1, 512]], ALU.is_gt, -128 * r_, -1)
        for l in range(NL):
            dma("sp", ppt[:, l * NPP:(l + 1) * NPP], pp[l], [], [PPB])

        def par(l, col):
            return ppt[:, l * NPP + col:l * NPP + col + 1]

        class WQ:
            def __init__(self):
                self.plan = []
                self.issued = 0
                self.consumed = 0

            def extend(self, items):
                self.plan.extend(items)

            def get(self, D=2):
                while self.issued < min(len(self.plan), self.consumed + 1 + D):
                    src, n = self.plan[self.issued]
                    slot = self.issued % NW
                    dma("pool", wsl[slot][:, :, 0:n], src.rearrange("(k p) n -> p k n", p=128), [], [WB[slot]])
                    self.issued += 1
                slot = self.consumed % NW
                self.consumed += 1
                return slot

        wq = WQ()

        def proj(slot, c0, n, tt, src=None, RB=None):
            if src is None:
                src, RB = xn, XN
            ps, pb = getps()
            for k in range(KC):
                mm(ps[0:n, :], wsl[slot][:, k, c0:c0 + n], src[:, k, tsl(tt)], k == 0, k == KC - 1, [WB[slot], RB[k][tt]], [pb])
            return ps, pb

        def dump(idx, src, RBs, bf):
            if not dbg:
                return
            for k in range(KC):
                dma("pool" if bf else "sp", dbg_t[idx, k * 128:(k + 1) * 128, :], src[:, k, :], RBs[k], [DBG])

        def rmsnorm(l, gcol):
            set_ring(ALLB)
            AR.reset()
            sqr = Ring([AR.bf16(TW) for _ in range(3)])
            srr = Ring([AR.f32(TW) for _ in range(2)])
            for tt in range(TT):
                ps, pb = getps()
                for k in range(KC):
                    sq, sqb = sqr.get()
                    act(sq, xb[:, k, tsl(tt)], AF.Square, [XB[k][tt]], [sqb])
                    mm(ps, ones_bf, sq, k == 0, k == KC - 1, [sqb, CB], [pb])
                sr, srb = srr.get()
                act(sr, ps, AF.Ln, [pb, CB], [srb], bias=eps_t, scale=1.0 / 1024.0)
                act(sr, sr, AF.Exp, [srb], [srb], scale=-0.5)
                for k in range(KC):
                    stt("dve", xn[:, k, tsl(tt)], xb[:, k, tsl(tt)], par(l, gcol + k), sr, ALU.mult, ALU.mult,
                        [XB[k][tt], srb, PPB], [XN[k][tt]])

        def spill_x():
            for k in range(KC):
                dma("sp", yT[k * 128:(k + 1) * 128, :], xb[:, k, :], XB[k], [YT[k]])

        def reload_x():
            for k in range(KC):
                dma("sp", xb[:, k, :], yT[k * 128:(k + 1) * 128, :], [YT[k]], XB[k])

        def gla(l):
            set_ring([0, 1, 2, 3, 6])
            AR.reset()
            AR2.reset()
            A = AR2
            adT = A.bf16(T); ADT = [Buf() for _ in range(TT)]
            wupa = A.bf16(512); WUPA = Buf()
            e1r = Ring([A.f32(TW) for _ in range(2)])
            Pr = Ring([A.bf16(TW) for _ in range(2)])
            ecr = Ring([A.f32(TW) for _ in range(2)])
            eir = Ring([A.f32(TW) for _ in range(2)])
            qd = A.bf16(T); QD = [Buf() for _ in range(TT)]
            ki = A.bf16(T); KI = [Buf() for _ in range(TT)]
            ki_tm = A.bf16(T); KITM = [Buf() for _ in range(TT)]
            v_tm = A.bf16(16 * 256); VTM = [Buf() for _ in range(8)]
            sg = [A.bf16(T), A.bf16(T)]; SG = [[Buf() for _ in range(TT)] for _ in range(2)]
            scr = Ring([A.bf16(128) for _ in range(3)])
            dec = A.f32(32); DEC = Buf()
            sqr = Ring([A.bf16(TW) for _ in range(2)])
            srr = Ring([A.f32(TW) for _ in range(2)])
            posb = [Ring([A.f32(TW), A.f32(TW)]), Ring([A.f32(TW), A.f32(TW)])]
            kvs = AR.f32(16 * 256); KVS = [Buf() for _ in range(16)]
            S_all = AR.f32(17 * 256); SAL = [Buf() for _ in range(17)]
            S_bfa = AR.bf16(16 * 256); SBA = [Buf() for _ in range(4)]
            for tt in range(TT):
                memset("dve", adT[0:33, tsl(tt)], 1.0, [ADT[tt]])
            memset("dve", wupa[0:33, :], 0.0, [WUPA])
            dma("pool", wupa[0:16, :], gla_w_up[l], [], [WUPA])
            dma("pool", wupa[32:33, :], gla_b_alpha[l:l + 1, :], [], [WUPA])
            plan = [(w_in[l][:, C_GD:C_GD + 16], 16)]
            for h in range(4):
                plan += [(w_in[l][:, C_GQ + h * 128:C_GQ + (h + 1) * 128], 128),
                         (w_in[l][:, C_GK + h * 128:C_GK + (h + 1) * 128], 128),
                         (w_in[l][:, C_GV + h * 256:C_GV + (h + 1) * 256], 256),
                         (w_in[l][:, C_GG + h * 256:C_GG + (h + 1) * 256], 256)]
            wq.extend(plan)
            s_ = wq.get(D=1)
            for tt in range(TT):
                ps, pb = proj(s_, 0, 16, tt)
                cp("act", adT[0:16, tsl(tt)], ps[0:16, :], [pb], [ADT[tt]])
            for h in range(4):
                s_q = wq.get(D=2)
                s_k = wq.get(D=2)
                for tt in range(TT + 1):
                    if tt < TT:
                        psy, pyb = getps()
                        for mi in range(4):
                            m = tt * 4 + mi
                            mm(psy[:, mi * 128:(mi + 1) * 128], adT[0:33, m * 128:(m + 1) * 128], wupa[0:33, h * 128:(h + 1) * 128],
                               True, True, [ADT[tt], WUPA], [pyb])
                        e1, e1b = e1r.get()
                        act(e1, psy, AF.Exp, [pyb], [e1b], scale=-1.0)
                        Pt, Ptb = Pr.get()
                        act(Pt, e1, AF.Ln, [e1b, CB], [Ptb], bias=one_t)
                        psq, pqb = proj(s_q, 0, 128, tt)
                        psk, pkb = proj(s_k, 0, 128, tt)
                        psc, pcb = getps()
                        for mi in range(4):
                            mm(psc[:, mi * 128:(mi + 1) * 128], Pt[:, mi * 128:(mi + 1) * 128], TriS, True, True, [Ptb, CB], [pcb])
                        ec, ecb = ecr.get()
                        ei, eib = eir.get()
                        act(ec, psc, AF.Exp, [pcb], [ecb])
                        act(ei, psc, AF.Exp, [pcb], [eib], scale=-1.0)
                        cp("dve", dec[:, tt * 8:(tt + 1) * 8], ec[:, 63::64], [ecb], [DEC])
                        stt("dve", qd[:, tsl(tt)], psq, 128.0 ** -0.5, ec, ALU.mult, ALU.mult, [pqb, ecb], [QD[tt]])
                        tto("dve", ki[:, tsl(tt)], psk, ei, ALU.mult, [pkb, eib], [KI[tt]])
                    if tt >= 1:
                        t2 = tt - 1
                        for mi in range(4):
                            m = t2 * 4 + mi
                            S.op("pe", (lambda o_, i_: (lambda e: e.transpose(o_, i_, ident_bf)))(ptr[:, mi * 128:(mi + 1) * 128], ki[:, m * 128:(m + 1) * 128]),
                                 [KI[t2], CB], [PTR])
                        cp("act", ki_tm[:, tsl(t2)], ptr[:, 0:512], [PTR], [KITM[t2]])
                s_v = wq.get(D=2)
                for m2 in range(8):
                    ps, pb = getps()
                    for mj in range(2):
                        m = m2 * 2 + mj
                        for k in range(KC):
                            mm(ps[:, mj * 256:(mj + 1) * 256], xn[:, k, m * 128:(m + 1) * 128], wsl[s_v][:, k, 0:256],
                               k == 0, k == KC - 1, [XN[k][m // 4], WB[s_v]], [pb])
                    cp("act", v_tm[:, m2 * 512:(m2 + 1) * 512], ps, [pb], [VTM[m2]])
                s_g = wq.get(D=2)
                for jb in range(2):
                    for tt in range(TT):
                        ps, pb = proj(s_g, jb * 128, 128, tt)
                        act(sg[jb][:, tsl(tt)], ps, AF.Silu, [pb], [SG[jb][tt]])
                pend = []

                def normrest(tt, pcs, h=h):
                    pn, pnb = getps()
                    for jb in range(2):
                        sq, sqb = sqr.get()
                        act(sq, pcs[jb][0], AF.Square, [pcs[jb][1]], [sqb])
                        mm(pn, ones_bf, sq, jb == 0, jb == 1, [sqb, CB], [pnb])
                    sr, srb = srr.get()
                    act(sr, pn, AF.Ln, [pnb, CB], [srb], bias=eps_t, scale=1.0 / 256.0)
                    act(sr, sr, AF.Exp, [srb], [srb], scale=-0.5)
                    for jb in range(2):
                        pc, pcb_ = pcs[jb]
                        tto("dve", pc, pc, sr, ALU.mult, [pcb_, srb], [pcb_])
                        stt("dve", ob[:, h * 2 + jb, tsl(tt)], pc, par(l, P_GLAN + jb), sg[jb][:, tsl(tt)], ALU.mult, ALU.mult,
                            [pcb_, PPB, SG[jb][tt]], [OB[h * 2 + jb][tt]])

                for hs in range(2):
                    for ci in range(16):
                        c = hs * 16 + ci
                        m = c // 2
                        half = c % 2
                        pkv, pkb = getps()
                        mm(pkv[:, 0:256], ki_tm[half * 64:(half + 1) * 64, m * 128:(m + 1) * 128],
                           v_tm[half * 64:(half + 1) * 64, m * 256:(m + 1) * 256], True, True, [KITM[m // 4], VTM[m // 2]], [pkb])
                        act(kvs[:, ci * 256:(ci + 1) * 256], pkv[:, 0:256], AF.Identity, [pkb, DEC], [KVS[ci]], scale=dec[:, c:c + 1])
                    if hs == 0:
                        memset("dve", S_all[:, 0:256], 0.0, [SAL[0]])
                    else:
                        cp("dve", S_all[:, 0:256], S_all[:, 16 * 256:17 * 256], [SAL[16]], [SAL[0]])
                    for ci in range(16):
                        c = hs * 16 + ci
                        stt("dve", S_all[:, (ci + 1) * 256:(ci + 2) * 256], S_all[:, ci * 256:(ci + 1) * 256], dec[:, c:c + 1],
                            kvs[:, ci * 256:(ci + 1) * 256], ALU.mult, ALU.add, [SAL[ci], DEC, KVS[ci]], [SAL[ci + 1]])
                        if ci % 4 == 3:
                            g4 = ci // 4
                            cp("act", S_bfa[:, g4 * 1024:(g4 + 1) * 1024], S_all[:, g4 * 1024:(g4 + 1) * 1024],
                               [SAL[g4 * 4 + j] for j in range(4)], [SBA[g4]])
                    for tl in range(2):
                        tt = hs * 2 + tl
                        po = [P4, P5]
                        POB = [PB4, PB5]
                        scl = {}
                        for it in range(5):
                            if it < 4:
                                mi = it
                                m = tt * 4 + mi
                                pss, psb = getps()
                                mm(pss[:, 0:128], ki[:, m * 128:(m + 1) * 128], qd[:, m * 128:(m + 1) * 128], True, True, [KI[tt], QD[tt]], [psb])
                                sc, scb = scr.get()
                                tto("dve", sc, pss[:, 0:128], maskG, ALU.mult, [psb, CB], [scb])
                                scl[mi] = (sc, scb)
                            if it >= 1:
                                mi = it - 1
                                m = tt * 4 + mi
                                sc, scb = scl.pop(mi)
                                for half in range(2):
                                    c = 2 * m + half
                                    ci = c - hs * 16
                                    for jb in range(2):
                                        col = mi * 128 + half * 64
                                        mm(po[jb][:, col:col + 64], S_bfa[:, ci * 256 + jb * 128:ci * 256 + (jb + 1) * 128], qd[:, c * 64:(c + 1) * 64],
                                           True, False, [SBA[ci // 4], QD[tt]], [POB[jb]])
                                        mm(po[jb][:, col:col + 64], v_tm[:, m * 256 + jb * 128:m * 256 + (jb + 1) * 128], sc[:, half * 64:(half + 1) * 64],
                                           False, True, [VTM[m // 2], scb], [POB[jb]])
                        if pend:
                            normrest(*pend.pop())
                        pcs = []
                        for jb in range(2):
                            pc, pcb_ = posb[jb].get()
                            cp("act", pc, po[jb], [POB[jb]], [pcb_])
                            pcs.append((pc, pcb_))
                        pend.append((tt, pcs))
                while pend:
                    normrest(*pend.pop())

        def branch(l, b):
            set_ring(ALLB)
            gr = br_gr
            plan = []
            for nbp in range(4):
                plan += [(w_br[b][l][:, nbp * 256:(nbp + 1) * 256], 256),
                         (w_in[l][:, C_GATE + b * 1024 + nbp * 256:C_GATE + b * 1024 + (nbp + 1) * 256], 256)]
            wq.extend(plan)
            for nbp in range(4):
                s1 = wq.get(D=2)
                s2 = wq.get(D=2)
                for nbi in range(2):
                    nb = nbp * 2 + nbi
                    for tt in range(TT):
                        ps1, p1b = proj(s1, nbi * 128, 128, tt, ob, OB)
                        ps2, p2b = proj(s2, nbi * 128, 128, tt)
                        g, gb = gr.get()
                        act(g, ps2, AF.Sigmoid, [p2b, PPB], [gb], bias=par(l, P_BG + b * 8 + nb))
                        if b == 0:
                            tto("dve", xb[:, nb, tsl(tt)], ps1, g, ALU.mult, [p1b, gb], [XB[nb][tt]])
                        else:
                            tto("dve", g, ps1, g, ALU.mult, [p1b, gb], [gb])
                            tto("dve", xb[:, nb, tsl(tt)], xb[:, nb, tsl(tt)], g, ALU.add, [gb, XB[nb][tt]], [XB[nb][tt]])

        def lru(l):
            set_ring(ALLB)
            AR.reset()
            A = AR
            cl = A.f32(8); CL = Buf()
            tmp8 = A.f32(8)
            lxbf = [A.bf16(T + 4), A.bf16(T + 4)]; LXB = [[Buf() for _ in range(TT)] for _ in range(2)]
            dg = [[A.bf16(128) for _ in range(4)] for _ in range(2)]; DG = Buf()
            wab = A.bf16(512).rearrange("p (i j) -> p i j", i=2); wxb = A.bf16(512).rearrange("p (i j) -> p i j", i=2); WAB = Buf(); WXB = Buf()
            xcf = [Ring([A.f32(TW) for _ in range(1)]) for _ in range(2)]
            xcb = [Ring([A.bf16(TW) for _ in range(1)]) for _ in range(2)]
            rr = Ring([A.f32(TW) for _ in range(2)])
            igr = Ring([A.f32(TW) for _ in range(2)])
            n2r = Ring([A.f32(TW) for _ in range(2)])
            hr = [Ring([A.f32(TW) for _ in range(2)]) for _ in range(2)]
            glr = Ring([A.f32(TW) for _ in range(2)])
            g2r = Ring([A.f32(TW) for _ in range(2)])
            act(tmp8, ppt[:, l * NPP + P_LAM:l * NPP + P_LAM + 8], AF.Exp, [PPB], [CL], scale=-1.0)
            act(tmp8, tmp8, AF.Ln, [CL, CB], [CL], bias=one_t)
            tsm("dve", cl, tmp8, -8.0, [CL], [CL])
            plan = []
            for hb in range(4):
                plan += [(w_in[l][:, C_LX + hb * 256:C_LX + (hb + 1) * 256], 256), (w_in[l][:, C_LG + hb * 256:C_LG + (hb + 1) * 256], 256)]
            wq.extend(plan)
            for hb in range(4):
                slx = wq.get(D=2)
                slg = wq.get(D=2)
                dma("pool", wab, lru_w_a[l, hb].rearrange("(i p) j -> p i j", p=128), [], [WAB])
                dma("pool", wxb, lru_w_x[l, hb].rearrange("(i p) j -> p i j", p=128), [], [WXB])
                for cbl in range(2):
                    memset("dve", lxbf[cbl][:, 0:3], 0.0, [LXB[cbl][0]])
                    for w_ in range(4):
                        tsm("dve", dg[cbl][w_], ident_bf, par(l, P_CW + (hb * 2 + cbl) * 4 + w_), [CB, PPB], [DG])
                hprev = [None, None]
                for tt in range(TT):
                    for cbl in range(2):
                        ps, pb = proj(slx, cbl * 128, 128, tt)
                        cp("act", lxbf[cbl][:, 3 + tt * TW:3 + (tt + 1) * TW], ps, [pb], [LXB[cbl][tt]])
                    xf = []
                    xh = []
                    for cbl in range(2):
                        ps, pb = getps()
                        rd = [LXB[cbl][tt], DG] + ([LXB[cbl][tt - 1]] if tt > 0 else [])
                        for w_ in range(4):
                            mm(ps, dg[cbl][w_], lxbf[cbl][:, tt * TW + w_:tt * TW + w_ + TW], w_ == 0, w_ == 3, rd, [pb])
                        f_, fb = xcf[cbl].get()
                        act(f_, ps, AF.Identity, [pb, PPB], [fb], bias=par(l, P_CB + hb * 2 + cbl))
                        h_, hb_ = xcb[cbl].get()
                        cp("dve", h_, f_, [fb], [hb_])
                        xf.append((f_, fb))
                        xh.append((h_, hb_))
                    rs, igs, gls, g2s, n2s, hhs = [], [], [], [], [], []
                    for jb in range(2):
                        c = hb * 2 + jb
                        psr, prb = getps()
                        for ib in range(2):
                            mm(psr, wab[:, ib, jb * 128:(jb + 1) * 128], xh[ib][0], ib == 0, ib == 1, [WAB, xh[ib][1]], [prb])
                        psi, pib = getps()
                        for ib in range(2):
                            mm(psi, wxb[:, ib, jb * 128:(jb + 1) * 128], xh[ib][0], ib == 0, ib == 1, [WXB, xh[ib][1]], [pib])
                        r_, rb = rr.get()
                        act(r_, psr, AF.Sigmoid, [prb, PPB], [rb], bias=par(l, P_BA + c))
                        ig, igb = igr.get()
                        act(ig, psi, AF.Sigmoid, [pib, PPB], [igb], bias=par(l, P_BX + c))
                        rs.append((r_, rb))
                        igs.append((ig, igb))
                    for jb in range(2):
                        c = hb * 2 + jb
                        a_, ab = rs[jb]
                        act(a_, a_, AF.Exp, [ab, CL], [ab], scale=cl[:, c:c + 1])
                    for jb in range(2):
                        psg, pgb = proj(slg, jb * 128, 128, tt)
                        gl, glb = glr.get()
                        act(gl, psg, AF.Identity, [pgb], [glb])
                        gls.append((gl, glb))
                    for jb in range(2):
                        a_, ab = rs[jb]
                        n2, n2b = n2r.get()
                        stt("dve", n2, a_, -1.0, a_, ALU.mult, ALU.mult, [ab], [n2b])
                        n2s.append((n2, n2b))
                        gl, glb = gls[jb]
                        g2, g2b = g2r.get()
                        stt("dve", g2, gl, 0.044715, gl, ALU.mult, ALU.mult, [glb], [g2b])
                        stt("dve", g2, g2, 1.0, gl, ALU.add, ALU.mult, [g2b, glb], [g2b])
                        g2s.append((g2, g2b))
                    for jb in range(2):
                        n2, n2b = n2s[jb]
                        act(n2, n2, AF.Sqrt, [n2b, CB], [n2b], bias=one_t)
                    for jb in range(2):
                        g2, g2b = g2s[jb]
                        act(g2, g2, AF.Sigmoid, [g2b], [g2b], scale=1.5957691216057308)
                    for jb in range(2):
                        c = hb * 2 + jb
                        a_, ab = rs[jb]
                        ig, igb = igs[jb]
                        n2, n2b = n2s[jb]
                        gl, glb = gls[jb]
                        g2, g2b = g2s[jb]
                        u_, ub = ig, igb
                        tto("dve", u_, n2, ig, ALU.mult, [n2b, igb], [ub])
                        tto("dve", u_, u_, xf[jb][0], ALU.mult, [ub, xf[jb][1]], [ub])
                        hh, hhb = hr[jb].get()
                        if hprev[jb] is None:
                            S.op("dve", (lambda o_, a0, u0: (lambda e: e.tensor_tensor_scan(out=o_, data0=a0, data1=u0, initial=0.0, op0=ALU.mult, op1=ALU.add)))(hh, a_, u_),
                                 [ab, ub], [hhb])
                        else:
                            hp, hpb = hprev[jb]
                            S.op("dve", (lambda o_, a0, u0, i0: (lambda e: e.tensor_tensor_scan(out=o_, data0=a0, data1=u0, initial=i0, op0=ALU.mult, op1=ALU.add)))(hh, a_, u_, hp[:, TW - 1:TW]),
                                 [ab, ub, hpb], [hhb])
                        hprev[jb] = (hh, hhb)
                        tto("dve", gl, gl, hh, ALU.mult, [glb, hhb], [glb])
                        tto("dve", ob[:, c, tsl(tt)], gl, g2, ALU.mult, [glb, g2b], [OB[c][tt]])

        def sbattn(l):
            set_ring([0, 1, 2, 3, 4, 7])
            AR.reset()
            A = AR
            Lkr = Ring([A.bf16(TW) for _ in range(5)])
            Rbr = Ring([A.bf16(TW) for _ in range(5)])
            R32 = A.f32(TW); R32B = Buf()
            qn = A.bf16(T); QN = [Buf() for _ in range(TT)]
            kn = A.bf16(T); KN = [Buf() for _ in range(TT)]
            v_tm = A.bf16(T); VT = [Buf() for _ in range(4)]
            Er = Ring([A.f32(TW) for _ in range(2)])
            wr = Ring([A.bf16(TW) for _ in range(4)])
            sqr = Ring([A.bf16(TW) for _ in range(2)])
            srr = Ring([A.f32(TW) for _ in range(2)])
            gq2 = A.f32(2); GQ = Buf()
            tsm("dve", gq2[:, 0:1], par(l, P_SQG), 128.0 ** -0.5, [PPB], [GQ])
            cp("dve", gq2[:, 1:2], par(l, P_SKG), [PPB], [GQ])
            plan = []
            for h in range(8):
                plan += [(w_in[l][:, C_SQ + h * 128:C_SQ + (h + 1) * 128], 128), (w_in[l][:, C_SK + h * 128:C_SK + (h + 1) * 128], 128),
                         (w_in[l][:, C_SV + h * 128:C_SV + (h + 1) * 128], 128)]
            wq.extend(plan)
            for h in range(8):
                slots_qk = [wq.get(D=2), wq.get(D=2)]
                items = [(which, tt) for which in range(2) for tt in range(TT)]
                stA = {}

                def stageA(i):
                    which, tt = items[i]
                    ps, pb = proj(slots_qk[which], 0, 128, tt)
                    sq, sqb = sqr.get()
                    act(sq, ps, AF.Square, [pb], [sqb])
                    stA[i] = (ps, pb, sq, sqb)

                def stageB(i):
                    which, tt = items[i]
                    dst, DBs = ((qn, QN), (kn, KN))[which]
                    ps, pb, sq, sqb = stA.pop(i)
                    pn, pnb = getps()
                    mm(pn, ones_bf, sq, True, True, [sqb, CB], [pnb])
                    sr, srb = srr.get()
                    act(sr, pn, AF.Ln, [pnb, CB], [srb], bias=eps_t, scale=1.0 / 128.0)
                    act(sr, sr, AF.Exp, [srb], [srb], scale=-0.5)
                    stt("dve", dst[:, tsl(tt)], ps, gq2[:, which:which + 1], sr, ALU.mult, ALU.mult, [pb, srb, GQ], [DBs[tt]])

                for i in range(len(items) + 1):
                    if i < len(items):
                        stageA(i)
                    if i >= 1:
                        stageB(i - 1)
                s_v = wq.get(D=2)
                for m4 in range(4):
                    ps, pb = getps()
                    for mi in range(4):
                        m = m4 * 4 + mi
                        for k in range(KC):
                            mm(ps[:, mi * 128:(mi + 1) * 128], xn[:, k, m * 128:(m + 1) * 128], wsl[s_v][:, k, 0:128],
                               k == 0, k == KC - 1, [XN[k][m4], WB[s_v]], [pb])
                    cp("act", v_tm[:, m4 * 512:(m4 + 1) * 512], ps, [pb], [VT[m4]])
                L1, L2 = 2, 2
                blocks = [(qt, a) for qt in range(TT) for a in reversed(range(4 * (qt + 1)))]
                stt_ = {}
                wl = {}
                curR = [None]

                def S1(qt, a):
                    nk = 4 * (qt + 1)
                    r_ = a - 4 * qt
                    c0 = 128 * r_ if r_ >= 0 else 0
                    cs_ = slice(c0, TW)
                    psz, pzb = getps()
                    mm(psz[:, cs_], kn[:, a * 128:(a + 1) * 128], qn[:, qt * TW + c0:(qt + 1) * TW], True, True, [KN[a // 4], QN[qt]], [pzb])
                    E, Eb_ = Er.get()
                    act(E[:, cs_], psz[:, cs_], AF.Exp, [pzb], [Eb_])
                    La, Lab = Lkr.get()
                    act(La[:, cs_], E[:, cs_], AF.Ln, [Eb_, CB], [Lab], bias=one_t)
                    if r_ >= 0:
                        tto("dve", La[:, c0:c0 + 128], La[:, c0:c0 + 128], DM[0][:, 0:128], ALU.mult, [Lab, CB], [Lab])
                    myR = None if a == nk - 1 else curR[0]
                    if a > 0:
                        if a == nk - 1:
                            if c0 > 0:
                                memset("dve", R32[:, 0:c0], 0.0, [R32B])
                            cp("dve", R32[:, cs_], La[:, cs_], [Lab], [R32B])
                        else:
                            tto("dve", R32[:, cs_], R32[:, cs_], La[:, cs_], ALU.add, [R32B, Lab], [R32B])
                        Rb, Rbb = Rbr.get()
                        cp("dve", Rb, R32, [R32B], [Rbb])
                        curR[0] = (Rb, Rbb)
                    stt_[(qt, a)] = (psz, pzb, La, Lab, myR, c0)

                def S2(qt, a):
                    psz, pzb, La, Lab, myR, c0 = stt_.pop((qt, a))
                    cs_ = slice(c0, TW)
                    mm(psz[:, cs_], NU, La[:, cs_], False, myR is None, [CB, Lab], [pzb], sgc=True)
                    if myR is not None:
                        mm(psz[:, cs_], nones_bf, myR[0][:, cs_], False, True, [CB, myR[1]], [pzb], sgc=True)
                    w_, wb_ = wr.get()
                    act(w_[:, cs_], psz[:, cs_], AF.Exp, [pzb], [wb_])
                    if a >= 4 * qt:
                        tto("dve", w_[:, c0:c0 + 128], w_[:, c0:c0 + 128], DM[0][:, 0:128], ALU.mult, [wb_, CB], [wb_])
                    if a == 4 * (qt + 1) - 1 and c0 > 0:
                        memset("dve", w_[:, 0:c0], 0.0, [wb_])
                    wl[(qt, a)] = (w_, wb_, c0)

                def S3(qt, a):
                    nk = 4 * (qt + 1)
                    po, pob = (P5, PB5) if qt % 2 == 0 else (P6, PB6)
                    w_, wb_, c0 = wl.pop((qt, a))
                    vv = v_tm[:, a * 128:(a + 1) * 128]
                    if a == nk - 1:
                        mm(po, vv, w_, True, a == 0, [VT[a // 4], wb_], [pob], sgc=True)
                    else:
                        mm(po[:, c0:TW], vv, w_[:, c0:TW], False, a == 0, [VT[a // 4], wb_], [pob], sgc=True)
                    if a == 0:
                        cp("dve", ob[:, h, tsl(qt)], po, [pob], [OB[h][qt]])

                nb_ = len(blocks)
                for it in range(nb_ + L1 + L2):
                    if it < nb_:
                        S1(*blocks[it])
                    if L1 <= it < nb_ + L1:
                        S2(*blocks[it - L1])
                    if it >= L1 + L2:
                        S3(*blocks[it - L1 - L2])

        def wout(l):
            set_ring(ALLB)
            AR.reset()
            for nb in range(KC):
                for tt in range(TT):
                    cp("act" if (nb + tt) % 2 == 0 else "dve", ob[:, nb, tsl(tt)], xb[:, nb, tsl(tt)], [XB[nb][tt]], [OB[nb][tt]])
            dump(4, ob, OB, True)
            reload_x()
            wq.extend([(w_out[l][:, i * 256:(i + 1) * 256], 256) for i in range(4)])
            for i in range(4):
                s_ = wq.get(D=2)
                for nbi in range(2):
                    nb = i * 2 + nbi
                    for tt in range(TT):
                        ps, pb = proj(s_, nbi * 128, 128, tt, ob, OB)
                        tto("dve", xb[:, nb, tsl(tt)], ps, xb[:, nb, tsl(tt)], ALU.add, [pb, XB[nb][tt]], [XB[nb][tt]])

        def mlp(l):
            rmsnorm(l, P_GMLP)
            rr = Ring([AR.f32(TW) for _ in range(3)])
            plan = []
            for fg in range(4):
                plan += [(w_up[l][:, fg * 1024 + i * 256:fg * 1024 + (i + 1) * 256], 256) for i in range(4)]
                plan += [(w_dn[l][fg * 1024:(fg + 1) * 1024, i * 256:(i + 1) * 256], 256) for i in range(4)]
            wq.extend(plan)
            for fg in range(4):
                for i in range(4):
                    s_ = wq.get(D=3)
                    for fbi in range(2):
                        fb = i * 2 + fbi
                        for tt in range(TT):
                            ps, pb = proj(s_, fbi * 128, 128, tt)
                            r_, rb = rr.get()
                            act(r_, ps, AF.Relu, [pb], [rb])
                            tto("dve", ob[:, fb, tsl(tt)], r_, r_, ALU.mult, [rb], [OB[fb][tt]])
                for i in range(4):
                    s_ = wq.get(D=3)
                    for nbi in range(2):
                        nb = i * 2 + nbi
                        for tt in range(TT):
                            ps, pb = proj(s_, nbi * 128, 128, tt, ob, OB)
                            tto("dve", xb[:, nb, tsl(tt)], ps, xb[:, nb, tsl(tt)], ALU.add, [pb, XB[nb][tt]], [XB[nb][tt]])

        S.barrier()
        for k in range(KC):
            dma("sp", xb[:, k, :], xT[k * 128:(k + 1) * 128, :], [], XB[k])
        for l in range(NL):
            rmsnorm(l, P_GMIX)
            if l == 0:
                dump(0, xn, XN, True)
            spill_x()
            S.barrier()
            gla(l)
            if l == 0:
                dump(1, ob, OB, True)
            S.barrier()
            branch(l, 0)
            lru(l)
            if l == 0:
                dump(2, ob, OB, True)
            S.barrier()
            branch(l, 1)
            sbattn(l)
            if l == 0:
                dump(3, ob, OB, True)
            S.barrier()
            branch(l, 2)
            wout(l)
            if l == 0:
                dump(5, xb, XB, False)
            mlp(l)
            S.barrier()
        for k in range(KC):
            dma("sp", yT[k * 128:(k + 1) * 128, :], xb[:, k, :], XB[k], [YT[k]])
        S.barrier()
        S.emit(st)
        print("instructions:", S.ninst, {e: len(S.streams[e]) for e in ENGS})
    return nc


def pack_params(inp, layers):
    def fm(v, nb):
        return np.ascontiguousarray(np.asarray(v, np.float32).reshape(nb, 128).T)

    out = np.zeros((len(layers), 128, NPP), np.float32)
    for i, l in enumerate(layers):
        out[i, :, P_GMIX:P_GMIX + 8] = fm(inp["norm_mix_g"][l], 8)
        out[i, :, P_GMLP:P_GMLP + 8] = fm(inp["norm_mlp_g"][l], 8)
        out[i, :, P_GLAN:P_GLAN + 2] = fm(inp["gla_norm_g"][l], 2)
        cw = np.asarray(inp["lru_conv_w"][l], np.float32)
        for cb in range(8):
            for w_ in range(4):
                out[i, :, P_CW + cb * 4 + w_] = cw[w_, cb * 128:(cb + 1) * 128]
        out[i, :, P_CB:P_CB + 8] = fm(inp["lru_conv_b"][l], 8)
        out[i, :, P_BA:P_BA + 8] = fm(inp["lru_b_a"][l], 8)
        out[i, :, P_BX:P_BX + 8] = fm(inp["lru_b_x"][l], 8)
        out[i, :, P_LAM:P_LAM + 8] = fm(inp["lru_lambda"][l], 8)
        out[i, :, P_SQG] = np.asarray(inp["sb_q_norm_g"][l], np.float32)
        out[i, :, P_SKG] = np.asarray(inp["sb_k_norm_g"][l], np.float32)
        out[i, :, P_BG:P_BG + 24] = fm(inp["b_gate"][l], 24)
    return out


_NC_CACHE = {}
WNAMES = ["w_in", "gla_w_up", "gla_b_alpha", "lru_w_a", "lru_w_x", "w_branch_a", "w_branch_b", "w_branch_c", "w_out", "w_mlp_up", "w_mlp_down"]


def _get_nc(NL, dbg=False):
    key = (NL, dbg)
    if key not in _NC_CACHE:
        _NC_CACHE[key] = build(NL, dbg)
    return _NC_CACHE[key]


def run_layers(inp, xT_list, layers, dbg=False, cores=8):
    nc = _get_nc(len(layers), dbg)
    ppk = pack_params(inp, layers)
    shared = {"pp": ppk}
    for n in WNAMES:
        shared[n] = np.ascontiguousarray(np.asarray(inp[n], np.float32)[layers[0]:layers[-1] + 1])
    in_maps = []
    for c in range(cores):
        d = dict(shared)
        d["xT"] = xT_list[c]
        in_maps.append(d)
    res = run_bass_kernel_spmd(nc, in_maps, core_ids=list(range(cores)))
    return res.results


def kernel(**inputs):
    x = np.asarray(inputs["x"], np.float32)
    B = x.shape[0]
    xT = [np.ascontiguousarray(x[b].T) for b in range(B)]
    if FUSED:
        res = run_layers(inputs, xT, list(range(DEPTH)))
        xT = [r["yT"] for r in res]
    else:
        for l in range(DEPTH):
            res = run_layers(inputs, xT, [l])
            xT = [np.ascontiguousarray(r["yT"]) for r in res]
    return np.stack([np.asarray(t).T for t in xT], axis=0).astype(np.float32)
```
